# Optimizing a Trainium2 kernel written in Bass

```python
import jax, jax.numpy as jnp
from jax import lax
import numpy as np

D_MODEL = 1024
BATCH = 8
SEQ = 4096
DEPTH = 2

F32 = jnp.float32
N_META = 16
BLOCK = 128
FRONT_PAD = (-N_META) % BLOCK
CONV_CH = D_MODEL // 2
CONV_WIDTH = 31
RWKV_HEAD = 64
RWKV_CH = D_MODEL - CONV_CH
RWKV_HEADS = RWKV_CH // RWKV_HEAD
DECAY_RANK = 64
AAA_RANK = 64
GATE_RANK = 128
RWKV_COLS = 3 * RWKV_CH + DECAY_RANK + AAA_RANK + GATE_RANK
IN0_COLS = 2 * CONV_CH + RWKV_COLS
HEAD_DIM = 64
N_Q_HEADS = D_MODEL // HEAD_DIM
N_KV_HEADS = max(1, N_Q_HEADS // 8)
GROUP = N_Q_HEADS // N_KV_HEADS
WINDOW = 128
ROPE_THETA = 10000.0
QKV_COLS = (N_Q_HEADS + 2 * N_KV_HEADS) * HEAD_DIM
D_FF_DENSE = ((8 * D_MODEL // 3 + 255) // 256) * 256
D_FF_EXPERT = 7 * D_MODEL // 2
N_EXPERTS = 8
TOP_K = 2
N_EVEN = (DEPTH + 1) // 2
N_ODD = DEPTH // 2
ALPHA = (2 * DEPTH) ** 0.25
BETA = (8 * DEPTH) ** -0.25
LN_EPS = 1e-5
LNX_EPS = 64e-5

kernel_name = "hybrid_conv_rwkv7_swa_sink_moe_deepnorm"


def layer_norm(x, g, b, eps=LN_EPS):
    xf = x.astype(F32)
    mu = jnp.mean(xf, -1, keepdims=True)
    var = jnp.mean(jnp.square(xf - mu), -1, keepdims=True)
    return ((xf - mu) * lax.rsqrt(var + eps) * g.astype(F32) + b.astype(F32)).astype(x.dtype)


def token_shift(z):
    return jnp.pad(z[:, :-1], ((0, 0), (1, 0), (0, 0)))


def conformer_conv(u, conv_w, conv_b, norm_g, norm_b):
    val, gate = jnp.split(u, 2, axis=-1)
    h = val * jax.nn.sigmoid(gate)
    h = lax.conv_general_dilated(h, conv_w[:, None, :], window_strides=(1,),
                                 padding=[(CONV_WIDTH - 1, 0)],
                                 dimension_numbers=('NWC', 'WIO', 'NWC'),
                                 feature_group_count=CONV_CH) + conv_b
    return jax.nn.silu(layer_norm(h, norm_g, norm_b))


def rwkv7_time_mix(z, shift_mu, w0, w2, a0, a2, g2, k_k, k_a, r_k, lnx_g, lnx_b):
    bsz, seqlen = z.shape[:2]
    z = z + (token_shift(z) - z) * shift_mu
    cuts = [int(c) for c in np.cumsum([RWKV_CH, RWKV_CH, RWKV_CH, DECAY_RANK, AAA_RANK])]
    r, k, v, w_lo, a_lo, g_lo = jnp.split(z, cuts, axis=-1)
    w_log = -jax.nn.softplus(-(w0 + jnp.tanh(w_lo) @ w2)) - 0.5
    decay = jnp.exp(-jnp.exp(w_log.astype(F32)))
    a = jax.nn.sigmoid(a0 + a_lo @ a2)
    g = jax.nn.sigmoid(g_lo) @ g2

    def heads(t):
        return t.reshape(bsz, seqlen, RWKV_HEADS, RWKV_HEAD).astype(F32)

    kk = heads(k * k_k)
    kk = kk / jnp.maximum(jnp.sqrt(jnp.sum(kk * kk, -1, keepdims=True)), 1e-12)
    k = k * (1.0 + (a - 1.0) * k_a)
    r_h, k_h, v_h, a_h, w_h = heads(r), heads(k), heads(v), heads(a), heads(decay)
    kka = kk * a_h

    def step(S, inp):
        r_t, w_t, k_t, v_t, kk_t, kka_t = inp
        sa = jnp.einsum('bhij,bhj->bhi', S, -kk_t)
        S = S * w_t[:, :, None, :] + sa[..., None] * kka_t[:, :, None, :] + v_t[..., None] * k_t[:, :, None, :]
        return S, jnp.einsum('bhij,bhj->bhi', S, r_t)

    xs = tuple(jnp.moveaxis(t, 1, 0) for t in (r_h, w_h, k_h, v_h, kk, kka))
    S0 = jnp.zeros((bsz, RWKV_HEADS, RWKV_HEAD, RWKV_HEAD), F32)
    _, y = lax.scan(step, S0, xs)
    y = jnp.moveaxis(y, 0, 1)
    mu = jnp.mean(y, -1, keepdims=True)
    var = jnp.mean(jnp.square(y - mu), -1, keepdims=True)
    y = ((y - mu) * lax.rsqrt(var + LNX_EPS)).reshape(bsz, seqlen, RWKV_CH) * lnx_g.astype(F32) + lnx_b.astype(F32)
    bonus = jnp.sum(r_h * k_h * r_k.astype(F32), -1, keepdims=True) * v_h
    y = (y + bonus.reshape(bsz, seqlen, RWKV_CH)) * g.astype(F32)
    return y.astype(z.dtype)


def rope(t, pos):
    half = HEAD_DIM // 2
    inv = ROPE_THETA ** (-jnp.arange(half, dtype=F32) / half)
    ang = pos.astype(F32)[:, None] * inv[None, :]
    cos = jnp.cos(ang)[None, :, None, :]
    sin = jnp.sin(ang)[None, :, None, :]
    t1 = t[..., :half].astype(F32)
    t2 = t[..., half:].astype(F32)
    return jnp.concatenate([t1 * cos - t2 * sin, t2 * cos + t1 * sin], -1).astype(t.dtype)


def sliding_window_sink_attention(h, w_qkv, b_qkv, sinks):
    bsz, seqlen, _ = h.shape
    qkv = h @ w_qkv + b_qkv
    q, k, v = jnp.split(qkv, [N_Q_HEADS * HEAD_DIM, (N_Q_HEADS + N_KV_HEADS) * HEAD_DIM], axis=-1)
    q = q.reshape(bsz, seqlen, N_Q_HEADS, HEAD_DIM)
    k = k.reshape(bsz, seqlen, N_KV_HEADS, HEAD_DIM)
    v = v.reshape(bsz, seqlen, N_KV_HEADS, HEAD_DIM)
    pos = jnp.arange(seqlen)
    q, k = rope(q, pos), rope(k, pos)
    padw = ((0, 0), (FRONT_PAD, 0), (0, 0), (0, 0))
    q, k, v = jnp.pad(q, padw), jnp.pad(k, padw), jnp.pad(v, padw)
    lp = seqlen + FRONT_PAD
    nb = lp // BLOCK
    qb = q.reshape(bsz, nb, BLOCK, N_KV_HEADS, GROUP, HEAD_DIM)

    def band(t):
        tb = t.reshape(bsz, nb, BLOCK, N_KV_HEADS, HEAD_DIM)
        prev = jnp.pad(tb[:, :-1], ((0, 0), (1, 0), (0, 0), (0, 0), (0, 0)))
        return jnp.concatenate([prev, tb], axis=2)

    kw, vw = band(k), band(v)
    s = jnp.einsum('bnqkgd,bnskd->bnkgqs', qb, kw, preferred_element_type=F32) * (HEAD_DIM ** -0.5)
    blk = jnp.arange(nb)[:, None, None]
    qi = blk * BLOCK + jnp.arange(BLOCK)[None, :, None]
    kj = (blk - 1) * BLOCK + jnp.arange(2 * BLOCK)[None, None, :]
    valid = (kj <= qi) & (kj > qi - WINDOW) & (kj >= FRONT_PAD)
    s = jnp.where(valid[None, :, None, None], s, -jnp.inf)
    sink = sinks.astype(F32).reshape(N_KV_HEADS, GROUP)[None, None, :, :, None, None]
    m = jnp.maximum(jnp.max(s, -1, keepdims=True), sink)
    p = jnp.exp(s - m)
    p = p / (jnp.sum(p, -1, keepdims=True) + jnp.exp(sink - m))
    o = jnp.einsum('bnkgqs,bnskd->bnqkgd', p.astype(vw.dtype), vw)
    return o.reshape(bsz, lp, N_Q_HEADS * HEAD_DIM)[:, FRONT_PAD:]


def swiglu(h, w_gate, w_up, w_down):
    return (jax.nn.silu(h @ w_gate) * (h @ w_up)) @ w_down


def moe_swiglu(h, router, e_gate, e_up, e_down):
    bsz, seqlen, d = h.shape
    xf = h.reshape(-1, d)
    logits = jnp.dot(xf, router, preferred_element_type=F32)
    top_v, top_i = lax.top_k(logits, TOP_K)
    top_w = jax.nn.softmax(top_v, axis=-1)
    gates = jnp.sum(jax.nn.one_hot(top_i, N_EXPERTS, dtype=F32) * top_w[..., None], axis=1)
    y = jnp.zeros(xf.shape, F32)
    for e in range(N_EXPERTS):
        y = y + gates[:, e:e + 1] * swiglu(xf, e_gate[e], e_up[e], e_down[e]).astype(F32)
    return y.reshape(bsz, seqlen, d).astype(h.dtype)


def setup_inputs(seed: int = 0) -> dict:
    key = jax.random.key(seed)
    keys = iter(jax.random.split(key, 64))

    def nrm(shape, scale):
        return jax.random.normal(next(keys), shape, F32) * scale

    def gain(shape):
        return 1.0 + nrm(shape, 0.02)

    E, O = N_EVEN, N_ODD
    return {
        "x": nrm((BATCH, SEQ, D_MODEL), 1.0),
        "meta_tokens": nrm((N_META, D_MODEL), 1.0),
        "ev_w_in": nrm((E, D_MODEL, IN0_COLS), D_MODEL ** -0.5),
        "ev_conv_w": nrm((E, CONV_WIDTH, CONV_CH), CONV_WIDTH ** -0.5),
        "ev_conv_b": nrm((E, CONV_CH), 0.02),
        "ev_convnorm_g": gain((E, CONV_CH)),
        "ev_convnorm_b": nrm((E, CONV_CH), 0.02),
        "ev_shift_mu": jax.random.uniform(next(keys), (E, RWKV_COLS), F32),
        "ev_w0": jax.random.uniform(next(keys), (E, RWKV_CH), F32, -6.0, -1.0),
        "ev_w2": nrm((E, DECAY_RANK, RWKV_CH), 0.5 * DECAY_RANK ** -0.5),
        "ev_a0": nrm((E, RWKV_CH), 0.1),
        "ev_a2": nrm((E, AAA_RANK, RWKV_CH), 0.5 * AAA_RANK ** -0.5),
        "ev_g2": nrm((E, GATE_RANK, RWKV_CH), GATE_RANK ** -0.5),
        "ev_k_k": 0.85 + nrm((E, RWKV_CH), 0.02),
        "ev_k_a": gain((E, RWKV_CH)),
        "ev_r_k": nrm((E, RWKV_HEADS, RWKV_HEAD), 0.1),
        "ev_lnx_g": gain((E, RWKV_CH)),
        "ev_lnx_b": nrm((E, RWKV_CH), 0.02),
        "ev_w_out": nrm((E, D_MODEL, D_MODEL), BETA * D_MODEL ** -0.5),
        "ev_ln1_g": gain((E, D_MODEL)),
        "ev_ln1_b": nrm((E, D_MODEL), 0.02),
        "ev_ffn_gate": nrm((E, D_MODEL, D_FF_DENSE), D_MODEL ** -0.5),
        "ev_ffn_up": nrm((E, D_MODEL, D_FF_DENSE), D_MODEL ** -0.5),
        "ev_ffn_down": nrm((E, D_FF_DENSE, D_MODEL), BETA * D_FF_DENSE ** -0.5),
        "ev_ln2_g": gain((E, D_MODEL)),
        "ev_ln2_b": nrm((E, D_MODEL), 0.02),
        "od_w_qkv": nrm((O, D_MODEL, QKV_COLS), D_MODEL ** -0.5),
        "od_b_qkv": nrm((O, QKV_COLS), 0.02),
        "od_sinks": nrm((O, N_Q_HEADS), 1.0),
        "od_w_o": nrm((O, N_Q_HEADS * HEAD_DIM, D_MODEL), BETA * (N_Q_HEADS * HEAD_DIM) ** -0.5),
        "od_b_o": nrm((O, D_MODEL), 0.02),
        "od_ln1_g": gain((O, D_MODEL)),
        "od_ln1_b": nrm((O, D_MODEL), 0.02),
        "od_router": nrm((O, D_MODEL, N_EXPERTS), D_MODEL ** -0.5),
        "od_exp_gate": nrm((O, N_EXPERTS, D_MODEL, D_FF_EXPERT), D_MODEL ** -0.5),
        "od_exp_up": nrm((O, N_EXPERTS, D_MODEL, D_FF_EXPERT), D_MODEL ** -0.5),
        "od_exp_down": nrm((O, N_EXPERTS, D_FF_EXPERT, D_MODEL), BETA * D_FF_EXPERT ** -0.5),
        "od_ln2_g": gain((O, D_MODEL)),
        "od_ln2_b": nrm((O, D_MODEL), 0.02),
    }


def reference(x, meta_tokens, ev_w_in, ev_conv_w, ev_conv_b, ev_convnorm_g, ev_convnorm_b,
              ev_shift_mu, ev_w0, ev_w2, ev_a0, ev_a2, ev_g2, ev_k_k, ev_k_a, ev_r_k,
              ev_lnx_g, ev_lnx_b, ev_w_out, ev_ln1_g, ev_ln1_b, ev_ffn_gate, ev_ffn_up,
              ev_ffn_down, ev_ln2_g, ev_ln2_b, od_w_qkv, od_b_qkv, od_sinks, od_w_o, od_b_o,
              od_ln1_g, od_ln1_b, od_router, od_exp_gate, od_exp_up, od_exp_down,
              od_ln2_g, od_ln2_b):
    bsz = x.shape[0]
    meta = jnp.broadcast_to(meta_tokens[None].astype(x.dtype), (bsz, N_META, D_MODEL))
    h = jnp.concatenate([meta, x], axis=1)
    for layer in range(DEPTH):
        i = layer // 2
        if layer % 2 == 0:
            u = h @ ev_w_in[i]
            a_out = conformer_conv(u[..., :2 * CONV_CH], ev_conv_w[i], ev_conv_b[i],
                                   ev_convnorm_g[i], ev_convnorm_b[i])
            b_out = rwkv7_time_mix(u[..., 2 * CONV_CH:], ev_shift_mu[i], ev_w0[i], ev_w2[i],
                                   ev_a0[i], ev_a2[i], ev_g2[i], ev_k_k[i], ev_k_a[i], ev_r_k[i],
                                   ev_lnx_g[i], ev_lnx_b[i])
            mix = jnp.concatenate([a_out, b_out], axis=-1) @ ev_w_out[i]
            h = layer_norm(ALPHA * h + mix, ev_ln1_g[i], ev_ln1_b[i])
            f = swiglu(h, ev_ffn_gate[i], ev_ffn_up[i], ev_ffn_down[i])
            h = layer_norm(ALPHA * h + f, ev_ln2_g[i], ev_ln2_b[i])
        else:
            att = sliding_window_sink_attention(h, od_w_qkv[i], od_b_qkv[i], od_sinks[i])
            mix = att @ od_w_o[i] + od_b_o[i]
            h = layer_norm(ALPHA * h + mix, od_ln1_g[i], od_ln1_b[i])
            f = moe_swiglu(h, od_router[i], od_exp_gate[i], od_exp_up[i], od_exp_down[i])
            h = layer_norm(ALPHA * h + f, od_ln2_g[i], od_ln2_b[i])
    return h[:, N_META:]
```

```python
import numpy as np
from contextlib import ExitStack
import concourse.bass as bass
import concourse.mybir as mybir
from concourse.bass_utils import run_bass_kernel_spmd

F32 = mybir.dt.float32
F32R = mybir.dt.float32r
I32 = mybir.dt.int32
AF = mybir.ActivationFunctionType
ALU = mybir.AluOpType
AX = mybir.AxisListType

D = 1024
SEQ = 4096
NMETA = 16
PADF = 112
T = SEQ + NMETA + PADF
NT = T // 128
DFF0 = 2816
DFFE = 3584
NEXP = 8
ALPHA = 4.0 ** 0.25
LN_EPS = 1e-5
LNX_EPS = 64e-5
DECAY_SCALE = float(np.exp(-0.5))


class Sched:
    EPOCH = 30000
    NDSEM = 6
    ENG = ('pe', 'act', 'dve', 'pool', 'sp')

    def __init__(self, nc=None, es=None, needs=None):
        self.dry = needs is None
        self.needs = [] if self.dry else needs
        self.nc = nc
        self.es = es
        self.gi = 0
        self.seg = []
        self.lastw = {}
        self.readers = {}
        self.last_on = {}
        if not self.dry:
            self.eobj = dict(pe=nc.tensor, act=nc.scalar, dve=nc.vector, pool=nc.gpsimd, sp=nc.sync)
            self.csem = {}
            self.nsig = {e: 0 for e in self.ENG}
            self.dsem = {q: [es.enter_context(nc.semaphore(f"d_{q}_{i}")) for i in range(self.NDSEM)]
                         for q in ('sp', 'pool')}
            self.ndma = {'sp': 0, 'pool': 0}
            self.dma_ev = {'sp': [], 'pool': []}
            self.seen = {e: {} for e in self.ENG}
            self.last_ev = {}
            self.events = {}

    def op(self, eng, fn, reads=(), writes=()):
        self._do(False, eng, fn, reads, writes)

    def dma(self, q, fn, reads=(), writes=()):
        self._do(True, q, fn, reads, writes)

    def _csem(self, eng, epoch):
        k = (eng, epoch)
        if k not in self.csem:
            self.csem[k] = self.es.enter_context(self.nc.semaphore(f"c_{eng}_{epoch}"))
        return self.csem[k]

    def _wait(self, eng, ev):
        key, sem, val = ev
        if self.seen[eng].get(key, 0) >= val:
            return
        self.eobj[eng].wait_ge(sem, val)
        self.seen[eng][key] = val

    def _do(self, isd, eng, fn, reads, writes):
        i = self.gi
        self.gi += 1
        d = set()
        for k in reads:
            if k in self.lastw:
                d.add(self.lastw[k])
        for k in writes:
            if k in self.lastw:
                d.add(self.lastw[k])
            for r in self.readers.get(k, ()):
                d.add(r)
        deps = []
        for (j, jd, je) in d:
            if (not jd) and je == eng and eng == 'pe':
                continue
            deps.append(j)
            if self.dry and not jd:
                self.needs[j] = True
        me = (i, isd, eng)
        for k in writes:
            self.lastw[k] = me
            self.readers[k] = []
        for k in reads:
            self.readers.setdefault(k, []).append(me)
        if not isd:
            self.last_on[eng] = i
        if self.dry:
            self.needs.append(False)
            return
        if isd:
            k = self.ndma[eng]
            if k >= self.NDSEM:
                self._wait(eng, self.dma_ev[eng][k - self.NDSEM])
        for j in sorted(deps):
            self._wait(eng, self.events[j])
        inst = fn(self.eobj[eng])
        if isd:
            k = self.ndma[eng]
            idx = k % self.NDSEM
            val = 16 * (k // self.NDSEM + 1)
            sem = self.dsem[eng][idx]
            inst.then_inc(sem, 16)
            ev = ((eng, 'd', idx), sem, val)
            self.dma_ev[eng].append(ev)
            self.ndma[eng] = k + 1
            self.events[i] = ev
        elif self.needs[i]:
            c = self.nsig[eng]
            epoch, v = divmod(c, self.EPOCH)
            sem = self._csem(eng, epoch)
            inst.then_inc(sem, 1)
            self.nsig[eng] = c + 1
            ev = ((eng, 'c', epoch), sem, v + 1)
            self.events[i] = ev
            self.last_ev[eng] = ev

    def flush(self):
        if self.dry:
            for e, i in self.last_on.items():
                self.needs[i] = True
        else:
            for e in self.ENG:
                for e2, ev in self.last_ev.items():
                    if e2 != e or e != 'pe':
                        self._wait(e, ev)
                for q in ('sp', 'pool'):
                    for ev in self.dma_ev[q][-self.NDSEM:]:
                        self._wait(e, ev)
            self.events = {}
        self.lastw = {}
        self.readers = {}
        self.last_on = {}


class Ctx:
    pass


DECLARED = set()
DBG = dict(nt=NT, stage=99)


def build_program(upto=99, debug=False):
    nc0 = bass.Bass("TRN2", target_bir_lowering=False)
    dry = Sched()
    with ExitStack() as es0:
        _build(nc0, es0, upto, debug, dry)
    nc = bass.Bass("TRN2", target_bir_lowering=False)
    with ExitStack() as es:
        sc = Sched(nc, es, needs=dry.needs)
        _build(nc, es, upto, debug, sc)
        assert sc.gi == dry.gi
    return nc


def _build(nc, es, upto, debug, sc):
    def din(name, shape):
        if upto < 5 and name.startswith("od_exp"):
            return None
        DECLARED.add(name)
        return nc.dram_tensor(name, list(shape), F32, kind="ExternalInput").ap()

    def dscr(name, shape):
        return nc.dram_tensor(name, list(shape), F32, kind="ExternalOutput" if debug else "Internal").ap()

    def dout(name, shape):
        return nc.dram_tensor(name, list(shape), F32, kind="ExternalOutput").ap()

    I = Ctx()
    I.x = din("x", [SEQ, D])
    I.meta = din("meta_tokens", [NMETA, D])
    I.w_in = din("ev_w_in", [D, 2816])
    I.conv_w = din("ev_conv_w", [31, 512])
    I.conv_b = din("ev_conv_b", [512])
    I.cn_g = din("ev_convnorm_g", [512])
    I.cn_b = din("ev_convnorm_b", [512])
    I.mu = din("ev_shift_mu", [1792])
    I.w0 = din("ev_w0", [512])
    I.w2 = din("ev_w2", [64, 512])
    I.a0 = din("ev_a0", [512])
    I.a2 = din("ev_a2", [64, 512])
    I.g2 = din("ev_g2", [128, 512])
    I.k_k = din("ev_k_k", [512])
    I.k_a = din("ev_k_a", [512])
    I.r_k = din("ev_r_k", [512])
    I.lnx_g = din("ev_lnx_g", [512])
    I.lnx_b = din("ev_lnx_b", [512])
    I.w_out = din("ev_w_out", [D, D])
    I.ln1_g = din("ev_ln1_g", [D])
    I.ln1_b = din("ev_ln1_b", [D])
    I.f_gate = din("ev_ffn_gate", [D, DFF0])
    I.f_up = din("ev_ffn_up", [D, DFF0])
    I.f_down = din("ev_ffn_down", [DFF0, D])
    I.ln2_g = din("ev_ln2_g", [D])
    I.ln2_b = din("ev_ln2_b", [D])
    I.w_qkv = din("od_w_qkv", [D, 1280])
    I.b_qkv = din("od_b_qkv", [1280])
    I.sinks = din("od_sinks", [16])
    I.w_o = din("od_w_o", [D, D])
    I.b_o = din("od_b_o", [D])
    I.oln1_g = din("od_ln1_g", [D])
    I.oln1_b = din("od_ln1_b", [D])
    I.router = din("od_router", [D, NEXP])
    I.e_gate = din("od_exp_gate", [NEXP, D, DFFE])
    I.e_up = din("od_exp_up", [NEXP, D, DFFE])
    I.e_down = din("od_exp_down", [NEXP, DFFE, D])
    I.oln2_g = din("od_ln2_g", [D])
    I.oln2_b = din("od_ln2_b", [D])
    I.rope_cos = din("rope_cos", [T, 32])
    I.rope_sin = din("rope_sin", [T, 32])
    I.out = dout("out", [SEQ, D])

    S = Ctx()
    S.aT = dscr("s_aT", [4, 128, T])
    S.h1 = dscr("s_h1", [T, D])
    S.h1T = dscr("s_h1T", [8, 128, T])
    S.h2 = dscr("s_h2", [T, D])
    S.h2T = dscr("s_h2T", [8, 128, T])
    S.h3 = dscr("s_h3", [T, D])
    S.h3T = dscr("s_h3T", [8, 128, T])
    S.gates = dscr("s_gates", [T, NEXP])
    K = Ctx()
    K.ps = [es.enter_context(nc.psum_tensor(f"ps{b}", [128, 512], F32)) for b in range(8)]
    K.bank = [0]

    def sb(st, name, shape, dt=F32):
        return st.enter_context(nc.sbuf_tensor(name, list(shape), dt))
    K.sb = sb

    def nb():
        b = K.bank[0]
        K.bank[0] = (b + 1) % 8
        return b
    K.nb = nb

    with nc.Block() as block:
        @block.sync
        def _(sync):
            build_consts(nc, es, sc, K, I)
            sc.flush()
            if upto >= 1:
                phase_1a(nc, sc, K, I, S)
            if upto >= 2:
                phase_1b(nc, sc, K, I, S)
            if upto >= 3:
                phase_ffn0(nc, sc, K, I, S)
            if upto >= 4:
                phase_attn(nc, sc, K, I, S)
            if upto >= 5:
                phase_moe(nc, sc, K, I, S)
            if upto < 5:
                phase_fin(nc, sc, K, I, S)


def build_consts(nc, es, sc, K, I):
    sb = K.sb
    K.dI = sb(es, "k_dI", [128, 128], I32)
    K.ident = sb(es, "k_ident", [128, 128], F32)
    K.identr = sb(es, "k_identr", [128, 128], F32R)
    K.onesr = sb(es, "k_onesr", [128, 128], F32R)
    K.blk1r = sb(es, "k_blk1r", [128, 128], F32R)
    K.m_lt = sb(es, "k_mlt", [128, 128], F32)
    K.m_gt = sb(es, "k_mgt", [128, 128], F32)
    K.m_ge = sb(es, "k_mge", [128, 128], F32)
    K.eps_ln = sb(es, "k_epsln", [128, 1], F32)
    K.eps_lnx = sb(es, "k_epslnx", [128, 1], F32)
    K.ones_f = sb(es, "k_onesf", [128, 512], F32)
    K.zero_f = sb(es, "k_zerof", [128, 512], F32)
    sc.op('pool', lambda e: e.iota(K.dI[:], pattern=[[1, 128]], base=0, channel_multiplier=-1), writes=['dI'])
    sc.op('dve', lambda e: e.tensor_single_scalar(out=K.ident[:], in_=K.dI[:], scalar=0, op=ALU.is_equal), reads=['dI'], writes=['ident'])
    sc.op('dve', lambda e: e.tensor_copy(out=K.identr[:], in_=K.ident[:]), reads=['ident'], writes=['identr'])
    sc.op('dve', lambda e: e.tensor_single_scalar(out=K.m_lt[:], in_=K.dI[:], scalar=0, op=ALU.is_lt), reads=['dI'], writes=['m_lt'])
    sc.op('dve', lambda e: e.tensor_single_scalar(out=K.m_gt[:], in_=K.dI[:], scalar=0, op=ALU.is_gt), reads=['dI'], writes=['m_gt'])
    sc.op('dve', lambda e: e.tensor_single_scalar(out=K.m_ge[:], in_=K.dI[:], scalar=0, op=ALU.is_ge), reads=['dI'], writes=['m_ge'])
    sc.op('dve', lambda e: e.memset(K.ones_f[:], 1.0), writes=['ones_f'])
    sc.op('dve', lambda e: e.memset(K.zero_f[:], 0.0), writes=['zero_f'])
    sc.op('dve', lambda e: e.tensor_copy(out=K.onesr[:], in_=K.ones_f[:, 0:128]), reads=['ones_f'], writes=['onesr'])
    sc.op('dve', lambda e: e.tensor_copy(out=K.blk1r[:], in_=K.zero_f[:, 0:128]), reads=['zero_f'], writes=['blk1r'])
    sc.op('dve', lambda e: e.tensor_copy(out=K.blk1r[0:64, 0:64], in_=K.ones_f[0:64, 0:64]), reads=['ones_f', 'blk1r'], writes=['blk1r'])
    sc.op('dve', lambda e: e.tensor_copy(out=K.blk1r[64:128, 64:128], in_=K.ones_f[64:128, 0:64]), reads=['ones_f', 'blk1r'], writes=['blk1r'])
    sc.op('dve', lambda e: e.memset(K.eps_ln[:], LN_EPS), writes=['eps_ln'])
    sc.op('dve', lambda e: e.memset(K.eps_lnx[:], LNX_EPS), writes=['eps_lnx'])


def load_cols(nc, sc, K, st, name, vec_ap, n):
    nch = n // 128
    rows = K.sb(st, name + "_r", [nch, 128], F32)
    cols = K.sb(st, name, [128, nch], F32)
    sc.dma('sp', lambda e: e.dma_start(out=rows[:], in_=vec_ap.rearrange("(c p) -> c p", p=128)), writes=[name + "_r"])
    b = K.nb()
    sc.op('pe', lambda e: e.transpose(K.ps[b][:, 0:nch], rows[:], K.ident[0:nch, 0:nch]), reads=[name + "_r", 'ident'], writes=[('ps', b)])
    sc.op('dve', lambda e: e.tensor_copy(out=cols[:], in_=K.ps[b][:, 0:nch]), reads=[('ps', b)], writes=[name])
    return cols


def load_bcast(nc, sc, K, st, name, vec_ap, n):
    t = K.sb(st, name, [128, n], F32)
    sc.dma('sp', lambda e: e.dma_start(out=t[:], in_=vec_ap.partition_broadcast(128)), writes=[name])
    return t


def load_h_tile(nc, sc, K, I, ht, key, ti):
    if ti == 0:
        sc.op('pool', lambda e: e.memset(ht[:], 0.0), writes=[key])
        sc.dma('sp', lambda e: e.dma_start(out=ht[PADF:128, :], in_=I.meta), writes=[key])
    else:
        sc.dma('sp', lambda e: e.dma_start(out=ht[:], in_=I.x[(ti - 1) * 128: ti * 128, :]), writes=[key])


def transpose_to(nc, sc, K, src, src_key, dst_fn, dst_key, nchunk, evac='act'):
    for c0 in range(0, nchunk, 4):
        n = min(4, nchunk - c0)
        b = K.nb()
        for j in range(n):
            c = c0 + j
            sc.op('pe', lambda e, c=c, j=j, b=b: e.transpose(K.ps[b][:, j * 128:(j + 1) * 128], src[:, c * 128:(c + 1) * 128], K.ident[:]),
                  reads=[src_key, 'ident'], writes=[('ps', b)])
        dst = dst_fn(c0, n)
        if evac == 'act':
            sc.op('act', lambda e, b=b, n=n, dst=dst: e.copy(out=dst, in_=K.ps[b][:, 0:n * 128].rearrange("p (c t) -> p c t", t=128)),
                  reads=[('ps', b)], writes=[dst_key])
        else:
            sc.op('dve', lambda e, b=b, n=n, dst=dst: e.tensor_copy(out=dst, in_=K.ps[b][:, 0:n * 128].rearrange("p (c t) -> p c t", t=128)),
                  reads=[('ps', b)], writes=[dst_key])


def load_weight_r(nc, sc, K, wt, key, w_ap, kchunks, ncols, col0=0, rows0=0):
    step = max(1, 4096 // ncols) if ncols <= 2048 else 1
    for k in range(kchunks):
        for c in range(0, ncols, 2048):
            cw = min(2048, ncols - c)
            sc.dma('pool', lambda e, k=k, c=c, cw=cw: e.dma_start(
                out=wt[:, k, c:c + cw], in_=w_ap[rows0 + k * 128: rows0 + (k + 1) * 128, col0 + c: col0 + c + cw]),
                writes=[key])


def phase_1a(nc, sc, K, I, S):
    with ExitStack() as st:
        sb = K.sb
        wA = sb(st, "a_w", [128, 8, 1024], F32R)
        load_weight_r(nc, sc, K, wA, 'a_w', I.w_in, 8, 1024, col0=0)
        cw = load_cols_multi = None
        convw = sb(st, "a_convw", [128, 4, 31], F32)
        cw_rows = sb(st, "a_convw_r", [31, 512], F32)
        sc.dma('sp', lambda e: e.dma_start(out=cw_rows[:], in_=I.conv_w), writes=['a_convw_r'])
        for c in range(4):
            b = K.nb()
            sc.op('pe', lambda e, c=c, b=b: e.transpose(K.ps[b][:, 0:31], cw_rows[:, c * 128:(c + 1) * 128], K.ident[0:31, 0:31]),
                  reads=['a_convw_r', 'ident'], writes=[('ps', b)])
            sc.op('dve', lambda e, c=c, b=b: e.tensor_copy(out=convw[:, c, :], in_=K.ps[b][:, 0:31]), reads=[('ps', b)], writes=['a_convw'])
        convb = load_cols(nc, sc, K, st, "a_convb", I.conv_b, 512)
        cng = load_cols(nc, sc, K, st, "a_cng", I.cn_g, 512)
        cnb = load_cols(nc, sc, K, st, "a_cnb", I.cn_b, 512)
        SP = 512
        ht = [sb(st, f"a_ht{i}", [128, D], F32) for i in range(2)]
        hT = [sb(st, f"a_hT{i}", [128, 8, SP], F32R) for i in range(2)]
        hg = sb(st, "a_hg", [128, 4, 30 + SP], F32)
        sgt = sb(st, "a_sg", [128, SP], F32)
        acc = [sb(st, f"a_acc{i}", [128, 4, SP], F32) for i in range(2)]
        accr = sb(st, "a_accr", [128, 4, SP], F32R)
        sq = sb(st, "a_sq", [128, 4, SP], F32R)
        mean = sb(st, "a_mean", [128, SP], F32)
        msq = sb(st, "a_msq", [128, SP], F32)
        rstd = sb(st, "a_rstd", [128, SP], F32)
        xc = sb(st, "a_xc", [128, SP], F32)
        yo = [sb(st, f"a_yo{i}", [128, 4, SP], F32) for i in range(2)]
        sc.op('dve', lambda e: e.memset(hg[:], 0.0), writes=['a_hg'])
        spans = [(s, min(4, NT - s)) for s in range(0, NT, 4)]
        nld = 0
        for si, (t0, ntl) in enumerate(spans):
            n = ntl * 128
            hTc = hT[si % 2]
            hk = f"a_hT{si % 2}"
            for j in range(ntl):
                hb = ht[nld % 2]
                hbk = f"a_ht{nld % 2}"
                nld += 1
                load_h_tile(nc, sc, K, I, hb, hbk, t0 + j)
                transpose_to(nc, sc, K, hb, hbk, lambda c0, nn, j=j, hTc=hTc: hTc[:, c0:c0 + nn, j * 128:(j + 1) * 128], hk, 8,
                             evac='act' if j % 2 == 0 else 'dve')
            if si > 0:
                sc.op('dve', lambda e: e.tensor_copy(out=hg[:, :, 0:30], in_=hg[:, :, SP:SP + 30]), reads=['a_hg'], writes=['a_hg'])
            for c in range(4):
                bv = K.nb()
                bg = K.nb()
                for k in range(8):
                    sc.op('pe', lambda e, k=k, c=c, bv=bv: e.matmul(K.ps[bv][:, 0:n], wA[:, k, c * 128:(c + 1) * 128], hTc[:, k, 0:n], start=(k == 0), stop=(k == 7)),
                          reads=['a_w', hk], writes=[('ps', bv)])
                for k in range(8):
                    sc.op('pe', lambda e, k=k, c=c, bg=bg: e.matmul(K.ps[bg][:, 0:n], wA[:, k, 512 + c * 128:512 + (c + 1) * 128], hTc[:, k, 0:n], start=(k == 0), stop=(k == 7)),
                          reads=['a_w', hk], writes=[('ps', bg)])
                sc.op('act', lambda e, bg=bg: e.activation(out=sgt[:, 0:n], in_=K.ps[bg][:, 0:n], func=AF.Sigmoid), reads=[('ps', bg)], writes=['a_sg'])
                sc.op('dve', lambda e, bv=bv, c=c: e.tensor_tensor(out=hg[:, c, 30:30 + n], in0=K.ps[bv][:, 0:n], in1=sgt[:, 0:n], op=ALU.mult),
                      reads=[('ps', bv), 'a_sg'], writes=['a_hg'])
            ac = acc[si % 2]
            ak = f"a_acc{si % 2}"
            acf = ac
            for c in range(4):
                sc.op('dve', lambda e, c=c: e.tensor_scalar(out=acf[:, c, 0:n], in0=hg[:, c, 0:n], scalar1=convw[:, c, 0:1], scalar2=convb[:, c:c + 1], op0=ALU.mult, op1=ALU.add),
                      reads=['a_hg', 'a_convw', 'a_convb'], writes=[ak])
                for k in range(1, 31):
                    o = acf
                    sc.op('dve', lambda e, c=c, k=k, o=o: e.scalar_tensor_tensor(out=o[:, c, 0:n], in0=hg[:, c, k:k + n], scalar=convw[:, c, k:k + 1], in1=acf[:, c, 0:n], op0=ALU.mult, op1=ALU.add),
                          reads=['a_hg', 'a_convw', ak], writes=[ak])
            sc.op('act', lambda e: e.activation(out=sq[:, :, 0:n], in_=acf[:, :, 0:n], func=AF.Square), reads=[ak], writes=['a_sq'])
            sc.op('act', lambda e: e.copy(out=accr[:, :, 0:n], in_=acf[:, :, 0:n]), reads=[ak], writes=['a_accr'])
            b1 = K.nb()
            b2 = K.nb()
            for c in range(4):
                sc.op('pe', lambda e, c=c: e.matmul(K.ps[b1][:, 0:n], K.onesr[:], accr[:, c, 0:n], start=(c == 0), stop=(c == 3)), reads=['onesr', 'a_accr'], writes=[('ps', b1)])
            for c in range(4):
                sc.op('pe', lambda e, c=c: e.matmul(K.ps[b2][:, 0:n], K.onesr[:], sq[:, c, 0:n], start=(c == 0), stop=(c == 3)), reads=['onesr', 'a_sq'], writes=[('ps', b2)])
            sc.op('act', lambda e: e.mul(out=mean[:, 0:n], in_=K.ps[b1][:, 0:n], mul=1.0 / 512), reads=[('ps', b1)], writes=['a_mean'])
            sc.op('dve', lambda e: e.tensor_tensor(out=msq[:, 0:n], in0=mean[:, 0:n], in1=mean[:, 0:n], op=ALU.mult), reads=['a_mean'], writes=['a_msq'])
            sc.op('dve', lambda e: e.scalar_tensor_tensor(out=rstd[:, 0:n], in0=K.ps[b2][:, 0:n], scalar=1.0 / 512, in1=msq[:, 0:n], op0=ALU.mult, op1=ALU.subtract),
                  reads=[('ps', b2), 'a_msq'], writes=['a_rstd'])
            sc.op('act', lambda e: e.activation(out=rstd[:, 0:n], in_=rstd[:, 0:n], func=AF.Sqrt, bias=K.eps_ln[:, 0:1], scale=1.0), reads=['a_rstd', 'eps_ln'], writes=['a_rstd'])
            sc.op('dve', lambda e: e.reciprocal(out=rstd[:, 0:n], in_=rstd[:, 0:n]), reads=['a_rstd'], writes=['a_rstd'])
            y = yo[si % 2]
            yk = f"a_yo{si % 2}"
            for c in range(4):
                sc.op('dve', lambda e, c=c: e.tensor_tensor(out=xc[:, 0:n], in0=acf[:, c, 0:n], in1=mean[:, 0:n], op=ALU.subtract), reads=[ak, 'a_mean'], writes=['a_xc'])
                sc.op('dve', lambda e, c=c: e.tensor_tensor(out=xc[:, 0:n], in0=xc[:, 0:n], in1=rstd[:, 0:n], op=ALU.mult), reads=['a_xc', 'a_rstd'], writes=['a_xc'])
                sc.op('act', lambda e, c=c: e.activation(out=y[:, c, 0:n], in_=xc[:, 0:n], func=AF.Silu, bias=cnb[:, c:c + 1], scale=cng[:, c:c + 1]),
                      reads=['a_xc', 'a_cnb', 'a_cng'], writes=[yk])
            for c in range(4):
                sc.dma('sp', lambda e, c=c: e.dma_start(out=S.aT[c, :, t0 * 128: t0 * 128 + n], in_=y[:, c, 0:n]), reads=[yk], writes=['S.aT'])
        sc.flush()


def phase_fin(nc, sc, K, I, S):
    with ExitStack() as st:
        buf = K.sb(st, "fin_buf", [128, D], F32)
        sc.op('dve', lambda e: e.memset(buf[:], 0.0), writes=['fin_buf'])
        sc.dma('sp', lambda e: e.dma_start(out=I.out[0:128, :], in_=buf[:]), reads=['fin_buf'], writes=['out'])
        sc.flush()


def layernorm_tm(sc, K, x, xk, g_bc, gk, b_bc, bk, out, ok, tmp):
    st6, mv, sd = tmp['st6'], tmp['mv'], tmp['sd']
    for hfi in range(2):
        sc.op('dve', lambda e, hfi=hfi: e.bn_stats(out=st6[:, hfi * 6:(hfi + 1) * 6], in_=x[:, hfi * 512:(hfi + 1) * 512]), reads=[xk], writes=['ln_st6'])
    sc.op('dve', lambda e: e.bn_aggr(out=mv[:, 0:2], in_=st6[:, 0:12]), reads=['ln_st6'], writes=['ln_mv'])
    sc.op('act', lambda e: e.activation(out=sd[:, 0:1], in_=mv[:, 1:2], func=AF.Sqrt, bias=K.eps_ln[:, 0:1], scale=1.0), reads=['ln_mv', 'eps_ln'], writes=['ln_sd'])
    sc.op('dve', lambda e: e.reciprocal(out=sd[:, 0:1], in_=sd[:, 0:1]), reads=['ln_sd'], writes=['ln_sd'])
    sc.op('dve', lambda e: e.tensor_scalar(out=out[:], in0=x[:], scalar1=mv[:, 0:1], scalar2=sd[:, 0:1], op0=ALU.subtract, op1=ALU.mult),
          reads=[xk, 'ln_mv', 'ln_sd'], writes=[ok])
    sc.op('dve', lambda e: e.tensor_tensor(out=out[:], in0=out[:], in1=g_bc[:], op=ALU.mult), reads=[ok, gk], writes=[ok])
    sc.op('dve', lambda e: e.tensor_tensor(out=out[:], in0=out[:], in1=b_bc[:], op=ALU.add), reads=[ok, bk], writes=[ok])


def ln_tmp(K, st, pfx):
    return dict(st6=K.sb(st, pfx + "_st6", [128, 12], F32), mv=K.sb(st, pfx + "_mv", [128, 2], F32), sd=K.sb(st, pfx + "_sd", [128, 1], F32))


def store_h_and_hT(nc, sc, K, h, hk, hT, hTk, dram_h, dram_hT, ti):
    sc.dma('sp', lambda e: e.dma_start(out=dram_h[ti * 128:(ti + 1) * 128, :], in_=h[:]), reads=[hk], writes=['dram_h'])
    transpose_to(nc, sc, K, h, hk, lambda c0, nn: hT[:, c0:c0 + nn, :], hTk, 8, evac='act')
    for c0 in (0, 4):
        sc.dma('sp', lambda e, c0=c0: e.dma_start(out=dram_hT[c0:c0 + 4, :, ti * 128:(ti + 1) * 128].rearrange("c p t -> p c t"), in_=(hT[:, c0:c0 + 4, :].bitcast(F32) if hT.dtype == F32R else hT[:, c0:c0 + 4, :])),
               reads=[hTk], writes=['dram_hT'])


def phase_1b(nc, sc, K, I, S):
    with ExitStack() as st:
        sb = K.sb
        V3 = [128, 4, 128]
        wB = sb(st, "b_wB", [128, 8, 1792], F32R)
        load_weight_r(nc, sc, K, wB, 'b_wB', I.w_in, 8, 1792, col0=1024)
        wO = sb(st, "b_wO", [128, 8, 1024], F32R)
        load_weight_r(nc, sc, K, wO, 'b_wO', I.w_out, 8, 1024)
        wa2 = sb(st, "b_wa2", [128, 512], F32R)
        sc.dma('pool', lambda e: e.dma_start(out=wa2[0:64, :], in_=I.w2), writes=['b_wa2'])
        sc.dma('pool', lambda e: e.dma_start(out=wa2[64:128, :], in_=I.a2), writes=['b_wa2'])
        g2r = sb(st, "b_g2r", [128, 512], F32R)
        sc.dma('pool', lambda e: e.dma_start(out=g2r[:], in_=I.g2), writes=['b_g2r'])
        mu = load_cols(nc, sc, K, st, "b_mu", I.mu, 1792)
        w0c = load_cols(nc, sc, K, st, "b_w0c", I.w0, 512)
        a0c = load_cols(nc, sc, K, st, "b_a0c", I.a0, 512)
        kkc = load_cols(nc, sc, K, st, "b_kkc", I.k_k, 512)
        kac = load_cols(nc, sc, K, st, "b_kac", I.k_a, 512)
        rkc = load_cols(nc, sc, K, st, "b_rkc", I.r_k, 512)
        omka = sb(st, "b_omka", [128, 4], F32)
        sc.op('dve', lambda e: e.tensor_scalar(out=omka[:], in0=kac[:], scalar1=-1.0, scalar2=1.0, op0=ALU.mult, op1=ALU.add), reads=['b_kac'], writes=['b_omka'])
        lnxg = load_bcast(nc, sc, K, st, "b_lnxg", I.lnx_g, 512)
        lnxb = load_bcast(nc, sc, K, st, "b_lnxb", I.lnx_b, 512)
        ln1g = load_bcast(nc, sc, K, st, "b_ln1g", I.ln1_g, D)
        ln1b = load_bcast(nc, sc, K, st, "b_ln1b", I.ln1_b, D)
        indf = sb(st, "b_indf", [128, 4, 8], F32)
        ind = sb(st, "b_ind", [128, 4, 8], F32R)
        sc.op('dve', lambda e: e.memset(indf[:], 0.0), writes=['b_indf'])
        for c in range(4):
            sc.op('dve', lambda e, c=c: e.memset(indf[0:64, c, 2 * c:2 * c + 1], 1.0), reads=['b_indf'], writes=['b_indf'])
            sc.op('dve', lambda e, c=c: e.memset(indf[64:128, c, 2 * c + 1:2 * c + 2], 1.0), reads=['b_indf'], writes=['b_indf'])
        sc.op('dve', lambda e: e.tensor_copy(out=ind[:], in_=indf[:]), reads=['b_indf'], writes=['b_ind'])
        ST = [sb(st, f"b_ST{i}", V3, F32R) for i in range(2)]
        sc.op('dve', lambda e: e.tensor_copy(out=ST[0][:], in_=K.zero_f[:, 0:512].rearrange("p (c t) -> p c t", t=128)), reads=['zero_f'], writes=['b_ST0'])
        zT = sb(st, "b_zT", [128, 14, 129], F32)
        sc.op('dve', lambda e: e.memset(zT[:], 0.0), writes=['b_zT'])
        hb = [sb(st, "b_h0", [128, D], F32)]
        hT = sb(st, "b_hT", [128, 8, 128], F32R)
        zl = sb(st, "b_zl", [128, 14, 128], F32)
        lo = sb(st, "b_lo", [128, 128], F32R)
        sgl = sb(st, "b_sgl", [128, 128], F32R)
        g_tm = sb(st, "b_gtm", [128, 512], F32)
        sgw = sb(st, "b_sgw", V3, F32)
        a_t = sb(st, "b_a", V3, F32)
        Lc = sb(st, "b_Lc", V3, F32)
        Wt = sb(st, "b_Wt", V3, F32)
        Winv = sb(st, "b_Winv", V3, F32)
        Wp = sb(st, "b_Wp", V3, F32)
        kk = sb(st, "b_kk", V3, F32)
        sqk = sb(st, "b_sqk", V3, F32R)
        nrm = Lc
        t1 = sb(st, "b_t1", V3, F32)
        kmod = sgw
        kt = sb(st, "b_kt", V3, F32R)
        ab = sb(st, "b_ab", V3, F32R)
        kb = sb(st, "b_kb", V3, F32R)
        rt = sb(st, "b_rt", V3, F32R)
        rkr = sb(st, "b_rkr", V3, F32R)
        bs = sb(st, "b_bs", [128, 8], F32)
        Vt = sb(st, "b_V", [128, 512], F32R)
        ab_tm = sb(st, "b_abtm", [128, 512], F32R)
        kb_tm = sb(st, "b_kbtm", [128, 512], F32R)
        H3 = [128, 8, 128]
        Pm = [sb(st, f"b_P{i}", H3, F32R) for i in range(2)]
        Qm = [sb(st, f"b_Q{i}", H3, F32R) for i in range(2)]
        Xs = sb(st, "b_X0", H3, F32R)
        Xm = [Xs, Xs]
        BT, ArT, BrT = Pm[0], Pm[1], Qm[0]
        Zs = Qm[1][:, 0:4, :].rearrange("p c t -> p (c t)")
        U = Qm[1][:, 4:8, :].rearrange("p c t -> p (c t)")
        ysb = sb(st, "b_ysb", [128, 512], F32)
        s1 = sb(st, "b_s1", [128, 8], F32)
        s2 = sb(st, "b_s2", [128, 8], F32)
        mixT = hT
        res = zl[:, 0:8, :].rearrange("p c t -> p (c t)")
        lt = ln_tmp(K, st, "b_ln")

        def bc4(ap):
            return ap.unsqueeze(2).broadcast_to(V3)

        def mask4(m):
            return m[:].unsqueeze(1).broadcast_to(V3)

        def v4(ap):
            return ap.rearrange("p (c t) -> p c t", t=128)

        def v8(ap):
            return ap.rearrange("p (h i) -> p h i", i=64)

        for ti in range(DBG["nt"]):
            h = hb[0]
            hk = "b_h0"
            load_h_tile(nc, sc, K, I, h, hk, ti)
            transpose_to(nc, sc, K, h, hk, lambda c0, nn: hT[:, c0:c0 + nn, :], 'b_hT', 8, evac='act')
            for c0 in range(0, 14, 4):
                nn = min(4, 14 - c0)
                b = K.nb()
                for j in range(nn):
                    for k in range(8):
                        sc.op('pe', lambda e, j=j, k=k: e.matmul(K.ps[b][:, j * 128:(j + 1) * 128], wB[:, k, (c0 + j) * 128:(c0 + j + 1) * 128], hT[:, k, :], start=(k == 0), stop=(k == 7)),
                              reads=['b_wB', 'b_hT'], writes=[('ps', b)])
                sc.op('act', lambda e: e.copy(out=zT[:, c0:c0 + nn, 1:129], in_=K.ps[b][:, 0:nn * 128].rearrange("p (c t) -> p c t", t=128)), reads=[('ps', b)], writes=['b_zT'])
            sc.op('dve', lambda e: e.tensor_tensor(out=zl[:], in0=zT[:, :, 0:128], in1=zT[:, :, 1:129], op=ALU.subtract), reads=['b_zT'], writes=['b_zl'])
            sc.op('dve', lambda e: e.tensor_tensor(out=zl[:], in0=zl[:], in1=mu[:].unsqueeze(2).broadcast_to([128, 14, 128]), op=ALU.mult), reads=['b_zl', 'b_mu'], writes=['b_zl'])
            sc.op('dve', lambda e: e.tensor_tensor(out=zl[:], in0=zl[:], in1=zT[:, :, 1:129], op=ALU.add), reads=['b_zl', 'b_zT'], writes=['b_zl'])
            sc.op('act', lambda e: e.copy(out=zT[:, :, 0:1], in_=zT[:, :, 128:129]), reads=['b_zT'], writes=['b_zT'])
            if DBG["stage"] <= 1:
                continue
            r_ = zl[:, 0:4, :]
            k_ = zl[:, 4:8, :]
            sc.op('act', lambda e: e.activation(out=lo[0:64, :], in_=zl[0:64, 12, :], func=AF.Tanh), reads=['b_zl'], writes=['b_lo'])
            sc.op('act', lambda e: e.copy(out=lo[64:128, :], in_=zl[64:128, 12, :]), reads=['b_zl'], writes=['b_lo'])
            sc.op('act', lambda e: e.activation(out=sgl[:], in_=zl[:, 13, :], func=AF.Sigmoid), reads=['b_zl'], writes=['b_sgl'])
            bw = K.nb()
            for c in range(4):
                sc.op('pe', lambda e, c=c: e.matmul(K.ps[bw][:, c * 128:(c + 1) * 128], wa2[0:64, c * 128:(c + 1) * 128], lo[0:64, :], start=True, stop=True), reads=['b_wa2', 'b_lo'], writes=[('ps', bw)])
            for c in range(4):
                sc.op('act', lambda e, c=c: e.activation(out=sgw[:, c, :], in_=K.ps[bw][:, c * 128:(c + 1) * 128], func=AF.Sigmoid, bias=w0c[:, c:c + 1], scale=1.0), reads=[('ps', bw), 'b_w0c'], writes=['b_sgw'])
            ba = K.nb()
            for c in range(4):
                sc.op('pe', lambda e, c=c: e.matmul(K.ps[ba][:, c * 128:(c + 1) * 128], wa2[64:128, c * 128:(c + 1) * 128], lo[64:128, :], start=True, stop=True), reads=['b_wa2', 'b_lo'], writes=[('ps', ba)])
            for c in range(4):
                sc.op('act', lambda e, c=c: e.activation(out=a_t[:, c, :], in_=K.ps[ba][:, c * 128:(c + 1) * 128], func=AF.Sigmoid, bias=a0c[:, c:c + 1], scale=1.0), reads=[('ps', ba), 'b_a0c'], writes=['b_a'])
            bg = K.nb()
            sc.op('pe', lambda e: e.matmul(K.ps[bg][:, 0:512], sgl[:], g2r[:], start=True, stop=True), reads=['b_sgl', 'b_g2r'], writes=[('ps', bg)])
            sc.op('act', lambda e: e.copy(out=g_tm[:], in_=K.ps[bg][:, 0:512]), reads=[('ps', bg)], writes=['b_gtm'])
            for c in range(4):
                sc.op('dve', lambda e, c=c: e.tensor_tensor_scan(out=Lc[:, c, :], data0=K.ones_f[:, 0:128], data1=sgw[:, c, :], initial=0.0, op0=ALU.mult, op1=ALU.add), reads=['b_sgw', 'ones_f'], writes=['b_Lc'])
            sc.op('dve', lambda e: e.tensor_tensor(out=t1[:], in0=Lc[:], in1=sgw[:], op=ALU.subtract), reads=['b_Lc', 'b_sgw'], writes=['b_t1'])
            sc.op('act', lambda e: e.activation(out=Wt[:], in_=Lc[:], func=AF.Exp, scale=-DECAY_SCALE), reads=['b_Lc'], writes=['b_Wt'])
            sc.op('act', lambda e: e.activation(out=Winv[:], in_=Lc[:], func=AF.Exp, scale=DECAY_SCALE), reads=['b_Lc'], writes=['b_Winv'])
            sc.op('act', lambda e: e.activation(out=Wp[:], in_=t1[:], func=AF.Exp, scale=-DECAY_SCALE), reads=['b_t1'], writes=['b_Wp'])
            sc.op('dve', lambda e: e.tensor_tensor(out=kk[:], in0=k_, in1=bc4(kkc[:]), op=ALU.mult), reads=['b_zl', 'b_kkc'], writes=['b_kk'])
            sc.op('act', lambda e: e.activation(out=sqk[:], in_=kk[:], func=AF.Square), reads=['b_kk'], writes=['b_sqk'])
            bsq = K.nb()
            for c in range(4):
                sc.op('pe', lambda e, c=c: e.matmul(K.ps[bsq][:, c * 128:(c + 1) * 128], K.blk1r[:], sqk[:, c, :], start=True, stop=True), reads=['blk1r', 'b_sqk'], writes=[('ps', bsq)])
            sc.op('act', lambda e: e.activation(out=nrm[:], in_=v4(K.ps[bsq][:, 0:512]), func=AF.Sqrt), reads=[('ps', bsq)], writes=['b_Lc'])
            sc.op('dve', lambda e: e.tensor_scalar_max(out=nrm[:], in0=nrm[:], scalar1=1e-12), reads=['b_Lc'], writes=['b_Lc'])
            sc.op('dve', lambda e: e.reciprocal(out=nrm[:], in_=nrm[:]), reads=['b_Lc'], writes=['b_Lc'])
            sc.op('dve', lambda e: e.tensor_tensor(out=kk[:], in0=kk[:], in1=nrm[:], op=ALU.mult), reads=['b_kk', 'b_Lc'], writes=['b_kk'])
            sc.op('dve', lambda e: e.tensor_tensor(out=t1[:], in0=a_t[:], in1=bc4(kac[:]), op=ALU.mult), reads=['b_a', 'b_kac'], writes=['b_t1'])
            sc.op('dve', lambda e: e.tensor_tensor(out=t1[:], in0=t1[:], in1=bc4(omka[:]), op=ALU.add), reads=['b_t1', 'b_omka'], writes=['b_t1'])
            sc.op('dve', lambda e: e.tensor_tensor(out=kmod[:], in0=k_, in1=t1[:], op=ALU.mult), reads=['b_zl', 'b_t1'], writes=['b_sgw'])
            sc.op('dve', lambda e: e.tensor_tensor(out=kt[:], in0=kk[:], in1=Wp[:], op=ALU.mult), reads=['b_kk', 'b_Wp'], writes=['b_kt'])
            sc.op('dve', lambda e: e.tensor_tensor(out=t1[:], in0=kk[:], in1=a_t[:], op=ALU.mult), reads=['b_kk', 'b_a', 'b_t1'], writes=['b_t1'])
            sc.op('dve', lambda e: e.tensor_tensor(out=ab[:], in0=t1[:], in1=Winv[:], op=ALU.mult), reads=['b_t1', 'b_Winv'], writes=['b_ab'])
            sc.op('dve', lambda e: e.tensor_tensor(out=kb[:], in0=kmod[:], in1=Winv[:], op=ALU.mult), reads=['b_sgw', 'b_Winv'], writes=['b_kb'])
            sc.op('dve', lambda e: e.tensor_tensor(out=rt[:], in0=r_, in1=Wt[:], op=ALU.mult), reads=['b_zl', 'b_Wt'], writes=['b_rt'])
            sc.op('dve', lambda e: e.tensor_tensor(out=t1[:], in0=r_, in1=kmod[:], op=ALU.mult), reads=['b_zl', 'b_sgw', 'b_t1'], writes=['b_t1'])
            sc.op('dve', lambda e: e.tensor_tensor(out=rkr[:], in0=t1[:], in1=bc4(rkc[:]), op=ALU.mult), reads=['b_t1', 'b_rkc'], writes=['b_rkr'])
            bb = K.nb()
            for c in range(4):
                sc.op('pe', lambda e, c=c: e.matmul(K.ps[bb][:, 0:8], rkr[:, c, :], ind[:, c, :], start=(c == 0), stop=(c == 3)), reads=['b_rkr', 'b_ind'], writes=[('ps', bb)])
            sc.op('act', lambda e: e.copy(out=bs[:], in_=K.ps[bb][:, 0:8]), reads=[('ps', bb)], writes=['b_bs'])
            if DBG["stage"] <= 2:
                continue
            for (src_fn, dst, dk, rk_) in ((lambda c: zl[:, 8 + c, :], Vt, 'b_V', 'b_zl'),
                                          (lambda c: ab[:, c, :], ab_tm, 'b_abtm', 'b_ab'),
                                          (lambda c: kb[:, c, :], kb_tm, 'b_kbtm', 'b_kb'))[DBG.get('sub0', 0):DBG.get('sub1', 3)]:
                b = K.nb()
                for c in range(4):
                    if rk_ == 'b_zl':
                        sc.op('pe', lambda e, c=c: e.transpose(K.ps[b][:, c * 128:(c + 1) * 128], src_fn(c), K.ident[:]), reads=[rk_, 'ident'], writes=[('ps', b)])
                    else:
                        sc.op('pe', lambda e, c=c: e.transpose(K.ps[b][:, c * 128:(c + 1) * 128].bitcast(F32R), src_fn(c), K.identr[:]), reads=[rk_, 'identr'], writes=[('ps', b)])
                sc.op('act', lambda e: e.copy(out=dst[:], in_=K.ps[b][:, 0:512]), reads=[('ps', b)], writes=[dk])
            if DBG["stage"] <= 3:
                continue
            def headmm(L, Lk, Rr, Rk, mask, dst, dk):
                for par in range(2):
                    b = K.nb()
                    p0 = 64 * par
                    for c in range(4):
                        sc.op('pe', lambda e, c=c: e.matmul(K.ps[b][:, c * 128:(c + 1) * 128], L[p0:p0 + 64, c, :], Rr[p0:p0 + 64, c, :], start=True, stop=True),
                              reads=[Lk, Rk], writes=[('ps', b)])
                    sc.op('dve', lambda e: e.tensor_tensor(out=dst[:, par::2, :], in0=v4(K.ps[b][:, 0:512]), in1=mask4(mask), op=ALU.mult),
                          reads=[('ps', b)], writes=[dk])
            headmm(kt, 'b_kt', ab, 'b_ab', K.m_lt, Pm[0], 'b_P0')
            headmm(ab, 'b_ab', kt, 'b_kt', K.m_gt, Qm[0], 'b_Q0')
            if DBG["stage"] <= 4:
                continue
            sc.op('dve', lambda e: e.tensor_tensor(out=Xm[0][:], in0=K.ident[:].unsqueeze(1).broadcast_to(H3), in1=Qm[0][:].bitcast(F32), op=ALU.subtract), reads=['ident', 'b_Q0'], writes=['b_X0'])
            cur = 0
            for lev in range(1, 7):
                nxt = 1 - cur
                Pc, Qc, Xc = Pm[cur], Qm[cur], Xm[cur]
                Pn, Qn, Xn = Pm[nxt], Qm[nxt], Xm[nxt]
                for hgp in range(2):
                    b = K.nb()
                    for hh in range(4):
                        hd = hgp * 4 + hh
                        sc.op('pe', lambda e, hh=hh, hd=hd: e.matmul(K.ps[b][:, hh * 128:(hh + 1) * 128], Qc[:, hd, :], Pc[:, hd, :], start=True, stop=True),
                              reads=[f'b_Q{cur}', f'b_P{cur}'], writes=[('ps', b)])
                    sc.op('act', lambda e: e.copy(out=Pn[:, hgp * 4:(hgp + 1) * 4, :], in_=v4(K.ps[b][:, 0:512])), reads=[('ps', b)], writes=[f'b_P{nxt}'])
                if lev < 6:
                    for hgp in range(2):
                        b = K.nb()
                        for hh in range(4):
                            hd = hgp * 4 + hh
                            sc.op('pe', lambda e, hh=hh, hd=hd: e.matmul(K.ps[b][:, hh * 128:(hh + 1) * 128], Pc[:, hd, :], Qc[:, hd, :], start=True, stop=True),
                                  reads=[f'b_Q{cur}', f'b_P{cur}'], writes=[('ps', b)])
                        sc.op('act', lambda e: e.copy(out=Qn[:, hgp * 4:(hgp + 1) * 4, :], in_=v4(K.ps[b][:, 0:512])), reads=[('ps', b)], writes=[f'b_Q{nxt}'])
                for hgp in range(2):
                    b = K.nb()
                    for hh in range(4):
                        hd = hgp * 4 + hh
                        sc.op('pe', lambda e, hh=hh, hd=hd: e.matmul(K.ps[b][:, hh * 128:(hh + 1) * 128], Pn[:, hd, :], Xc[:, hd, :], start=True, stop=True),
                              reads=[f'b_P{nxt}', 'b_X0'], writes=[('ps', b)])
                    sc.op('dve', lambda e: e.tensor_tensor(out=Xn[:, hgp * 4:(hgp + 1) * 4, :], in0=v4(K.ps[b][:, 0:512]), in1=Xc[:, hgp * 4:(hgp + 1) * 4, :].bitcast(F32), op=ALU.add),
                          reads=[('ps', b), 'b_X0'], writes=['b_X0'])
                cur = nxt
            if DBG["stage"] <= 5:
                continue
            X = Xm[cur]
            headmm(kb, 'b_kb', kt, 'b_kt', K.m_gt, BT, 'b_P0')
            headmm(ab, 'b_ab', rt, 'b_rt', K.m_ge, ArT, 'b_P1')
            headmm(kb, 'b_kb', rt, 'b_rt', K.m_ge, BrT, 'b_Q0')
            Xk = 'b_X0'
            S0 = ST[ti % 2]
            S0k = f'b_ST{ti % 2}'
            S1 = ST[1 - ti % 2]
            S1k = f'b_ST{1 - ti % 2}'
            def q4(ap, par):
                return ap.rearrange("p (c q i) -> p c q i", q=2, i=64)[:, :, par, :]
            for par in range(2):
                bz = K.nb()
                p0 = 64 * par
                for c in range(4):
                    hd = 2 * c + par
                    sc.op('pe', lambda e, hd=hd, c=c: e.matmul(K.ps[bz][:, c * 64:(c + 1) * 64], kt[p0:p0 + 64, c, :], S0[p0:p0 + 64, c, p0:p0 + 64], start=True, stop=False),
                          reads=['b_kt', S0k], writes=[('ps', bz)])
                    sc.op('pe', lambda e, hd=hd, c=c: e.matmul(K.ps[bz][:, c * 64:(c + 1) * 64], BT[:, hd, :], Vt[:, hd * 64:(hd + 1) * 64], start=False, stop=True),
                          reads=['b_P0', 'b_V'], writes=[('ps', bz)])
                sc.op('act', lambda e: e.mul(out=q4(Zs, par), in_=K.ps[bz][:, 0:256].rearrange("p (c i) -> p c i", i=64), mul=-1.0), reads=[('ps', bz)], writes=['b_Q1'])
            bu = K.nb()
            for hd in range(8):
                sc.op('pe', lambda e, hd=hd: e.matmul(K.ps[bu][:, hd * 64:(hd + 1) * 64], X[:, hd, :], Zs[:, hd * 64:(hd + 1) * 64], start=True, stop=True),
                      reads=[Xk, 'b_Q1'], writes=[('ps', bu)])
            sc.op('act', lambda e: e.copy(out=U, in_=K.ps[bu][:, 0:512]), reads=[('ps', bu)], writes=['b_Q1'])
            for par in range(2):
                by = K.nb()
                p0 = 64 * par
                for c in range(4):
                    hd = 2 * c + par
                    sc.op('pe', lambda e, hd=hd, c=c: e.matmul(K.ps[by][:, c * 64:(c + 1) * 64], rt[p0:p0 + 64, c, :], S0[p0:p0 + 64, c, p0:p0 + 64], start=True, stop=False),
                          reads=['b_rt', S0k], writes=[('ps', by)])
                    sc.op('pe', lambda e, hd=hd, c=c: e.matmul(K.ps[by][:, c * 64:(c + 1) * 64], ArT[:, hd, :], U[:, hd * 64:(hd + 1) * 64], start=False, stop=False),
                          reads=['b_P1', 'b_Q1'], writes=[('ps', by)])
                    sc.op('pe', lambda e, hd=hd, c=c: e.matmul(K.ps[by][:, c * 64:(c + 1) * 64], BrT[:, hd, :], Vt[:, hd * 64:(hd + 1) * 64], start=False, stop=True),
                          reads=['b_Q0', 'b_V'], writes=[('ps', by)])
                sc.op('act', lambda e: e.copy(out=q4(ysb[:], par), in_=K.ps[by][:, 0:256].rearrange("p (c i) -> p c i", i=64)), reads=[('ps', by)], writes=['b_ysb'])
            bs2 = K.nb()
            for c in range(4):
                cs = slice(c * 128, (c + 1) * 128)
                sc.op('pe', lambda e, cs=cs: e.matmul(K.ps[bs2][:, cs], ab_tm[:, cs], U[:, cs], start=True, stop=False), reads=['b_abtm', 'b_Q1'], writes=[('ps', bs2)])
                sc.op('pe', lambda e, cs=cs: e.matmul(K.ps[bs2][:, cs], kb_tm[:, cs], Vt[:, cs], start=False, stop=False), reads=['b_kbtm', 'b_V'], writes=[('ps', bs2)])
                sc.op('pe', lambda e, cs=cs, c=c: e.matmul(K.ps[bs2][:, cs], K.identr[:], S0[:, c, :], start=False, stop=True), reads=['identr', S0k], writes=[('ps', bs2)])
            sc.op('dve', lambda e: e.tensor_tensor(out=S1[:], in0=v4(K.ps[bs2][:, 0:512]), in1=Wt[:, :, 127:128].broadcast_to(V3), op=ALU.mult), reads=[('ps', bs2), 'b_Wt'], writes=[S1k])
            if DBG["stage"] <= 6:
                continue
            sc.op('dve', lambda e: e.tensor_reduce(out=s1[:], in_=v8(ysb[:]), axis=AX.X, op=ALU.add), reads=['b_ysb'], writes=['b_s1'])
            sc.op('dve', lambda e: e.tensor_single_scalar(out=s1[:], in_=s1[:], scalar=1.0 / 64, op=ALU.mult), reads=['b_s1'], writes=['b_s1'])
            sc.op('dve', lambda e: e.tensor_tensor(out=v8(ysb[:]), in0=v8(ysb[:]), in1=s1[:].unsqueeze(2).broadcast_to([128, 8, 64]), op=ALU.subtract), reads=['b_ysb', 'b_s1'], writes=['b_ysb'])
            sc.op('act', lambda e: e.activation(out=res[:, 0:512], in_=ysb[:], func=AF.Square), reads=['b_ysb'], writes=['b_zl'])
            sc.op('dve', lambda e: e.tensor_reduce(out=s2[:], in_=v8(res[:, 0:512]), axis=AX.X, op=ALU.add), reads=['b_zl'], writes=['b_s2'])
            sc.op('act', lambda e: e.activation(out=s2[:], in_=s2[:], func=AF.Sqrt, bias=K.eps_lnx[:, 0:1], scale=1.0 / 64), reads=['b_s2', 'eps_lnx'], writes=['b_s2'])
            sc.op('dve', lambda e: e.reciprocal(out=s2[:], in_=s2[:]), reads=['b_s2'], writes=['b_s2'])
            sc.op('dve', lambda e: e.tensor_tensor(out=v8(ysb[:]), in0=v8(ysb[:]), in1=s2[:].unsqueeze(2).broadcast_to([128, 8, 64]), op=ALU.mult), reads=['b_ysb', 'b_s2'], writes=['b_ysb'])
            sc.op('dve', lambda e: e.tensor_tensor(out=ysb[:], in0=ysb[:], in1=lnxg[:], op=ALU.mult), reads=['b_ysb', 'b_lnxg'], writes=['b_ysb'])
            sc.op('dve', lambda e: e.tensor_tensor(out=ysb[:], in0=ysb[:], in1=lnxb[:], op=ALU.add), reads=['b_ysb', 'b_lnxb'], writes=['b_ysb'])
            sc.op('dve', lambda e: e.tensor_tensor(out=v8(res[:, 512:1024]), in0=v8(Vt[:].bitcast(F32)), in1=bs[:].unsqueeze(2).broadcast_to([128, 8, 64]), op=ALU.mult), reads=['b_V', 'b_bs', 'b_zl'], writes=['b_zl'])
            sc.op('dve', lambda e: e.tensor_tensor(out=ysb[:], in0=ysb[:], in1=res[:, 512:1024], op=ALU.add), reads=['b_ysb', 'b_zl'], writes=['b_ysb'])
            sc.op('dve', lambda e: e.tensor_tensor(out=ysb[:], in0=ysb[:], in1=g_tm[:], op=ALU.mult), reads=['b_ysb', 'b_gtm'], writes=['b_ysb'])
            if DBG["stage"] <= 7:
                continue
            sc.dma('pool', lambda e: e.dma_start(out=mixT[:, 0:4, :], in_=S.aT[:, :, ti * 128:(ti + 1) * 128].rearrange("c p t -> p c t")), writes=['b_hT'])
            transpose_to(nc, sc, K, ysb, 'b_ysb', lambda c0, nn: mixT[:, 4 + c0:4 + c0 + nn, :], 'b_hT', 4, evac='act')
            for hf in range(2):
                b = K.nb()
                for k in range(8):
                    sc.op('pe', lambda e, k=k: e.matmul(K.ps[b][:, 0:512], mixT[:, k, :], wO[:, k, hf * 512:(hf + 1) * 512], start=(k == 0), stop=(k == 7)),
                          reads=['b_hT', 'b_hT', 'b_wO'], writes=[('ps', b)])
                sc.op('dve', lambda e: e.scalar_tensor_tensor(out=res[:, hf * 512:(hf + 1) * 512], in0=h[:, hf * 512:(hf + 1) * 512], scalar=ALPHA, in1=K.ps[b][:, 0:512], op0=ALU.mult, op1=ALU.add),
                      reads=[hk, ('ps', b)], writes=['b_zl'])
            if DBG["stage"] <= 8:
                continue
            layernorm_tm(sc, K, res, 'b_zl', ln1g, 'b_ln1g', ln1b, 'b_ln1b', h, hk, lt)
            store_h_and_hT(nc, sc, K, h, hk, hT, 'b_hT', S.h1, S.h1T, ti)
        sc.flush()


def ffn_generic(nc, sc, K, pfx, hT_d, h_d, experts, FF, gates_d, ln_g, ln_b, out_fn, FP=2):
    nfg = FF // (128 * FP)
    groups = [(0, 9), (9, 8), (17, 8), (25, 8)]
    with ExitStack() as st:
        sb = K.sb
        GM = 9 * 128
        hT = sb(st, pfx + "hT", [128, 8, GM], F32R)
        yac = sb(st, pfx + "yac", [128, 9, D], F32)
        actT = sb(st, pfx + "actT", [128, FP, GM], F32R)
        sg = sb(st, pfx + "sg", [128, 512], F32)
        wg = [sb(st, pfx + f"wg{i}", [128, 8, FP * 128], F32R) for i in range(2)]
        wu = [sb(st, pfx + f"wu{i}", [128, 8, FP * 128], F32R) for i in range(2)]
        wd = [sb(st, pfx + f"wd{i}", [128, FP, D], F32R) for i in range(2)]
        gbc = load_bcast(nc, sc, K, st, pfx + "lng", ln_g, D)
        bbc = load_bcast(nc, sc, K, st, pfx + "lnb", ln_b, D)
        gt = sb(st, pfx + "gt", [128, 9, NEXP], F32)
        hoT = sb(st, pfx + "hoT", [128, 8, 128], F32R) if gates_d is None else None
        lt = ln_tmp(K, st, pfx + "ln")
        it = 0
        for (t0, nt) in groups[:DBG.get('fng', 4)]:
            n = nt * 128
            for k in range(8):
                sc.dma('pool', lambda e, k=k: e.dma_start(out=hT[:, k, 0:n], in_=hT_d[k, :, t0 * 128:t0 * 128 + n]), writes=[pfx + 'hT'])
            sc.dma('sp', lambda e: e.dma_start(out=yac[:, 0:nt, :], in_=h_d[t0 * 128:t0 * 128 + n, :].rearrange("(j p) d -> p j d", p=128)), writes=[pfx + 'yac'])
            sc.op('act', lambda e: e.mul(out=yac[:, 0:nt, :], in_=yac[:, 0:nt, :], mul=ALPHA), reads=[pfx + 'yac'], writes=[pfx + 'yac'])
            if gates_d is not None:
                sc.dma('sp', lambda e: e.dma_start(out=gt[:, 0:nt, :], in_=gates_d[t0 * 128:t0 * 128 + n, :].rearrange("(j p) d -> p j d", p=128)), writes=[pfx + 'gt'])
            spans = [(0, 3), (3, 3), (6, 3)] if nt == 9 else [(0, 4), (4, 4)]
            if DBG.get('fst', 99) <= 1:
                continue
            for ei, (Wg, Wu, Wd) in enumerate(experts):
                for fg in range(nfg):
                    bi = it % 2
                    it += 1
                    f0 = fg * FP * 128
                    sc.dma('pool', lambda e: e.dma_start(out=wg[bi][:], in_=Wg[:, f0:f0 + FP * 128].rearrange("(k p) f -> p k f", p=128)), writes=[pfx + f'wg{bi}'])
                    sc.dma('pool', lambda e: e.dma_start(out=wu[bi][:], in_=Wu[:, f0:f0 + FP * 128].rearrange("(k p) f -> p k f", p=128)), writes=[pfx + f'wu{bi}'])
                    sc.dma('pool', lambda e: e.dma_start(out=wd[bi][:], in_=Wd[f0:f0 + FP * 128, :].rearrange("(c p) d -> p c d", p=128)), writes=[pfx + f'wd{bi}'])
                    for (s0, sn) in spans:
                        c0 = s0 * 128
                        m = sn * 128
                        for fc in range(FP):
                            bgp = K.nb()
                            bup = K.nb()
                            for k in range(8):
                                sc.op('pe', lambda e, k=k: e.matmul(K.ps[bgp][:, 0:m], wg[bi][:, k, fc * 128:(fc + 1) * 128], hT[:, k, c0:c0 + m], start=(k == 0), stop=(k == 7)),
                                      reads=[pfx + f'wg{bi}', pfx + 'hT'], writes=[('ps', bgp)])
                            for k in range(8):
                                sc.op('pe', lambda e, k=k: e.matmul(K.ps[bup][:, 0:m], wu[bi][:, k, fc * 128:(fc + 1) * 128], hT[:, k, c0:c0 + m], start=(k == 0), stop=(k == 7)),
                                      reads=[pfx + f'wu{bi}', pfx + 'hT'], writes=[('ps', bup)])
                            sc.op('act', lambda e: e.activation(out=sg[:, 0:m], in_=K.ps[bgp][:, 0:m], func=AF.Silu), reads=[('ps', bgp)], writes=[pfx + 'sg'])
                            sc.op('dve', lambda e: e.tensor_tensor(out=actT[:, fc, c0:c0 + m], in0=K.ps[bup][:, 0:m], in1=sg[:, 0:m], op=ALU.mult),
                                  reads=[('ps', bup), pfx + 'sg'], writes=[pfx + 'actT'])
                    for j in range(nt):
                        for hf in range(2):
                            b = K.nb()
                            for fc in range(FP):
                                sc.op('pe', lambda e, fc=fc: e.matmul(K.ps[b][:, 0:512], actT[:, fc, j * 128:(j + 1) * 128], wd[bi][:, fc, hf * 512:(hf + 1) * 512], start=(fc == 0), stop=(fc == FP - 1)),
                                      reads=[pfx + 'actT', pfx + f'wd{bi}'], writes=[('ps', b)])
                            ysl = yac[:, j, hf * 512:(hf + 1) * 512]
                            if gates_d is not None:
                                sc.op('dve', lambda e: e.scalar_tensor_tensor(out=ysl, in0=K.ps[b][:, 0:512], scalar=gt[:, j, ei:ei + 1], in1=ysl, op0=ALU.mult, op1=ALU.add),
                                      reads=[('ps', b), pfx + 'gt', pfx + 'yac'], writes=[pfx + 'yac'])
                            else:
                                sc.op('dve', lambda e: e.tensor_tensor(out=ysl, in0=K.ps[b][:, 0:512], in1=ysl, op=ALU.add), reads=[('ps', b), pfx + 'yac'], writes=[pfx + 'yac'])
            if DBG.get('fst', 99) <= 2:
                continue
            for j in range(nt):
                ho = yac[:, j, :]
                layernorm_tm(sc, K, ho, pfx + 'yac', gbc, pfx + 'lng', bbc, pfx + 'lnb', ho, pfx + 'yac', lt)
                if DBG.get('fst', 99) <= 3:
                    continue
                out_fn(t0 + j, ho, pfx + 'yac', hoT, pfx + 'hoT')
        sc.flush()


def phase_ffn0(nc, sc, K, I, S):
    def out_fn(ti, ho, hok, hoT, hoTk):
        store_h_and_hT(nc, sc, K, ho, hok, hoT, hoTk, S.h2, S.h2T, ti)
    ffn_generic(nc, sc, K, "f_", S.h1T, S.h1, [(I.f_gate, I.f_up, I.f_down)], DFF0, None, I.ln2_g, I.ln2_b, out_fn)


def phase_moe(nc, sc, K, I, S):
    def out_fn(ti, ho, hok, hoT, hoTk):
        if ti == 0:
            return
        sc.dma('sp', lambda e: e.dma_start(out=I.out[(ti - 1) * 128:ti * 128, :], in_=ho[:]), reads=[hok], writes=['out'])
    experts = [(I.e_gate[e], I.e_up[e], I.e_down[e]) for e in range(NEXP)]
    ffn_generic(nc, sc, K, "m_", S.h3T, S.h3, experts, DFFE, S.gates, I.oln2_g, I.oln2_b, out_fn, FP=4)


def phase_attn(nc, sc, K, I, S):
    with ExitStack() as st:
        sb = K.sb
        wq = sb(st, "t_wq", [128, 8, 1280], F32R)
        load_weight_r(nc, sc, K, wq, 't_wq', I.w_qkv, 8, 1280)
        wo = sb(st, "t_wo", [128, 8, 1024], F32R)
        load_weight_r(nc, sc, K, wo, 't_wo', I.w_o, 8, 1024)
        bq = load_bcast(nc, sc, K, st, "t_bq", I.b_qkv, 1280)
        bo = load_bcast(nc, sc, K, st, "t_bo", I.b_o, D)
        g1 = load_bcast(nc, sc, K, st, "t_g1", I.oln1_g, D)
        b1 = load_bcast(nc, sc, K, st, "t_b1", I.oln1_b, D)
        snk = load_bcast(nc, sc, K, st, "t_snk", I.sinks, 16)
        sc.op('act', lambda e: e.activation(out=snk[:], in_=snk[:], func=AF.Exp), reads=['t_snk'], writes=['t_snk'])
        rtr = sb(st, "t_rtr", [128, 8, NEXP], F32R)
        sc.dma('pool', lambda e: e.dma_start(out=rtr[:], in_=I.router.rearrange("(k p) e -> p k e", p=128)), writes=['t_rtr'])
        rowi = sb(st, "t_rowi", [128, 128], I32)
        rowm = sb(st, "t_rowm", [128, 128], F32)
        tmpm = sb(st, "t_tmpm", [128, 128], F32)
        sc.op('pool', lambda e: e.iota(rowi[:], pattern=[[0, 128]], base=-PADF, channel_multiplier=1), writes=['t_rowi'])
        sc.op('dve', lambda e: e.tensor_single_scalar(out=rowm[:], in_=rowi[:], scalar=0, op=ALU.is_ge), reads=['t_rowi'], writes=['t_rowm'])
        mbs = {}
        for nm, m, rm in (('cur', K.m_ge, False), ('prev', K.m_lt, False), ('cur0', K.m_ge, True), ('prev1', K.m_lt, True)):
            t = sb(st, "t_mb_" + nm, [128, 128], F32R)
            if rm:
                sc.op('dve', lambda e, m=m: e.tensor_tensor(out=tmpm[:], in0=m[:], in1=rowm[:], op=ALU.mult), reads=['t_rowm', 't_tmpm'], writes=['t_tmpm'])
                sc.op('dve', lambda e, t=t: e.tensor_scalar(out=t[:], in0=tmpm[:], scalar1=-1.0, scalar2=30000.0, op0=ALU.add, op1=ALU.mult), reads=['t_tmpm'], writes=['t_mb'])
            else:
                sc.op('dve', lambda e, t=t, m=m: e.tensor_scalar(out=t[:], in0=m[:], scalar1=-1.0, scalar2=30000.0, op0=ALU.add, op1=ALU.mult), writes=['t_mb'])
            mbs[nm] = t
        hT2 = sb(st, "t_hT", [128, 8, 128], F32R)
        h2 = sb(st, "t_h2", [128, D], F32)
        qkv = sb(st, "t_qkv", [128, 1280], F32)
        qr = sb(st, "t_qr", [128, 1152], F32)
        ta = sb(st, "t_ta", [128, 8, 32], F32)
        tb = sb(st, "t_tb", [128, 8, 32], F32)
        cs = sb(st, "t_cos", [128, 32], F32)
        sn = sb(st, "t_sin", [128, 32], F32)
        qT = sb(st, "t_qT", [128, 8, 128], F32R)
        kT = [sb(st, f"t_kT{i}", [128, 128], F32R) for i in range(2)]
        v1 = [sb(st, f"t_v{i}", [128, 2, 66], F32R) for i in range(2)]
        Ec = sb(st, "t_Ec", [128, 512], F32R)
        Ep = sb(st, "t_Ep", [128, 512], F32R)
        osb = sb(st, "t_osb", [128, 16, 66], F32)
        den = sb(st, "t_den", [128, 16], F32)
        att = sb(st, "t_att", [128, D], F32)
        attT = sb(st, "t_attT", [128, 8, 128], F32R)
        res = sb(st, "t_res", [128, D], F32)
        h3 = sb(st, "t_h3", [128, D], F32)
        h3T = sb(st, "t_h3T", [128, 8, 128], F32R)
        lg = sb(st, "t_lg", [128, 8], F32)
        m8 = sb(st, "t_m8", [128, 8], F32)
        ex = sb(st, "t_ex", [128, 8], F32)
        gsm = sb(st, "t_gsm", [128, 2], F32)
        lt = ln_tmp(K, st, "t_ln")
        for i in range(2):
            sc.op('dve', lambda e, i=i: e.tensor_copy(out=v1[i][:, :, 0:64], in_=K.zero_f[:, 0:128].rearrange("p (g d) -> p g d", g=2)), reads=['zero_f'], writes=[f't_v{i}'])
            sc.op('dve', lambda e, i=i: e.tensor_copy(out=v1[i][:, :, 64:65], in_=K.ones_f[:, 0:2].unsqueeze(2)), reads=['ones_f', f't_v{i}'], writes=[f't_v{i}'])
            sc.op('dve', lambda e, i=i: e.tensor_copy(out=v1[i][:, :, 65:66], in_=K.zero_f[:, 0:2].unsqueeze(2)), reads=['zero_f', f't_v{i}'], writes=[f't_v{i}'])
        B83 = [128, 8, 32]
        for ti in range(NT):
            cu, pv = ti % 2, 1 - ti % 2
            sc.dma('pool', lambda e: e.dma_start(out=hT2[:], in_=S.h2T[:, :, ti * 128:(ti + 1) * 128].rearrange("c p t -> p c t")), writes=['t_hT'])
            sc.dma('sp', lambda e: e.dma_start(out=h2[:], in_=S.h2[ti * 128:(ti + 1) * 128, :]), writes=['t_h2'])
            sc.dma('sp', lambda e: e.dma_start(out=cs[:], in_=I.rope_cos[ti * 128:(ti + 1) * 128, :]), writes=['t_cos'])
            sc.dma('sp', lambda e: e.dma_start(out=sn[:], in_=I.rope_sin[ti * 128:(ti + 1) * 128, :]), writes=['t_sin'])
            for (c0, cw) in ((0, 512), (512, 512), (1024, 256)):
                b = K.nb()
                for k in range(8):
                    sc.op('pe', lambda e, k=k: e.matmul(K.ps[b][:, 0:cw], hT2[:, k, :], wq[:, k, c0:c0 + cw], start=(k == 0), stop=(k == 7)), reads=['t_hT', 't_wq'], writes=[('ps', b)])
                sc.op('dve', lambda e: e.tensor_tensor(out=qkv[:, c0:c0 + cw], in0=K.ps[b][:, 0:cw], in1=bq[:, c0:c0 + cw], op=ALU.add), reads=[('ps', b), 't_bq'], writes=['t_qkv'])
            cb = cs[:].unsqueeze(1)
            sb_ = sn[:].unsqueeze(1)
            parts = []
            for g in range(2):
                vin = qkv[:, g * 512:(g + 1) * 512].rearrange("p (c two d) -> p c two d", two=2, d=32)
                vout = qr[:, 0:1024].rearrange("p (c g two d) -> p c g two d", g=2, two=2, d=32)
                parts.append((8, vin[:, :, 0, :], vin[:, :, 1, :], vout[:, :, g, 0, :], vout[:, :, g, 1, :]))
            vin = qkv[:, 1024:1152].rearrange("p (c two d) -> p c two d", two=2, d=32)
            vout = qr[:, 1024:1152].rearrange("p (c two d) -> p c two d", two=2, d=32)
            parts.append((2, vin[:, :, 0, :], vin[:, :, 1, :], vout[:, :, 0, :], vout[:, :, 1, :]))
            for (nh, x1, x2, o1, o2) in parts:
                shp = [128, nh, 32]
                cbb = cb.broadcast_to(shp)
                sbb = sb_.broadcast_to(shp)
                sc.op('dve', lambda e: e.tensor_tensor(out=ta[:, 0:nh, :], in0=x1, in1=cbb, op=ALU.mult), reads=['t_qkv', 't_cos'], writes=['t_ta'])
                sc.op('dve', lambda e: e.tensor_tensor(out=tb[:, 0:nh, :], in0=x2, in1=sbb, op=ALU.mult), reads=['t_qkv', 't_sin'], writes=['t_tb'])
                sc.op('dve', lambda e: e.tensor_tensor(out=o1, in0=ta[:, 0:nh, :], in1=tb[:, 0:nh, :], op=ALU.subtract), reads=['t_ta', 't_tb'], writes=['t_qr'])
                sc.op('dve', lambda e: e.tensor_tensor(out=ta[:, 0:nh, :], in0=x2, in1=cbb, op=ALU.mult), reads=['t_qkv', 't_cos', 't_ta'], writes=['t_ta'])
                sc.op('dve', lambda e: e.tensor_tensor(out=tb[:, 0:nh, :], in0=x1, in1=sbb, op=ALU.mult), reads=['t_qkv', 't_sin', 't_tb'], writes=['t_tb'])
                sc.op('dve', lambda e: e.tensor_tensor(out=o2, in0=ta[:, 0:nh, :], in1=tb[:, 0:nh, :], op=ALU.add), reads=['t_ta', 't_tb'], writes=['t_qr'])
            transpose_to(nc, sc, K, qr, 't_qr', lambda c0, nn: qT[:, c0:c0 + nn, :], 't_qT', 8, evac='act')
            bk = K.nb()
            sc.op('pe', lambda e: e.transpose(K.ps[bk][:, 0:128], qr[:, 1024:1152], K.ident[:]), reads=['t_qr', 'ident'], writes=[('ps', bk)])
            sc.op('act', lambda e: e.copy(out=kT[cu][:], in_=K.ps[bk][:, 0:128]), reads=[('ps', bk)], writes=[f't_kT{cu}'])
            sc.op('act', lambda e: e.copy(out=v1[cu][:, :, 0:64], in_=qkv[:, 1152:1280].rearrange("p (g d) -> p g d", g=2)), reads=['t_qkv'], writes=[f't_v{cu}'])
            mcur = mbs['cur0'] if ti == 0 else mbs['cur']
            mprev = mbs['prev1'] if ti == 1 else mbs['prev']
            for g in range(2):
                p0 = 64 * g
                for hf in range(2):
                    bc_ = K.nb()
                    for j in range(4):
                        c = hf * 4 + j
                        sc.op('pe', lambda e, j=j, c=c: e.matmul(K.ps[bc_][:, j * 128:(j + 1) * 128], kT[cu][p0:p0 + 64, :], qT[p0:p0 + 64, c, :], start=True, stop=False), reads=[f't_kT{cu}', 't_qT'], writes=[('ps', bc_)])
                        sc.op('pe', lambda e, j=j: e.matmul(K.ps[bc_][:, j * 128:(j + 1) * 128], K.identr[:], mcur[:], start=False, stop=True), reads=['identr', 't_mb'], writes=[('ps', bc_)])
                    sc.op('act', lambda e: e.activation(out=Ec[:], in_=K.ps[bc_][:, 0:512], func=AF.Exp, scale=0.125), reads=[('ps', bc_)], writes=['t_Ec'])
                    if ti > 0:
                        bp_ = K.nb()
                        for j in range(4):
                            c = hf * 4 + j
                            sc.op('pe', lambda e, j=j, c=c: e.matmul(K.ps[bp_][:, j * 128:(j + 1) * 128], kT[pv][p0:p0 + 64, :], qT[p0:p0 + 64, c, :], start=True, stop=False), reads=[f't_kT{pv}', 't_qT'], writes=[('ps', bp_)])
                            sc.op('pe', lambda e, j=j: e.matmul(K.ps[bp_][:, j * 128:(j + 1) * 128], K.identr[:], mprev[:], start=False, stop=True), reads=['identr', 't_mb'], writes=[('ps', bp_)])
                        sc.op('act', lambda e: e.activation(out=Ep[:], in_=K.ps[bp_][:, 0:512], func=AF.Exp, scale=0.125), reads=[('ps', bp_)], writes=['t_Ep'])
                    bo_ = K.nb()
                    for j in range(4):
                        osl = K.ps[bo_][:, j * 66:(j + 1) * 66]
                        if ti > 0:
                            sc.op('pe', lambda e, j=j, osl=osl: e.matmul(osl, Ep[:, j * 128:(j + 1) * 128], v1[pv][:, g, :], start=True, stop=False), reads=['t_Ep', f't_v{pv}'], writes=[('ps', bo_)])
                        sc.op('pe', lambda e, j=j, osl=osl: e.matmul(osl, Ec[:, j * 128:(j + 1) * 128], v1[cu][:, g, :], start=(ti == 0), stop=True), reads=['t_Ec', f't_v{cu}'], writes=[('ps', bo_)])
                    h0 = 8 * g + 4 * hf
                    sc.op('act', lambda e: e.copy(out=osb[:, h0:h0 + 4, :], in_=K.ps[bo_][:, 0:264].rearrange("p (h d) -> p h d", d=66)), reads=[('ps', bo_)], writes=['t_osb'])
            sc.op('dve', lambda e: e.tensor_tensor(out=den[:].unsqueeze(2), in0=osb[:, :, 64:65], in1=snk[:].unsqueeze(2), op=ALU.add), reads=['t_osb', 't_snk'], writes=['t_den'])
            sc.op('dve', lambda e: e.reciprocal(out=den[:], in_=den[:]), reads=['t_den'], writes=['t_den'])
            sc.op('dve', lambda e: e.tensor_tensor(out=att[:].rearrange("p (h d) -> p h d", d=64), in0=osb[:, :, 0:64], in1=den[:].unsqueeze(2).broadcast_to([128, 16, 64]), op=ALU.mult), reads=['t_osb', 't_den'], writes=['t_att'])
            transpose_to(nc, sc, K, att, 't_att', lambda c0, nn: attT[:, c0:c0 + nn, :], 't_attT', 8, evac='act')
            for hf in range(2):
                b = K.nb()
                for k in range(8):
                    sc.op('pe', lambda e, k=k: e.matmul(K.ps[b][:, 0:512], attT[:, k, :], wo[:, k, hf * 512:(hf + 1) * 512], start=(k == 0), stop=(k == 7)), reads=['t_attT', 't_wo'], writes=[('ps', b)])
                sc.op('dve', lambda e: e.scalar_tensor_tensor(out=res[:, hf * 512:(hf + 1) * 512], in0=h2[:, hf * 512:(hf + 1) * 512], scalar=ALPHA, in1=K.ps[b][:, 0:512], op0=ALU.mult, op1=ALU.add), reads=['t_h2', ('ps', b)], writes=['t_res'])
            sc.op('dve', lambda e: e.tensor_tensor(out=res[:], in0=res[:], in1=bo[:], op=ALU.add), reads=['t_res', 't_bo'], writes=['t_res'])
            layernorm_tm(sc, K, res, 't_res', g1, 't_g1', b1, 't_b1', h3, 't_h3', lt)
            store_h_and_hT(nc, sc, K, h3, 't_h3', h3T, 't_h3T', S.h3, S.h3T, ti)
            br = K.nb()
            for k in range(8):
                sc.op('pe', lambda e, k=k: e.matmul(K.ps[br][:, 0:8], h3T[:, k, :], rtr[:, k, :], start=(k == 0), stop=(k == 7)), reads=['t_h3T', 't_rtr'], writes=[('ps', br)])
            sc.op('act', lambda e: e.copy(out=lg[:], in_=K.ps[br][:, 0:8]), reads=[('ps', br)], writes=['t_lg'])
            sc.op('dve', lambda e: e.max(out=m8[:], in_=lg[:]), reads=['t_lg'], writes=['t_m8'])
            sc.op('dve', lambda e: e.tensor_single_scalar(out=gsm[:, 0:1], in_=m8[:, 0:1], scalar=-1.0, op=ALU.mult), reads=['t_m8'], writes=['t_gsm'])
            sc.op('act', lambda e: e.activation(out=ex[:], in_=lg[:], func=AF.Exp, bias=gsm[:, 0:1], scale=1.0), reads=['t_lg', 't_gsm'], writes=['t_ex'])
            sc.op('dve', lambda e: e.tensor_scalar(out=lg[:], in0=lg[:], scalar1=m8[:, 1:2], scalar2=None, op0=ALU.is_ge), reads=['t_lg', 't_m8'], writes=['t_lg'])
            sc.op('dve', lambda e: e.tensor_tensor(out=ex[:], in0=ex[:], in1=lg[:], op=ALU.mult), reads=['t_ex', 't_lg'], writes=['t_ex'])
            sc.op('dve', lambda e: e.tensor_reduce(out=gsm[:, 1:2], in_=ex[:], axis=AX.X, op=ALU.add), reads=['t_ex', 't_gsm'], writes=['t_gsm'])
            sc.op('dve', lambda e: e.reciprocal(out=gsm[:, 1:2], in_=gsm[:, 1:2]), reads=['t_gsm'], writes=['t_gsm'])
            sc.op('dve', lambda e: e.tensor_scalar(out=ex[:], in0=ex[:], scalar1=gsm[:, 1:2], scalar2=None, op0=ALU.mult), reads=['t_ex', 't_gsm'], writes=['t_ex'])
            sc.dma('sp', lambda e: e.dma_start(out=S.gates[ti * 128:(ti + 1) * 128, :], in_=ex[:]), reads=['t_ex'], writes=['S.gates'])
        sc.flush()


_PARAM_KEYS = ["meta_tokens", "ev_w_in", "ev_conv_w", "ev_conv_b", "ev_convnorm_g", "ev_convnorm_b", "ev_shift_mu",
               "ev_w0", "ev_w2", "ev_a0", "ev_a2", "ev_g2", "ev_k_k", "ev_k_a", "ev_r_k", "ev_lnx_g", "ev_lnx_b",
               "ev_w_out", "ev_ln1_g", "ev_ln1_b", "ev_ffn_gate", "ev_ffn_up", "ev_ffn_down", "ev_ln2_g", "ev_ln2_b",
               "od_w_qkv", "od_b_qkv", "od_sinks", "od_w_o", "od_b_o", "od_ln1_g", "od_ln1_b", "od_router",
               "od_exp_gate", "od_exp_up", "od_exp_down", "od_ln2_g", "od_ln2_b"]


def make_in_maps(inputs, cores):
    shared = {}
    for k in _PARAM_KEYS:
        a = np.asarray(inputs[k], dtype=np.float32)
        if k != "meta_tokens":
            a = a[0]
        if k == "ev_r_k":
            a = a.reshape(512)
        shared[k] = np.ascontiguousarray(a)
    pos = (np.arange(T, dtype=np.float64) - PADF)[:, None]
    inv = 10000.0 ** (-np.arange(32, dtype=np.float64) / 32.0)[None, :]
    shared["rope_cos"] = np.cos(pos * inv).astype(np.float32)
    shared["rope_sin"] = np.sin(pos * inv).astype(np.float32)
    maps = []
    for b in cores:
        m = dict(shared)
        m["x"] = np.ascontiguousarray(np.asarray(inputs["x"][b], dtype=np.float32))
        maps.append(m)
    return maps


def kernel(**inputs):
    nc = build_program()
    in_maps = make_in_maps(inputs, list(range(8)))
    res = run_bass_kernel_spmd(nc, in_maps, core_ids=list(range(8)))
    out = np.stack([np.asarray(r["out"]) for r in res.results], axis=0)
    return out.astype(np.float32)
```

```python
import numpy as np
from contextlib import ExitStack
import concourse.bass as bass
import concourse.mybir as mybir
from concourse.bass_utils import run_bass_kernel_spmd

F32 = mybir.dt.float32
F32R = mybir.dt.float32r
I32 = mybir.dt.int32
AF = mybir.ActivationFunctionType
ALU = mybir.AluOpType
AX = mybir.AxisListType

D = 1024
SEQ = 4096
NMETA = 16
PADF = 112
T = SEQ + NMETA + PADF
NT = T // 128
DFF0 = 2816
DFFE = 3584
NEXP = 8
ALPHA = 4.0 ** 0.25
LN_EPS = 1e-5
LNX_EPS = 64e-5
DECAY_SCALE = float(np.exp(-0.5))


class Sched:
    EPOCH = 30000
    NDSEM = 6
    ENG = ('pe', 'act', 'dve', 'pool', 'sp')

    def __init__(self, nc=None, es=None, needs=None):
        self.dry = needs is None
        self.needs = [] if self.dry else needs
        self.nc = nc
        self.es = es
        self.gi = 0
        self.seg = []
        self.lastw = {}
        self.readers = {}
        self.last_on = {}
        if not self.dry:
            self.eobj = dict(pe=nc.tensor, act=nc.scalar, dve=nc.vector, pool=nc.gpsimd, sp=nc.sync)
            self.csem = {}
            self.nsig = {e: 0 for e in self.ENG}
            self.dsem = {q: [es.enter_context(nc.semaphore(f"d_{q}_{i}")) for i in range(self.NDSEM)]
                         for q in ('sp', 'pool')}
            self.ndma = {'sp': 0, 'pool': 0}
            self.dma_ev = {'sp': [], 'pool': []}
            self.seen = {e: {} for e in self.ENG}
            self.last_ev = {}
            self.events = {}

    def op(self, eng, fn, reads=(), writes=()):
        self._do(False, eng, fn, reads, writes)

    def dma(self, q, fn, reads=(), writes=()):
        self._do(True, q, fn, reads, writes)

    def _csem(self, eng, epoch):
        k = (eng, epoch)
        if k not in self.csem:
            self.csem[k] = self.es.enter_context(self.nc.semaphore(f"c_{eng}_{epoch}"))
        return self.csem[k]

    def _wait(self, eng, ev):
        key, sem, val = ev
        if self.seen[eng].get(key, 0) >= val:
            return
        self.eobj[eng].wait_ge(sem, val)
        self.seen[eng][key] = val

    def _do(self, isd, eng, fn, reads, writes):
        i = self.gi
        self.gi += 1
        d = set()
        for k in reads:
            if k in self.lastw:
                d.add(self.lastw[k])
        for k in writes:
            if k in self.lastw:
                d.add(self.lastw[k])
            for r in self.readers.get(k, ()):
                d.add(r)
        deps = []
        for (j, jd, je) in d:
            if (not jd) and je == eng and eng == 'pe':
                continue
            deps.append(j)
            if self.dry and not jd:
                self.needs[j] = True
        me = (i, isd, eng)
        for k in writes:
            self.lastw[k] = me
            self.readers[k] = []
        for k in reads:
            self.readers.setdefault(k, []).append(me)
        if not isd:
            self.last_on[eng] = i
        if self.dry:
            self.needs.append(False)
            return
        if isd:
            k = self.ndma[eng]
            if k >= self.NDSEM:
                self._wait(eng, self.dma_ev[eng][k - self.NDSEM])
        for j in sorted(deps):
            self._wait(eng, self.events[j])
        inst = fn(self.eobj[eng])
        if isd:
            k = self.ndma[eng]
            idx = k % self.NDSEM
            val = 16 * (k // self.NDSEM + 1)
            sem = self.dsem[eng][idx]
            inst.then_inc(sem, 16)
            ev = ((eng, 'd', idx), sem, val)
            self.dma_ev[eng].append(ev)
            self.ndma[eng] = k + 1
            self.events[i] = ev
        elif self.needs[i]:
            c = self.nsig[eng]
            epoch, v = divmod(c, self.EPOCH)
            sem = self._csem(eng, epoch)
            inst.then_inc(sem, 1)
            self.nsig[eng] = c + 1
            ev = ((eng, 'c', epoch), sem, v + 1)
            self.events[i] = ev
            self.last_ev[eng] = ev

    def flush(self):
        if self.dry:
            for e, i in self.last_on.items():
                self.needs[i] = True
        else:
            for e in self.ENG:
                for e2, ev in self.last_ev.items():
                    if e2 != e or e != 'pe':
                        self._wait(e, ev)
                for q in ('sp', 'pool'):
                    for ev in self.dma_ev[q][-self.NDSEM:]:
                        self._wait(e, ev)
            self.events = {}
        self.lastw = {}
        self.readers = {}
        self.last_on = {}


class Ctx:
    pass


DECLARED = set()
DBG = dict(nt=NT, stage=99)


def build_program(upto=99, debug=False):
    nc0 = bass.Bass("TRN2", target_bir_lowering=False)
    dry = Sched()
    with ExitStack() as es0:
        _build(nc0, es0, upto, debug, dry)
    nc = bass.Bass("TRN2", target_bir_lowering=False)
    with ExitStack() as es:
        sc = Sched(nc, es, needs=dry.needs)
        _build(nc, es, upto, debug, sc)
        assert sc.gi == dry.gi
    return nc


def _build(nc, es, upto, debug, sc):
    def din(name, shape):
        if upto < 5 and name.startswith("od_exp"):
            return None
        DECLARED.add(name)
        return nc.dram_tensor(name, list(shape), F32, kind="ExternalInput").ap()

    def dscr(name, shape):
        return nc.dram_tensor(name, list(shape), F32, kind="ExternalOutput" if debug else "Internal").ap()

    def dout(name, shape):
        return nc.dram_tensor(name, list(shape), F32, kind="ExternalOutput").ap()

    I = Ctx()
    I.x = din("x", [SEQ, D])
    I.meta = din("meta_tokens", [NMETA, D])
    I.w_in = din("ev_w_in", [D, 2816])
    I.conv_w = din("ev_conv_w", [31, 512])
    I.conv_b = din("ev_conv_b", [512])
    I.cn_g = din("ev_convnorm_g", [512])
    I.cn_b = din("ev_convnorm_b", [512])
    I.mu = din("ev_shift_mu", [1792])
    I.w0 = din("ev_w0", [512])
    I.w2 = din("ev_w2", [64, 512])
    I.a0 = din("ev_a0", [512])
    I.a2 = din("ev_a2", [64, 512])
    I.g2 = din("ev_g2", [128, 512])
    I.k_k = din("ev_k_k", [512])
    I.k_a = din("ev_k_a", [512])
    I.r_k = din("ev_r_k", [512])
    I.lnx_g = din("ev_lnx_g", [512])
    I.lnx_b = din("ev_lnx_b", [512])
    I.w_out = din("ev_w_out", [D, D])
    I.ln1_g = din("ev_ln1_g", [D])
    I.ln1_b = din("ev_ln1_b", [D])
    I.f_gate = din("ev_ffn_gate", [D, DFF0])
    I.f_up = din("ev_ffn_up", [D, DFF0])
    I.f_down = din("ev_ffn_down", [DFF0, D])
    I.ln2_g = din("ev_ln2_g", [D])
    I.ln2_b = din("ev_ln2_b", [D])
    I.w_qkv = din("od_w_qkv", [D, 1280])
    I.b_qkv = din("od_b_qkv", [1280])
    I.sinks = din("od_sinks", [16])
    I.w_o = din("od_w_o", [D, D])
    I.b_o = din("od_b_o", [D])
    I.oln1_g = din("od_ln1_g", [D])
    I.oln1_b = din("od_ln1_b", [D])
    I.router = din("od_router", [D, NEXP])
    I.e_gate = din("od_exp_gate", [NEXP, D, DFFE])
    I.e_up = din("od_exp_up", [NEXP, D, DFFE])
    I.e_down = din("od_exp_down", [NEXP, DFFE, D])
    I.oln2_g = din("od_ln2_g", [D])
    I.oln2_b = din("od_ln2_b", [D])
    I.rope_cos = din("rope_cos", [T, 32])
    I.rope_sin = din("rope_sin", [T, 32])
    I.out = dout("out", [SEQ, D])

    S = Ctx()
    S.aT = dscr("s_aT", [4, 128, T])
    S.h1 = dscr("s_h1", [T, D])
    S.h1T = dscr("s_h1T", [8, 128, T])
    S.h2 = dscr("s_h2", [T, D])
    S.h2T = dscr("s_h2T", [8, 128, T])
    S.h3 = dscr("s_h3", [T, D])
    S.h3T = dscr("s_h3T", [8, 128, T])
    S.gates = dscr("s_gates", [T, NEXP])
    K = Ctx()
    K.psall = es.enter_context(nc.psum_tensor("psall", [128, 4096], F32))
    K.ps = [K.psall[:, b * 512:(b + 1) * 512] for b in range(8)]
    K.bank = [0]

    def sb(st, name, shape, dt=F32):
        return st.enter_context(nc.sbuf_tensor(name, list(shape), dt))
    K.sb = sb

    def nb():
        b = K.bank[0]
        K.bank[0] = (b + 1) % 8
        return b
    K.nb = nb

    def nb2():
        b = K.bank[0]
        if b % 2:
            b = (b + 1) % 8
        K.bank[0] = (b + 2) % 8
        return b
    K.nb2 = nb2

    with nc.Block() as block:
        @block.sync
        def _(sync):
            build_consts(nc, es, sc, K, I)
            sc.flush()
            if upto >= 1:
                phase_1a(nc, sc, K, I, S)
            if upto >= 2:
                phase_1b(nc, sc, K, I, S)
            if upto >= 3:
                phase_ffn0(nc, sc, K, I, S)
            if upto >= 4:
                phase_attn(nc, sc, K, I, S)
            if upto >= 5:
                phase_moe(nc, sc, K, I, S)
            if upto < 5:
                phase_fin(nc, sc, K, I, S)


def build_consts(nc, es, sc, K, I):
    sb = K.sb
    K.dI = sb(es, "k_dI", [128, 128], I32)
    K.ident = sb(es, "k_ident", [128, 128], F32)
    K.identr = sb(es, "k_identr", [128, 128], F32R)
    K.onesr = sb(es, "k_onesr", [128, 128], F32R)
    K.blk1r = sb(es, "k_blk1r", [128, 128], F32R)
    K.m_lt = sb(es, "k_mlt", [128, 128], F32)
    K.m_gt = sb(es, "k_mgt", [128, 128], F32)
    K.m_ge = sb(es, "k_mge", [128, 128], F32)
    K.eps_ln = sb(es, "k_epsln", [128, 1], F32)
    K.eps_lnx = sb(es, "k_epslnx", [128, 1], F32)
    K.ones_f = sb(es, "k_onesf", [128, 512], F32)
    K.zero_f = sb(es, "k_zerof", [128, 512], F32)
    sc.op('pool', lambda e: e.iota(K.dI[:], pattern=[[1, 128]], base=0, channel_multiplier=-1), writes=['dI'])
    sc.op('dve', lambda e: e.tensor_single_scalar(out=K.ident[:], in_=K.dI[:], scalar=0, op=ALU.is_equal), reads=['dI'], writes=['ident'])
    sc.op('dve', lambda e: e.tensor_copy(out=K.identr[:], in_=K.ident[:]), reads=['ident'], writes=['identr'])
    sc.op('dve', lambda e: e.tensor_single_scalar(out=K.m_lt[:], in_=K.dI[:], scalar=0, op=ALU.is_lt), reads=['dI'], writes=['m_lt'])
    sc.op('dve', lambda e: e.tensor_single_scalar(out=K.m_gt[:], in_=K.dI[:], scalar=0, op=ALU.is_gt), reads=['dI'], writes=['m_gt'])
    sc.op('dve', lambda e: e.tensor_single_scalar(out=K.m_ge[:], in_=K.dI[:], scalar=0, op=ALU.is_ge), reads=['dI'], writes=['m_ge'])
    sc.op('dve', lambda e: e.memset(K.ones_f[:], 1.0), writes=['ones_f'])
    sc.op('dve', lambda e: e.memset(K.zero_f[:], 0.0), writes=['zero_f'])
    sc.op('dve', lambda e: e.tensor_copy(out=K.onesr[:], in_=K.ones_f[:, 0:128]), reads=['ones_f'], writes=['onesr'])
    sc.op('dve', lambda e: e.tensor_copy(out=K.blk1r[:], in_=K.zero_f[:, 0:128]), reads=['zero_f'], writes=['blk1r'])
    sc.op('dve', lambda e: e.tensor_copy(out=K.blk1r[0:64, 0:64], in_=K.ones_f[0:64, 0:64]), reads=['ones_f', 'blk1r'], writes=['blk1r'])
    sc.op('dve', lambda e: e.tensor_copy(out=K.blk1r[64:128, 64:128], in_=K.ones_f[64:128, 0:64]), reads=['ones_f', 'blk1r'], writes=['blk1r'])
    sc.op('dve', lambda e: e.memset(K.eps_ln[:], LN_EPS), writes=['eps_ln'])
    sc.op('dve', lambda e: e.memset(K.eps_lnx[:], LNX_EPS), writes=['eps_lnx'])


def load_cols(nc, sc, K, st, name, vec_ap, n):
    nch = n // 128
    rows = K.sb(st, name + "_r", [nch, 128], F32)
    cols = K.sb(st, name, [128, nch], F32)
    sc.dma('sp', lambda e: e.dma_start(out=rows[:], in_=vec_ap.rearrange("(c p) -> c p", p=128)), writes=[name + "_r"])
    b = K.nb()
    sc.op('pe', lambda e: e.transpose(K.ps[b][:, 0:nch], rows[:], K.ident[0:nch, 0:nch]), reads=[name + "_r", 'ident'], writes=[('ps', b)])
    sc.op('dve', lambda e: e.tensor_copy(out=cols[:], in_=K.ps[b][:, 0:nch]), reads=[('ps', b)], writes=[name])
    return cols


def load_bcast(nc, sc, K, st, name, vec_ap, n):
    t = K.sb(st, name, [128, n], F32)
    sc.dma('sp', lambda e: e.dma_start(out=t[:], in_=vec_ap.partition_broadcast(128)), writes=[name])
    return t


def load_h_tile(nc, sc, K, I, ht, key, ti):
    if ti == 0:
        sc.op('pool', lambda e: e.memset(ht[:], 0.0), writes=[key])
        sc.dma('sp', lambda e: e.dma_start(out=ht[PADF:128, :], in_=I.meta), writes=[key])
    else:
        sc.dma('sp', lambda e: e.dma_start(out=ht[:], in_=I.x[(ti - 1) * 128: ti * 128, :]), writes=[key])


def transpose_to(nc, sc, K, src, src_key, dst_fn, dst_key, nchunk, evac='act'):
    for c0 in range(0, nchunk, 4):
        n = min(4, nchunk - c0)
        b = K.nb()
        for j in range(n):
            c = c0 + j
            sc.op('pe', lambda e, c=c, j=j, b=b: e.transpose(K.ps[b][:, j * 128:(j + 1) * 128], src[:, c * 128:(c + 1) * 128], K.ident[:]),
                  reads=[src_key, 'ident'], writes=[('ps', b)])
        dst = dst_fn(c0, n)
        if evac == 'act':
            sc.op('act', lambda e, b=b, n=n, dst=dst: e.copy(out=dst, in_=K.ps[b][:, 0:n * 128].rearrange("p (c t) -> p c t", t=128)),
                  reads=[('ps', b)], writes=[dst_key])
        else:
            sc.op('dve', lambda e, b=b, n=n, dst=dst: e.tensor_copy(out=dst, in_=K.ps[b][:, 0:n * 128].rearrange("p (c t) -> p c t", t=128)),
                  reads=[('ps', b)], writes=[dst_key])


def load_weight_r(nc, sc, K, wt, key, w_ap, kchunks, ncols, col0=0, rows0=0):
    step = max(1, 4096 // ncols) if ncols <= 2048 else 1
    for k in range(kchunks):
        for c in range(0, ncols, 2048):
            cw = min(2048, ncols - c)
            sc.dma('pool', lambda e, k=k, c=c, cw=cw: e.dma_start(
                out=wt[:, k, c:c + cw], in_=w_ap[rows0 + k * 128: rows0 + (k + 1) * 128, col0 + c: col0 + c + cw]),
                writes=[key])


def phase_1a(nc, sc, K, I, S):
    with ExitStack() as st:
        sb = K.sb
        wA = sb(st, "a_w", [128, 8, 1024], F32R)
        load_weight_r(nc, sc, K, wA, 'a_w', I.w_in, 8, 1024, col0=0)
        cw = load_cols_multi = None
        convw = sb(st, "a_convw", [128, 4, 31], F32)
        cw_rows = sb(st, "a_convw_r", [31, 512], F32)
        sc.dma('sp', lambda e: e.dma_start(out=cw_rows[:], in_=I.conv_w), writes=['a_convw_r'])
        for c in range(4):
            b = K.nb()
            sc.op('pe', lambda e, c=c, b=b: e.transpose(K.ps[b][:, 0:31], cw_rows[:, c * 128:(c + 1) * 128], K.ident[0:31, 0:31]),
                  reads=['a_convw_r', 'ident'], writes=[('ps', b)])
            sc.op('dve', lambda e, c=c, b=b: e.tensor_copy(out=convw[:, c, :], in_=K.ps[b][:, 0:31]), reads=[('ps', b)], writes=['a_convw'])
        convb = load_cols(nc, sc, K, st, "a_convb", I.conv_b, 512)
        cng = load_cols(nc, sc, K, st, "a_cng", I.cn_g, 512)
        cnb = load_cols(nc, sc, K, st, "a_cnb", I.cn_b, 512)
        SP = 512
        ht = [sb(st, f"a_ht{i}", [128, D], F32) for i in range(2)]
        hT = [sb(st, f"a_hT{i}", [128, 8, SP], F32R) for i in range(2)]
        hg = sb(st, "a_hg", [128, 4, 30 + SP], F32)
        sgt = sb(st, "a_sg", [128, SP], F32)
        acc = [sb(st, f"a_acc{i}", [128, 4, SP], F32) for i in range(2)]
        accr = sb(st, "a_accr", [128, 4, SP], F32R)
        sq = sb(st, "a_sq", [128, 4, SP], F32R)
        mean = sb(st, "a_mean", [128, SP], F32)
        msq = sb(st, "a_msq", [128, SP], F32)
        rstd = sb(st, "a_rstd", [128, SP], F32)
        xc = sb(st, "a_xc", [128, SP], F32)
        yo = [sb(st, f"a_yo{i}", [128, 4, SP], F32) for i in range(2)]
        sc.op('dve', lambda e: e.memset(hg[:], 0.0), writes=['a_hg'])
        spans = [(s, min(4, NT - s)) for s in range(0, NT, 4)]
        nld = 0
        for si, (t0, ntl) in enumerate(spans):
            n = ntl * 128
            hTc = hT[si % 2]
            hk = f"a_hT{si % 2}"
            for j in range(ntl):
                hb = ht[nld % 2]
                hbk = f"a_ht{nld % 2}"
                nld += 1
                load_h_tile(nc, sc, K, I, hb, hbk, t0 + j)
                transpose_to(nc, sc, K, hb, hbk, lambda c0, nn, j=j, hTc=hTc: hTc[:, c0:c0 + nn, j * 128:(j + 1) * 128], hk, 8,
                             evac='act' if j % 2 == 0 else 'dve')
            if si > 0:
                sc.op('dve', lambda e: e.tensor_copy(out=hg[:, :, 0:30], in_=hg[:, :, SP:SP + 30]), reads=['a_hg'], writes=['a_hg'])
            for c in range(4):
                bv = K.nb()
                bg = K.nb()
                for k in range(8):
                    sc.op('pe', lambda e, k=k, c=c, bv=bv: e.matmul(K.ps[bv][:, 0:n], wA[:, k, c * 128:(c + 1) * 128], hTc[:, k, 0:n], start=(k == 0), stop=(k == 7)),
                          reads=['a_w', hk], writes=[('ps', bv)])
                for k in range(8):
                    sc.op('pe', lambda e, k=k, c=c, bg=bg: e.matmul(K.ps[bg][:, 0:n], wA[:, k, 512 + c * 128:512 + (c + 1) * 128], hTc[:, k, 0:n], start=(k == 0), stop=(k == 7)),
                          reads=['a_w', hk], writes=[('ps', bg)])
                sc.op('act', lambda e, bg=bg: e.activation(out=sgt[:, 0:n], in_=K.ps[bg][:, 0:n], func=AF.Sigmoid), reads=[('ps', bg)], writes=['a_sg'])
                sc.op('dve', lambda e, bv=bv, c=c: e.tensor_tensor(out=hg[:, c, 30:30 + n], in0=K.ps[bv][:, 0:n], in1=sgt[:, 0:n], op=ALU.mult),
                      reads=[('ps', bv), 'a_sg'], writes=['a_hg'])
            ac = acc[si % 2]
            ak = f"a_acc{si % 2}"
            acf = ac
            for c in range(4):
                sc.op('dve', lambda e, c=c: e.tensor_scalar(out=acf[:, c, 0:n], in0=hg[:, c, 0:n], scalar1=convw[:, c, 0:1], scalar2=convb[:, c:c + 1], op0=ALU.mult, op1=ALU.add),
                      reads=['a_hg', 'a_convw', 'a_convb'], writes=[ak])
                for k in range(1, 31):
                    o = acf
                    sc.op('dve', lambda e, c=c, k=k, o=o: e.scalar_tensor_tensor(out=o[:, c, 0:n], in0=hg[:, c, k:k + n], scalar=convw[:, c, k:k + 1], in1=acf[:, c, 0:n], op0=ALU.mult, op1=ALU.add),
                          reads=['a_hg', 'a_convw', ak], writes=[ak])
            sc.op('act', lambda e: e.activation(out=sq[:, :, 0:n], in_=acf[:, :, 0:n], func=AF.Square), reads=[ak], writes=['a_sq'])
            sc.op('act', lambda e: e.copy(out=accr[:, :, 0:n], in_=acf[:, :, 0:n]), reads=[ak], writes=['a_accr'])
            b1 = K.nb()
            b2 = K.nb()
            for c in range(4):
                sc.op('pe', lambda e, c=c: e.matmul(K.ps[b1][:, 0:n], K.onesr[:], accr[:, c, 0:n], start=(c == 0), stop=(c == 3)), reads=['onesr', 'a_accr'], writes=[('ps', b1)])
            for c in range(4):
                sc.op('pe', lambda e, c=c: e.matmul(K.ps[b2][:, 0:n], K.onesr[:], sq[:, c, 0:n], start=(c == 0), stop=(c == 3)), reads=['onesr', 'a_sq'], writes=[('ps', b2)])
            sc.op('act', lambda e: e.mul(out=mean[:, 0:n], in_=K.ps[b1][:, 0:n], mul=1.0 / 512), reads=[('ps', b1)], writes=['a_mean'])
            sc.op('dve', lambda e: e.tensor_tensor(out=msq[:, 0:n], in0=mean[:, 0:n], in1=mean[:, 0:n], op=ALU.mult), reads=['a_mean'], writes=['a_msq'])
            sc.op('dve', lambda e: e.scalar_tensor_tensor(out=rstd[:, 0:n], in0=K.ps[b2][:, 0:n], scalar=1.0 / 512, in1=msq[:, 0:n], op0=ALU.mult, op1=ALU.subtract),
                  reads=[('ps', b2), 'a_msq'], writes=['a_rstd'])
            sc.op('act', lambda e: e.activation(out=rstd[:, 0:n], in_=rstd[:, 0:n], func=AF.Sqrt, bias=K.eps_ln[:, 0:1], scale=1.0), reads=['a_rstd', 'eps_ln'], writes=['a_rstd'])
            sc.op('dve', lambda e: e.reciprocal(out=rstd[:, 0:n], in_=rstd[:, 0:n]), reads=['a_rstd'], writes=['a_rstd'])
            y = yo[si % 2]
            yk = f"a_yo{si % 2}"
            for c in range(4):
                sc.op('dve', lambda e, c=c: e.tensor_tensor(out=xc[:, 0:n], in0=acf[:, c, 0:n], in1=mean[:, 0:n], op=ALU.subtract), reads=[ak, 'a_mean'], writes=['a_xc'])
                sc.op('dve', lambda e, c=c: e.tensor_tensor(out=xc[:, 0:n], in0=xc[:, 0:n], in1=rstd[:, 0:n], op=ALU.mult), reads=['a_xc', 'a_rstd'], writes=['a_xc'])
                sc.op('act', lambda e, c=c: e.activation(out=y[:, c, 0:n], in_=xc[:, 0:n], func=AF.Silu, bias=cnb[:, c:c + 1], scale=cng[:, c:c + 1]),
                      reads=['a_xc', 'a_cnb', 'a_cng'], writes=[yk])
            for c in range(4):
                sc.dma('sp', lambda e, c=c: e.dma_start(out=S.aT[c, :, t0 * 128: t0 * 128 + n], in_=y[:, c, 0:n]), reads=[yk], writes=['S.aT'])
        sc.flush()


def phase_fin(nc, sc, K, I, S):
    with ExitStack() as st:
        buf = K.sb(st, "fin_buf", [128, D], F32)
        sc.op('dve', lambda e: e.memset(buf[:], 0.0), writes=['fin_buf'])
        sc.dma('sp', lambda e: e.dma_start(out=I.out[0:128, :], in_=buf[:]), reads=['fin_buf'], writes=['out'])
        sc.flush()


def layernorm_tm(sc, K, x, xk, g_bc, gk, b_bc, bk, out, ok, tmp):
    st6, mv, sd = tmp['st6'], tmp['mv'], tmp['sd']
    for hfi in range(2):
        sc.op('dve', lambda e, hfi=hfi: e.bn_stats(out=st6[:, hfi * 6:(hfi + 1) * 6], in_=x[:, hfi * 512:(hfi + 1) * 512]), reads=[xk], writes=['ln_st6'])
    sc.op('dve', lambda e: e.bn_aggr(out=mv[:, 0:2], in_=st6[:, 0:12]), reads=['ln_st6'], writes=['ln_mv'])
    sc.op('act', lambda e: e.activation(out=sd[:, 0:1], in_=mv[:, 1:2], func=AF.Sqrt, bias=K.eps_ln[:, 0:1], scale=1.0), reads=['ln_mv', 'eps_ln'], writes=['ln_sd'])
    sc.op('dve', lambda e: e.reciprocal(out=sd[:, 0:1], in_=sd[:, 0:1]), reads=['ln_sd'], writes=['ln_sd'])
    sc.op('dve', lambda e: e.tensor_scalar(out=out[:], in0=x[:], scalar1=mv[:, 0:1], scalar2=sd[:, 0:1], op0=ALU.subtract, op1=ALU.mult),
          reads=[xk, 'ln_mv', 'ln_sd'], writes=[ok])
    sc.op('dve', lambda e: e.tensor_tensor(out=out[:], in0=out[:], in1=g_bc[:], op=ALU.mult), reads=[ok, gk], writes=[ok])
    sc.op('dve', lambda e: e.tensor_tensor(out=out[:], in0=out[:], in1=b_bc[:], op=ALU.add), reads=[ok, bk], writes=[ok])


def ln_tmp(K, st, pfx):
    return dict(st6=K.sb(st, pfx + "_st6", [128, 12], F32), mv=K.sb(st, pfx + "_mv", [128, 2], F32), sd=K.sb(st, pfx + "_sd", [128, 1], F32))


def store_h_and_hT(nc, sc, K, h, hk, hT, hTk, dram_h, dram_hT, ti):
    sc.dma('sp', lambda e: e.dma_start(out=dram_h[ti * 128:(ti + 1) * 128, :], in_=h[:]), reads=[hk], writes=['dram_h'])
    transpose_to(nc, sc, K, h, hk, lambda c0, nn: hT[:, c0:c0 + nn, :], hTk, 8, evac='act')
    for c0 in (0, 4):
        sc.dma('sp', lambda e, c0=c0: e.dma_start(out=dram_hT[c0:c0 + 4, :, ti * 128:(ti + 1) * 128].rearrange("c p t -> p c t"), in_=(hT[:, c0:c0 + 4, :].bitcast(F32) if hT.dtype == F32R else hT[:, c0:c0 + 4, :])),
               reads=[hTk], writes=['dram_hT'])


def phase_1b(nc, sc, K, I, S):
    with ExitStack() as st:
        sb = K.sb
        V3 = [128, 4, 128]
        wB = sb(st, "b_wB", [128, 8, 1792], F32R)
        load_weight_r(nc, sc, K, wB, 'b_wB', I.w_in, 8, 1792, col0=1024)
        wO = sb(st, "b_wO", [128, 8, 1024], F32R)
        load_weight_r(nc, sc, K, wO, 'b_wO', I.w_out, 8, 1024)
        wa2 = sb(st, "b_wa2", [128, 512], F32R)
        sc.dma('pool', lambda e: e.dma_start(out=wa2[0:64, :], in_=I.w2), writes=['b_wa2'])
        sc.dma('pool', lambda e: e.dma_start(out=wa2[64:128, :], in_=I.a2), writes=['b_wa2'])
        g2r = sb(st, "b_g2r", [128, 512], F32R)
        sc.dma('pool', lambda e: e.dma_start(out=g2r[:], in_=I.g2), writes=['b_g2r'])
        mu = load_cols(nc, sc, K, st, "b_mu", I.mu, 1792)
        w0c = load_cols(nc, sc, K, st, "b_w0c", I.w0, 512)
        a0c = load_cols(nc, sc, K, st, "b_a0c", I.a0, 512)
        kkc = load_cols(nc, sc, K, st, "b_kkc", I.k_k, 512)
        kac = load_cols(nc, sc, K, st, "b_kac", I.k_a, 512)
        rkc = load_cols(nc, sc, K, st, "b_rkc", I.r_k, 512)
        omka = sb(st, "b_omka", [128, 4], F32)
        sc.op('dve', lambda e: e.tensor_scalar(out=omka[:], in0=kac[:], scalar1=-1.0, scalar2=1.0, op0=ALU.mult, op1=ALU.add), reads=['b_kac'], writes=['b_omka'])
        lnxg = load_bcast(nc, sc, K, st, "b_lnxg", I.lnx_g, 512)
        lnxb = load_bcast(nc, sc, K, st, "b_lnxb", I.lnx_b, 512)
        ln1g = load_bcast(nc, sc, K, st, "b_ln1g", I.ln1_g, D)
        ln1b = load_bcast(nc, sc, K, st, "b_ln1b", I.ln1_b, D)
        indf = sb(st, "b_indf", [128, 4, 8], F32)
        ind = sb(st, "b_ind", [128, 4, 8], F32R)
        sc.op('dve', lambda e: e.memset(indf[:], 0.0), writes=['b_indf'])
        for c in range(4):
            sc.op('dve', lambda e, c=c: e.memset(indf[0:64, c, 2 * c:2 * c + 1], 1.0), reads=['b_indf'], writes=['b_indf'])
            sc.op('dve', lambda e, c=c: e.memset(indf[64:128, c, 2 * c + 1:2 * c + 2], 1.0), reads=['b_indf'], writes=['b_indf'])
        sc.op('dve', lambda e: e.tensor_copy(out=ind[:], in_=indf[:]), reads=['b_indf'], writes=['b_ind'])
        ST = [sb(st, f"b_ST{i}", V3, F32R) for i in range(2)]
        sc.op('dve', lambda e: e.tensor_copy(out=ST[0][:], in_=K.zero_f[:, 0:512].rearrange("p (c t) -> p c t", t=128)), reads=['zero_f'], writes=['b_ST0'])
        zT = sb(st, "b_zT", [128, 14, 129], F32)
        sc.op('dve', lambda e: e.memset(zT[:], 0.0), writes=['b_zT'])
        hb = [sb(st, "b_h0", [128, D], F32)]
        hT = sb(st, "b_hT", [128, 8, 128], F32R)
        zl = sb(st, "b_zl", [128, 14, 128], F32)
        lo = sb(st, "b_lo", [128, 128], F32R)
        sgl = sb(st, "b_sgl", [128, 128], F32R)
        g_tm = sb(st, "b_gtm", [128, 512], F32)
        sgw = sb(st, "b_sgw", V3, F32)
        a_t = sb(st, "b_a", V3, F32)
        Lc = sb(st, "b_Lc", V3, F32)
        Wt = sb(st, "b_Wt", V3, F32)
        Winv = sb(st, "b_Winv", V3, F32)
        Wp = sb(st, "b_Wp", V3, F32)
        kk = sb(st, "b_kk", V3, F32)
        sqk = sb(st, "b_sqk", V3, F32R)
        nrm = Lc
        t1 = sb(st, "b_t1", V3, F32)
        kmod = sgw
        kt = sb(st, "b_kt", V3, F32R)
        ab = sb(st, "b_ab", V3, F32R)
        kb = sb(st, "b_kb", V3, F32R)
        rt = sb(st, "b_rt", V3, F32R)
        rkr = sb(st, "b_rkr", V3, F32R)
        bs = sb(st, "b_bs", [128, 8], F32)
        Vt = sb(st, "b_V", [128, 512], F32R)
        ab_tm = sb(st, "b_abtm", [128, 512], F32R)
        kb_tm = sb(st, "b_kbtm", [128, 512], F32R)
        H3 = [128, 8, 128]
        Pm = [sb(st, f"b_P{i}", H3, F32R) for i in range(2)]
        Qm = [sb(st, f"b_Q{i}", H3, F32R) for i in range(2)]
        Xs = sb(st, "b_X0", H3, F32R)
        Xm = [Xs, Xs]
        BT, ArT, BrT = Pm[0], Pm[1], Qm[0]
        Zs = Qm[1][:, 0:4, :].rearrange("p c t -> p (c t)")
        U = Qm[1][:, 4:8, :].rearrange("p c t -> p (c t)")
        ysb = sb(st, "b_ysb", [128, 512], F32)
        s1 = sb(st, "b_s1", [128, 8], F32)
        s2 = sb(st, "b_s2", [128, 8], F32)
        mixT = hT
        res = zl[:, 0:8, :].rearrange("p c t -> p (c t)")
        lt = ln_tmp(K, st, "b_ln")

        def bc4(ap):
            return ap.unsqueeze(2).broadcast_to(V3)

        def mask4(m):
            return m[:].unsqueeze(1).broadcast_to(V3)

        def v4(ap):
            return ap.rearrange("p (c t) -> p c t", t=128)

        def v8(ap):
            return ap.rearrange("p (h i) -> p h i", i=64)

        for ti in range(DBG["nt"]):
            h = hb[0]
            hk = "b_h0"
            load_h_tile(nc, sc, K, I, h, hk, ti)
            transpose_to(nc, sc, K, h, hk, lambda c0, nn: hT[:, c0:c0 + nn, :], 'b_hT', 8, evac='act')
            for c0 in range(0, 14, 4):
                nn = min(4, 14 - c0)
                b = K.nb()
                for j in range(nn):
                    for k in range(8):
                        sc.op('pe', lambda e, j=j, k=k: e.matmul(K.ps[b][:, j * 128:(j + 1) * 128], wB[:, k, (c0 + j) * 128:(c0 + j + 1) * 128], hT[:, k, :], start=(k == 0), stop=(k == 7)),
                              reads=['b_wB', 'b_hT'], writes=[('ps', b)])
                sc.op('act', lambda e: e.copy(out=zT[:, c0:c0 + nn, 1:129], in_=K.ps[b][:, 0:nn * 128].rearrange("p (c t) -> p c t", t=128)), reads=[('ps', b)], writes=['b_zT'])
            sc.op('dve', lambda e: e.tensor_tensor(out=zl[:], in0=zT[:, :, 0:128], in1=zT[:, :, 1:129], op=ALU.subtract), reads=['b_zT'], writes=['b_zl'])
            sc.op('dve', lambda e: e.tensor_tensor(out=zl[:], in0=zl[:], in1=mu[:].unsqueeze(2).broadcast_to([128, 14, 128]), op=ALU.mult), reads=['b_zl', 'b_mu'], writes=['b_zl'])
            sc.op('dve', lambda e: e.tensor_tensor(out=zl[:], in0=zl[:], in1=zT[:, :, 1:129], op=ALU.add), reads=['b_zl', 'b_zT'], writes=['b_zl'])
            sc.op('act', lambda e: e.copy(out=zT[:, :, 0:1], in_=zT[:, :, 128:129]), reads=['b_zT'], writes=['b_zT'])
            if DBG["stage"] <= 1:
                continue
            r_ = zl[:, 0:4, :]
            k_ = zl[:, 4:8, :]
            sc.op('act', lambda e: e.activation(out=lo[0:64, :], in_=zl[0:64, 12, :], func=AF.Tanh), reads=['b_zl'], writes=['b_lo'])
            sc.op('act', lambda e: e.copy(out=lo[64:128, :], in_=zl[64:128, 12, :]), reads=['b_zl'], writes=['b_lo'])
            sc.op('act', lambda e: e.activation(out=sgl[:], in_=zl[:, 13, :], func=AF.Sigmoid), reads=['b_zl'], writes=['b_sgl'])
            bw = K.nb()
            for c in range(4):
                sc.op('pe', lambda e, c=c: e.matmul(K.ps[bw][:, c * 128:(c + 1) * 128], wa2[0:64, c * 128:(c + 1) * 128], lo[0:64, :], start=True, stop=True), reads=['b_wa2', 'b_lo'], writes=[('ps', bw)])
            for c in range(4):
                sc.op('act', lambda e, c=c: e.activation(out=sgw[:, c, :], in_=K.ps[bw][:, c * 128:(c + 1) * 128], func=AF.Sigmoid, bias=w0c[:, c:c + 1], scale=1.0), reads=[('ps', bw), 'b_w0c'], writes=['b_sgw'])
            ba = K.nb()
            for c in range(4):
                sc.op('pe', lambda e, c=c: e.matmul(K.ps[ba][:, c * 128:(c + 1) * 128], wa2[64:128, c * 128:(c + 1) * 128], lo[64:128, :], start=True, stop=True), reads=['b_wa2', 'b_lo'], writes=[('ps', ba)])
            for c in range(4):
                sc.op('act', lambda e, c=c: e.activation(out=a_t[:, c, :], in_=K.ps[ba][:, c * 128:(c + 1) * 128], func=AF.Sigmoid, bias=a0c[:, c:c + 1], scale=1.0), reads=[('ps', ba), 'b_a0c'], writes=['b_a'])
            bg = K.nb()
            sc.op('pe', lambda e: e.matmul(K.ps[bg][:, 0:512], sgl[:], g2r[:], start=True, stop=True), reads=['b_sgl', 'b_g2r'], writes=[('ps', bg)])
            sc.op('act', lambda e: e.copy(out=g_tm[:], in_=K.ps[bg][:, 0:512]), reads=[('ps', bg)], writes=['b_gtm'])
            for c in range(4):
                sc.op('dve', lambda e, c=c: e.tensor_tensor_scan(out=Lc[:, c, :], data0=K.ones_f[:, 0:128], data1=sgw[:, c, :], initial=0.0, op0=ALU.mult, op1=ALU.add), reads=['b_sgw', 'ones_f'], writes=['b_Lc'])
            sc.op('dve', lambda e: e.tensor_tensor(out=t1[:], in0=Lc[:], in1=sgw[:], op=ALU.subtract), reads=['b_Lc', 'b_sgw'], writes=['b_t1'])
            sc.op('act', lambda e: e.activation(out=Wt[:], in_=Lc[:], func=AF.Exp, scale=-DECAY_SCALE), reads=['b_Lc'], writes=['b_Wt'])
            sc.op('act', lambda e: e.activation(out=Winv[:], in_=Lc[:], func=AF.Exp, scale=DECAY_SCALE), reads=['b_Lc'], writes=['b_Winv'])
            sc.op('act', lambda e: e.activation(out=Wp[:], in_=t1[:], func=AF.Exp, scale=-DECAY_SCALE), reads=['b_t1'], writes=['b_Wp'])
            sc.op('dve', lambda e: e.tensor_tensor(out=kk[:], in0=k_, in1=bc4(kkc[:]), op=ALU.mult), reads=['b_zl', 'b_kkc'], writes=['b_kk'])
            sc.op('act', lambda e: e.activation(out=sqk[:], in_=kk[:], func=AF.Square), reads=['b_kk'], writes=['b_sqk'])
            bsq = K.nb()
            for c in range(4):
                sc.op('pe', lambda e, c=c: e.matmul(K.ps[bsq][:, c * 128:(c + 1) * 128], K.blk1r[:], sqk[:, c, :], start=True, stop=True), reads=['blk1r', 'b_sqk'], writes=[('ps', bsq)])
            sc.op('act', lambda e: e.activation(out=nrm[:], in_=v4(K.ps[bsq][:, 0:512]), func=AF.Sqrt), reads=[('ps', bsq)], writes=['b_Lc'])
            sc.op('dve', lambda e: e.tensor_scalar_max(out=nrm[:], in0=nrm[:], scalar1=1e-12), reads=['b_Lc'], writes=['b_Lc'])
            sc.op('dve', lambda e: e.reciprocal(out=nrm[:], in_=nrm[:]), reads=['b_Lc'], writes=['b_Lc'])
            sc.op('dve', lambda e: e.tensor_tensor(out=kk[:], in0=kk[:], in1=nrm[:], op=ALU.mult), reads=['b_kk', 'b_Lc'], writes=['b_kk'])
            sc.op('dve', lambda e: e.tensor_tensor(out=t1[:], in0=a_t[:], in1=bc4(kac[:]), op=ALU.mult), reads=['b_a', 'b_kac'], writes=['b_t1'])
            sc.op('dve', lambda e: e.tensor_tensor(out=t1[:], in0=t1[:], in1=bc4(omka[:]), op=ALU.add), reads=['b_t1', 'b_omka'], writes=['b_t1'])
            sc.op('dve', lambda e: e.tensor_tensor(out=kmod[:], in0=k_, in1=t1[:], op=ALU.mult), reads=['b_zl', 'b_t1'], writes=['b_sgw'])
            sc.op('dve', lambda e: e.tensor_tensor(out=kt[:], in0=kk[:], in1=Wp[:], op=ALU.mult), reads=['b_kk', 'b_Wp'], writes=['b_kt'])
            sc.op('dve', lambda e: e.tensor_tensor(out=t1[:], in0=kk[:], in1=a_t[:], op=ALU.mult), reads=['b_kk', 'b_a', 'b_t1'], writes=['b_t1'])
            sc.op('dve', lambda e: e.tensor_tensor(out=ab[:], in0=t1[:], in1=Winv[:], op=ALU.mult), reads=['b_t1', 'b_Winv'], writes=['b_ab'])
            sc.op('dve', lambda e: e.tensor_tensor(out=kb[:], in0=kmod[:], in1=Winv[:], op=ALU.mult), reads=['b_sgw', 'b_Winv'], writes=['b_kb'])
            sc.op('dve', lambda e: e.tensor_tensor(out=rt[:], in0=r_, in1=Wt[:], op=ALU.mult), reads=['b_zl', 'b_Wt'], writes=['b_rt'])
            sc.op('dve', lambda e: e.tensor_tensor(out=t1[:], in0=r_, in1=kmod[:], op=ALU.mult), reads=['b_zl', 'b_sgw', 'b_t1'], writes=['b_t1'])
            sc.op('dve', lambda e: e.tensor_tensor(out=rkr[:], in0=t1[:], in1=bc4(rkc[:]), op=ALU.mult), reads=['b_t1', 'b_rkc'], writes=['b_rkr'])
            bb = K.nb()
            for c in range(4):
                sc.op('pe', lambda e, c=c: e.matmul(K.ps[bb][:, 0:8], rkr[:, c, :], ind[:, c, :], start=(c == 0), stop=(c == 3)), reads=['b_rkr', 'b_ind'], writes=[('ps', bb)])
            sc.op('act', lambda e: e.copy(out=bs[:], in_=K.ps[bb][:, 0:8]), reads=[('ps', bb)], writes=['b_bs'])
            if DBG["stage"] <= 2:
                continue
            for (src_fn, dst, dk, rk_) in ((lambda c: zl[:, 8 + c, :], Vt, 'b_V', 'b_zl'),
                                          (lambda c: ab[:, c, :], ab_tm, 'b_abtm', 'b_ab'),
                                          (lambda c: kb[:, c, :], kb_tm, 'b_kbtm', 'b_kb'))[DBG.get('sub0', 0):DBG.get('sub1', 3)]:
                b = K.nb()
                for c in range(4):
                    if rk_ == 'b_zl':
                        sc.op('pe', lambda e, c=c: e.transpose(K.ps[b][:, c * 128:(c + 1) * 128], src_fn(c), K.ident[:]), reads=[rk_, 'ident'], writes=[('ps', b)])
                    else:
                        sc.op('pe', lambda e, c=c: e.transpose(K.ps[b][:, c * 128:(c + 1) * 128].bitcast(F32R), src_fn(c), K.identr[:]), reads=[rk_, 'identr'], writes=[('ps', b)])
                sc.op('act', lambda e: e.copy(out=dst[:], in_=K.ps[b][:, 0:512]), reads=[('ps', b)], writes=[dk])
            if DBG["stage"] <= 3:
                continue
            def headmm(L, Lk, Rr, Rk, mask, dst, dk):
                b2 = K.nb2()
                for par in range(2):
                    p0 = 64 * par
                    for c in range(4):
                        sc.op('pe', lambda e, c=c: e.matmul(K.ps[b2 + par][:, c * 128:(c + 1) * 128], L[p0:p0 + 64, c, :], Rr[p0:p0 + 64, c, :], start=True, stop=True),
                              reads=[Lk, Rk], writes=[('ps', b2 + par)])
                sc.op('dve', lambda e: e.tensor_tensor(out=dst[:].rearrange("p (c q) t -> p q c t", q=2),
                                                       in0=K.psall[:, b2 * 512:(b2 + 2) * 512].rearrange("p (q c t) -> p q c t", q=2, t=128),
                                                       in1=mask[:].unsqueeze(1).unsqueeze(1).broadcast_to([128, 2, 4, 128]), op=ALU.mult),
                      reads=[('ps', b2), ('ps', b2 + 1)], writes=[dk])
            headmm(kt, 'b_kt', ab, 'b_ab', K.m_lt, Pm[0], 'b_P0')
            headmm(ab, 'b_ab', kt, 'b_kt', K.m_gt, Qm[0], 'b_Q0')
            if DBG["stage"] <= 4:
                continue
            sc.op('dve', lambda e: e.tensor_tensor(out=Xm[0][:], in0=K.ident[:].unsqueeze(1).broadcast_to(H3), in1=Qm[0][:].bitcast(F32), op=ALU.subtract), reads=['ident', 'b_Q0'], writes=['b_X0'])
            cur = 0
            for lev in range(1, 7):
                nxt = 1 - cur
                Pc, Qc, Xc = Pm[cur], Qm[cur], Xm[cur]
                Pn, Qn, Xn = Pm[nxt], Qm[nxt], Xm[nxt]
                def ps8(b2):
                    return K.psall[:, b2 * 512:(b2 + 2) * 512].rearrange("p (h t) -> p h t", t=128)
                bP = K.nb2()
                for hd in range(8):
                    sc.op('pe', lambda e, hd=hd: e.matmul(K.ps[bP + hd // 4][:, (hd % 4) * 128:(hd % 4 + 1) * 128], Qc[:, hd, :], Pc[:, hd, :], start=True, stop=True),
                          reads=[f'b_Q{cur}', f'b_P{cur}'], writes=[('ps', bP + hd // 4)])
                if lev < 6:
                    bQ = K.nb2()
                    for hd in range(8):
                        sc.op('pe', lambda e, hd=hd: e.matmul(K.ps[bQ + hd // 4][:, (hd % 4) * 128:(hd % 4 + 1) * 128], Pc[:, hd, :], Qc[:, hd, :], start=True, stop=True),
                              reads=[f'b_Q{cur}', f'b_P{cur}'], writes=[('ps', bQ + hd // 4)])
                sc.op('act', lambda e: e.copy(out=Pn[:], in_=ps8(bP)), reads=[('ps', bP), ('ps', bP + 1)], writes=[f'b_P{nxt}'])
                if lev < 6:
                    sc.op('dve', lambda e: e.tensor_copy(out=Qn[:], in_=ps8(bQ)), reads=[('ps', bQ), ('ps', bQ + 1)], writes=[f'b_Q{nxt}'])
                bX = K.nb2()
                for hd in range(8):
                    sc.op('pe', lambda e, hd=hd: e.matmul(K.ps[bX + hd // 4][:, (hd % 4) * 128:(hd % 4 + 1) * 128], Pn[:, hd, :], Xc[:, hd, :], start=True, stop=True),
                          reads=[f'b_P{nxt}', 'b_X0'], writes=[('ps', bX + hd // 4)])
                sc.op('dve', lambda e: e.tensor_tensor(out=Xn[:], in0=ps8(bX), in1=Xc[:].bitcast(F32), op=ALU.add),
                      reads=[('ps', bX), ('ps', bX + 1), 'b_X0'], writes=['b_X0'])
                cur = nxt
            if DBG["stage"] <= 5:
                continue
            X = Xm[cur]
            headmm(kb, 'b_kb', kt, 'b_kt', K.m_gt, BT, 'b_P0')
            headmm(ab, 'b_ab', rt, 'b_rt', K.m_ge, ArT, 'b_P1')
            headmm(kb, 'b_kb', rt, 'b_rt', K.m_ge, BrT, 'b_Q0')
            Xk = 'b_X0'
            S0 = ST[ti % 2]
            S0k = f'b_ST{ti % 2}'
            S1 = ST[1 - ti % 2]
            S1k = f'b_ST{1 - ti % 2}'
            def q4(ap, par):
                return ap.rearrange("p (c q i) -> p c q i", q=2, i=64)[:, :, par, :]
            def ps_q(b2):
                return K.psall[:, b2 * 512:(b2 + 2) * 512].rearrange("p (q x) -> p q x", q=2)[:, :, 0:256].rearrange("p q (c i) -> p q c i", i=64)

            def sb_q(ap):
                return ap.rearrange("p (c q i) -> p q c i", q=2, i=64)
            bz2 = K.nb2()
            for par in range(2):
                bz = bz2 + par
                p0 = 64 * par
                for c in range(4):
                    hd = 2 * c + par
                    sc.op('pe', lambda e, hd=hd, c=c: e.matmul(K.ps[bz][:, c * 64:(c + 1) * 64], kt[p0:p0 + 64, c, :], S0[p0:p0 + 64, c, p0:p0 + 64], start=True, stop=False),
                          reads=['b_kt', S0k], writes=[('ps', bz)])
                    sc.op('pe', lambda e, hd=hd, c=c: e.matmul(K.ps[bz][:, c * 64:(c + 1) * 64], BT[:, hd, :], Vt[:, hd * 64:(hd + 1) * 64], start=False, stop=True),
                          reads=['b_P0', 'b_V'], writes=[('ps', bz)])
            sc.op('act', lambda e: e.mul(out=sb_q(Zs), in_=ps_q(bz2), mul=-1.0), reads=[('ps', bz2), ('ps', bz2 + 1)], writes=['b_Q1'])
            bu = K.nb()
            for hd in range(8):
                sc.op('pe', lambda e, hd=hd: e.matmul(K.ps[bu][:, hd * 64:(hd + 1) * 64], X[:, hd, :], Zs[:, hd * 64:(hd + 1) * 64], start=True, stop=True),
                      reads=[Xk, 'b_Q1'], writes=[('ps', bu)])
            sc.op('act', lambda e: e.copy(out=U, in_=K.ps[bu][:, 0:512]), reads=[('ps', bu)], writes=['b_Q1'])
            by2 = K.nb2()
            for par in range(2):
                by = by2 + par
                p0 = 64 * par
                for c in range(4):
                    hd = 2 * c + par
                    sc.op('pe', lambda e, hd=hd, c=c: e.matmul(K.ps[by][:, c * 64:(c + 1) * 64], rt[p0:p0 + 64, c, :], S0[p0:p0 + 64, c, p0:p0 + 64], start=True, stop=False),
                          reads=['b_rt', S0k], writes=[('ps', by)])
                    sc.op('pe', lambda e, hd=hd, c=c: e.matmul(K.ps[by][:, c * 64:(c + 1) * 64], ArT[:, hd, :], U[:, hd * 64:(hd + 1) * 64], start=False, stop=False),
                          reads=['b_P1', 'b_Q1'], writes=[('ps', by)])
                    sc.op('pe', lambda e, hd=hd, c=c: e.matmul(K.ps[by][:, c * 64:(c + 1) * 64], BrT[:, hd, :], Vt[:, hd * 64:(hd + 1) * 64], start=False, stop=True),
                          reads=['b_Q0', 'b_V'], writes=[('ps', by)])
            sc.op('act', lambda e: e.copy(out=sb_q(ysb[:]), in_=ps_q(by2)), reads=[('ps', by2), ('ps', by2 + 1)], writes=['b_ysb'])
            bs2 = K.nb()
            for c in range(4):
                cs = slice(c * 128, (c + 1) * 128)
                sc.op('pe', lambda e, cs=cs: e.matmul(K.ps[bs2][:, cs], ab_tm[:, cs], U[:, cs], start=True, stop=False), reads=['b_abtm', 'b_Q1'], writes=[('ps', bs2)])
                sc.op('pe', lambda e, cs=cs: e.matmul(K.ps[bs2][:, cs], kb_tm[:, cs], Vt[:, cs], start=False, stop=False), reads=['b_kbtm', 'b_V'], writes=[('ps', bs2)])
                sc.op('pe', lambda e, cs=cs, c=c: e.matmul(K.ps[bs2][:, cs], K.identr[:], S0[:, c, :], start=False, stop=True), reads=['identr', S0k], writes=[('ps', bs2)])
            sc.op('dve', lambda e: e.tensor_tensor(out=S1[:], in0=v4(K.ps[bs2][:, 0:512]), in1=Wt[:, :, 127:128].broadcast_to(V3), op=ALU.mult), reads=[('ps', bs2), 'b_Wt'], writes=[S1k])
            if DBG["stage"] <= 6:
                continue
            sc.op('dve', lambda e: e.tensor_reduce(out=s1[:], in_=v8(ysb[:]), axis=AX.X, op=ALU.add), reads=['b_ysb'], writes=['b_s1'])
            sc.op('dve', lambda e: e.tensor_single_scalar(out=s1[:], in_=s1[:], scalar=1.0 / 64, op=ALU.mult), reads=['b_s1'], writes=['b_s1'])
            sc.op('dve', lambda e: e.tensor_tensor(out=v8(ysb[:]), in0=v8(ysb[:]), in1=s1[:].unsqueeze(2).broadcast_to([128, 8, 64]), op=ALU.subtract), reads=['b_ysb', 'b_s1'], writes=['b_ysb'])
            sc.op('act', lambda e: e.activation(out=res[:, 0:512], in_=ysb[:], func=AF.Square), reads=['b_ysb'], writes=['b_zl'])
            sc.op('dve', lambda e: e.tensor_reduce(out=s2[:], in_=v8(res[:, 0:512]), axis=AX.X, op=ALU.add), reads=['b_zl'], writes=['b_s2'])
            sc.op('act', lambda e: e.activation(out=s2[:], in_=s2[:], func=AF.Sqrt, bias=K.eps_lnx[:, 0:1], scale=1.0 / 64), reads=['b_s2', 'eps_lnx'], writes=['b_s2'])
            sc.op('dve', lambda e: e.reciprocal(out=s2[:], in_=s2[:]), reads=['b_s2'], writes=['b_s2'])
            sc.op('dve', lambda e: e.tensor_tensor(out=v8(ysb[:]), in0=v8(ysb[:]), in1=s2[:].unsqueeze(2).broadcast_to([128, 8, 64]), op=ALU.mult), reads=['b_ysb', 'b_s2'], writes=['b_ysb'])
            sc.op('dve', lambda e: e.tensor_tensor(out=ysb[:], in0=ysb[:], in1=lnxg[:], op=ALU.mult), reads=['b_ysb', 'b_lnxg'], writes=['b_ysb'])
            sc.op('dve', lambda e: e.tensor_tensor(out=ysb[:], in0=ysb[:], in1=lnxb[:], op=ALU.add), reads=['b_ysb', 'b_lnxb'], writes=['b_ysb'])
            sc.op('dve', lambda e: e.tensor_tensor(out=v8(res[:, 512:1024]), in0=v8(Vt[:].bitcast(F32)), in1=bs[:].unsqueeze(2).broadcast_to([128, 8, 64]), op=ALU.mult), reads=['b_V', 'b_bs', 'b_zl'], writes=['b_zl'])
            sc.op('dve', lambda e: e.tensor_tensor(out=ysb[:], in0=ysb[:], in1=res[:, 512:1024], op=ALU.add), reads=['b_ysb', 'b_zl'], writes=['b_ysb'])
            sc.op('dve', lambda e: e.tensor_tensor(out=ysb[:], in0=ysb[:], in1=g_tm[:], op=ALU.mult), reads=['b_ysb', 'b_gtm'], writes=['b_ysb'])
            if DBG["stage"] <= 7:
                continue
            sc.dma('pool', lambda e: e.dma_start(out=mixT[:, 0:4, :], in_=S.aT[:, :, ti * 128:(ti + 1) * 128].rearrange("c p t -> p c t")), writes=['b_hT'])
            transpose_to(nc, sc, K, ysb, 'b_ysb', lambda c0, nn: mixT[:, 4 + c0:4 + c0 + nn, :], 'b_hT', 4, evac='act')
            for hf in range(2):
                b = K.nb()
                for k in range(8):
                    sc.op('pe', lambda e, k=k: e.matmul(K.ps[b][:, 0:512], mixT[:, k, :], wO[:, k, hf * 512:(hf + 1) * 512], start=(k == 0), stop=(k == 7)),
                          reads=['b_hT', 'b_hT', 'b_wO'], writes=[('ps', b)])
                sc.op('dve', lambda e: e.scalar_tensor_tensor(out=res[:, hf * 512:(hf + 1) * 512], in0=h[:, hf * 512:(hf + 1) * 512], scalar=ALPHA, in1=K.ps[b][:, 0:512], op0=ALU.mult, op1=ALU.add),
                      reads=[hk, ('ps', b)], writes=['b_zl'])
            if DBG["stage"] <= 8:
                continue
            layernorm_tm(sc, K, res, 'b_zl', ln1g, 'b_ln1g', ln1b, 'b_ln1b', h, hk, lt)
            store_h_and_hT(nc, sc, K, h, hk, hT, 'b_hT', S.h1, S.h1T, ti)
        sc.flush()


def ffn_generic(nc, sc, K, pfx, hT_d, h_d, experts, FF, gates_d, ln_g, ln_b, out_fn, FP=2):
    nfg = FF // (128 * FP)
    groups = [(0, 9), (9, 8), (17, 8), (25, 8)]
    with ExitStack() as st:
        sb = K.sb
        GM = 9 * 128
        hT = sb(st, pfx + "hT", [128, 8, GM], F32R)
        yac = sb(st, pfx + "yac", [128, 9, D], F32)
        actT = sb(st, pfx + "actT", [128, FP, GM], F32R)
        sg = sb(st, pfx + "sg", [128, 512], F32)
        wg = [sb(st, pfx + f"wg{i}", [128, 8, FP * 128], F32R) for i in range(2)]
        wu = [sb(st, pfx + f"wu{i}", [128, 8, FP * 128], F32R) for i in range(2)]
        wd = [sb(st, pfx + f"wd{i}", [128, FP, D], F32R) for i in range(2)]
        gbc = load_bcast(nc, sc, K, st, pfx + "lng", ln_g, D)
        bbc = load_bcast(nc, sc, K, st, pfx + "lnb", ln_b, D)
        gt = sb(st, pfx + "gt", [128, 9, NEXP], F32)
        hoT = sb(st, pfx + "hoT", [128, 8, 128], F32R) if gates_d is None else None
        lt = ln_tmp(K, st, pfx + "ln")
        it = 0
        for (t0, nt) in groups[:DBG.get('fng', 4)]:
            n = nt * 128
            for k in range(8):
                sc.dma('pool', lambda e, k=k: e.dma_start(out=hT[:, k, 0:n], in_=hT_d[k, :, t0 * 128:t0 * 128 + n]), writes=[pfx + 'hT'])
            sc.dma('sp', lambda e: e.dma_start(out=yac[:, 0:nt, :], in_=h_d[t0 * 128:t0 * 128 + n, :].rearrange("(j p) d -> p j d", p=128)), writes=[pfx + 'yac'])
            sc.op('act', lambda e: e.mul(out=yac[:, 0:nt, :], in_=yac[:, 0:nt, :], mul=ALPHA), reads=[pfx + 'yac'], writes=[pfx + 'yac'])
            if gates_d is not None:
                sc.dma('sp', lambda e: e.dma_start(out=gt[:, 0:nt, :], in_=gates_d[t0 * 128:t0 * 128 + n, :].rearrange("(j p) d -> p j d", p=128)), writes=[pfx + 'gt'])
            spans = [(0, 3), (3, 3), (6, 3)] if nt == 9 else [(0, 4), (4, 4)]
            if DBG.get('fst', 99) <= 1:
                continue
            for ei, (Wg, Wu, Wd) in enumerate(experts):
                for fg in range(nfg):
                    bi = it % 2
                    it += 1
                    f0 = fg * FP * 128
                    sc.dma('pool', lambda e: e.dma_start(out=wg[bi][:], in_=Wg[:, f0:f0 + FP * 128].rearrange("(k p) f -> p k f", p=128)), writes=[pfx + f'wg{bi}'])
                    sc.dma('pool', lambda e: e.dma_start(out=wu[bi][:], in_=Wu[:, f0:f0 + FP * 128].rearrange("(k p) f -> p k f", p=128)), writes=[pfx + f'wu{bi}'])
                    sc.dma('pool', lambda e: e.dma_start(out=wd[bi][:], in_=Wd[f0:f0 + FP * 128, :].rearrange("(c p) d -> p c d", p=128)), writes=[pfx + f'wd{bi}'])
                    for (s0, sn) in spans:
                        c0 = s0 * 128
                        m = sn * 128
                        for fc in range(FP):
                            bgp = K.nb()
                            bup = K.nb()
                            for k in range(8):
                                sc.op('pe', lambda e, k=k: e.matmul(K.ps[bgp][:, 0:m], wg[bi][:, k, fc * 128:(fc + 1) * 128], hT[:, k, c0:c0 + m], start=(k == 0), stop=(k == 7)),
                                      reads=[pfx + f'wg{bi}', pfx + 'hT'], writes=[('ps', bgp)])
                            for k in range(8):
                                sc.op('pe', lambda e, k=k: e.matmul(K.ps[bup][:, 0:m], wu[bi][:, k, fc * 128:(fc + 1) * 128], hT[:, k, c0:c0 + m], start=(k == 0), stop=(k == 7)),
                                      reads=[pfx + f'wu{bi}', pfx + 'hT'], writes=[('ps', bup)])
                            sc.op('act', lambda e: e.activation(out=sg[:, 0:m], in_=K.ps[bgp][:, 0:m], func=AF.Silu), reads=[('ps', bgp)], writes=[pfx + 'sg'])
                            sc.op('dve', lambda e: e.tensor_tensor(out=actT[:, fc, c0:c0 + m], in0=K.ps[bup][:, 0:m], in1=sg[:, 0:m], op=ALU.mult),
                                  reads=[('ps', bup), pfx + 'sg'], writes=[pfx + 'actT'])
                    for j in range(nt):
                        for hf in range(2):
                            b = K.nb()
                            for fc in range(FP):
                                sc.op('pe', lambda e, fc=fc: e.matmul(K.ps[b][:, 0:512], actT[:, fc, j * 128:(j + 1) * 128], wd[bi][:, fc, hf * 512:(hf + 1) * 512], start=(fc == 0), stop=(fc == FP - 1)),
                                      reads=[pfx + 'actT', pfx + f'wd{bi}'], writes=[('ps', b)])
                            ysl = yac[:, j, hf * 512:(hf + 1) * 512]
                            if gates_d is not None:
                                sc.op('dve', lambda e: e.scalar_tensor_tensor(out=ysl, in0=K.ps[b][:, 0:512], scalar=gt[:, j, ei:ei + 1], in1=ysl, op0=ALU.mult, op1=ALU.add),
                                      reads=[('ps', b), pfx + 'gt', pfx + 'yac'], writes=[pfx + 'yac'])
                            else:
                                sc.op('dve', lambda e: e.tensor_tensor(out=ysl, in0=K.ps[b][:, 0:512], in1=ysl, op=ALU.add), reads=[('ps', b), pfx + 'yac'], writes=[pfx + 'yac'])
            if DBG.get('fst', 99) <= 2:
                continue
            for j in range(nt):
                ho = yac[:, j, :]
                layernorm_tm(sc, K, ho, pfx + 'yac', gbc, pfx + 'lng', bbc, pfx + 'lnb', ho, pfx + 'yac', lt)
                if DBG.get('fst', 99) <= 3:
                    continue
                out_fn(t0 + j, ho, pfx + 'yac', hoT, pfx + 'hoT')
        sc.flush()


def phase_ffn0(nc, sc, K, I, S):
    def out_fn(ti, ho, hok, hoT, hoTk):
        store_h_and_hT(nc, sc, K, ho, hok, hoT, hoTk, S.h2, S.h2T, ti)
    ffn_generic(nc, sc, K, "f_", S.h1T, S.h1, [(I.f_gate, I.f_up, I.f_down)], DFF0, None, I.ln2_g, I.ln2_b, out_fn)


def phase_moe(nc, sc, K, I, S):
    def out_fn(ti, ho, hok, hoT, hoTk):
        if ti == 0:
            return
        sc.dma('sp', lambda e: e.dma_start(out=I.out[(ti - 1) * 128:ti * 128, :], in_=ho[:]), reads=[hok], writes=['out'])
    experts = [(I.e_gate[e], I.e_up[e], I.e_down[e]) for e in range(NEXP)]
    ffn_generic(nc, sc, K, "m_", S.h3T, S.h3, experts, DFFE, S.gates, I.oln2_g, I.oln2_b, out_fn, FP=4)


def phase_attn(nc, sc, K, I, S):
    with ExitStack() as st:
        sb = K.sb
        wq = sb(st, "t_wq", [128, 8, 1280], F32R)
        load_weight_r(nc, sc, K, wq, 't_wq', I.w_qkv, 8, 1280)
        wo = sb(st, "t_wo", [128, 8, 1024], F32R)
        load_weight_r(nc, sc, K, wo, 't_wo', I.w_o, 8, 1024)
        bq = load_bcast(nc, sc, K, st, "t_bq", I.b_qkv, 1280)
        bo = load_bcast(nc, sc, K, st, "t_bo", I.b_o, D)
        g1 = load_bcast(nc, sc, K, st, "t_g1", I.oln1_g, D)
        b1 = load_bcast(nc, sc, K, st, "t_b1", I.oln1_b, D)
        snk = load_bcast(nc, sc, K, st, "t_snk", I.sinks, 16)
        sc.op('act', lambda e: e.activation(out=snk[:], in_=snk[:], func=AF.Exp), reads=['t_snk'], writes=['t_snk'])
        rtr = sb(st, "t_rtr", [128, 8, NEXP], F32R)
        sc.dma('pool', lambda e: e.dma_start(out=rtr[:], in_=I.router.rearrange("(k p) e -> p k e", p=128)), writes=['t_rtr'])
        rowi = sb(st, "t_rowi", [128, 128], I32)
        rowm = sb(st, "t_rowm", [128, 128], F32)
        tmpm = sb(st, "t_tmpm", [128, 128], F32)
        sc.op('pool', lambda e: e.iota(rowi[:], pattern=[[0, 128]], base=-PADF, channel_multiplier=1), writes=['t_rowi'])
        sc.op('dve', lambda e: e.tensor_single_scalar(out=rowm[:], in_=rowi[:], scalar=0, op=ALU.is_ge), reads=['t_rowi'], writes=['t_rowm'])
        mbs = {}
        for nm, m, rm in (('cur', K.m_ge, False), ('prev', K.m_lt, False), ('cur0', K.m_ge, True), ('prev1', K.m_lt, True)):
            t = sb(st, "t_mb_" + nm, [128, 128], F32R)
            if rm:
                sc.op('dve', lambda e, m=m: e.tensor_tensor(out=tmpm[:], in0=m[:], in1=rowm[:], op=ALU.mult), reads=['t_rowm', 't_tmpm'], writes=['t_tmpm'])
                sc.op('dve', lambda e, t=t: e.tensor_scalar(out=t[:], in0=tmpm[:], scalar1=-1.0, scalar2=30000.0, op0=ALU.add, op1=ALU.mult), reads=['t_tmpm'], writes=['t_mb'])
            else:
                sc.op('dve', lambda e, t=t, m=m: e.tensor_scalar(out=t[:], in0=m[:], scalar1=-1.0, scalar2=30000.0, op0=ALU.add, op1=ALU.mult), writes=['t_mb'])
            mbs[nm] = t
        hT2 = sb(st, "t_hT", [128, 8, 128], F32R)
        h2 = sb(st, "t_h2", [128, D], F32)
        qkv = sb(st, "t_qkv", [128, 1280], F32)
        qr = sb(st, "t_qr", [128, 1152], F32)
        ta = sb(st, "t_ta", [128, 8, 32], F32)
        tb = sb(st, "t_tb", [128, 8, 32], F32)
        cs = sb(st, "t_cos", [128, 32], F32)
        sn = sb(st, "t_sin", [128, 32], F32)
        qT = sb(st, "t_qT", [128, 8, 128], F32R)
        kT = [sb(st, f"t_kT{i}", [128, 128], F32R) for i in range(2)]
        v1 = [sb(st, f"t_v{i}", [128, 2, 66], F32R) for i in range(2)]
        Ec = sb(st, "t_Ec", [128, 512], F32R)
        Ep = sb(st, "t_Ep", [128, 512], F32R)
        osb = sb(st, "t_osb", [128, 16, 66], F32)
        den = sb(st, "t_den", [128, 16], F32)
        att = sb(st, "t_att", [128, D], F32)
        attT = sb(st, "t_attT", [128, 8, 128], F32R)
        res = sb(st, "t_res", [128, D], F32)
        h3 = sb(st, "t_h3", [128, D], F32)
        h3T = sb(st, "t_h3T", [128, 8, 128], F32R)
        lg = sb(st, "t_lg", [128, 8], F32)
        m8 = sb(st, "t_m8", [128, 8], F32)
        ex = sb(st, "t_ex", [128, 8], F32)
        gsm = sb(st, "t_gsm", [128, 2], F32)
        lt = ln_tmp(K, st, "t_ln")
        for i in range(2):
            sc.op('dve', lambda e, i=i: e.tensor_copy(out=v1[i][:, :, 0:64], in_=K.zero_f[:, 0:128].rearrange("p (g d) -> p g d", g=2)), reads=['zero_f'], writes=[f't_v{i}'])
            sc.op('dve', lambda e, i=i: e.tensor_copy(out=v1[i][:, :, 64:65], in_=K.ones_f[:, 0:2].unsqueeze(2)), reads=['ones_f', f't_v{i}'], writes=[f't_v{i}'])
            sc.op('dve', lambda e, i=i: e.tensor_copy(out=v1[i][:, :, 65:66], in_=K.zero_f[:, 0:2].unsqueeze(2)), reads=['zero_f', f't_v{i}'], writes=[f't_v{i}'])
        B83 = [128, 8, 32]
        for ti in range(NT):
            cu, pv = ti % 2, 1 - ti % 2
            sc.dma('pool', lambda e: e.dma_start(out=hT2[:], in_=S.h2T[:, :, ti * 128:(ti + 1) * 128].rearrange("c p t -> p c t")), writes=['t_hT'])
            sc.dma('sp', lambda e: e.dma_start(out=h2[:], in_=S.h2[ti * 128:(ti + 1) * 128, :]), writes=['t_h2'])
            sc.dma('sp', lambda e: e.dma_start(out=cs[:], in_=I.rope_cos[ti * 128:(ti + 1) * 128, :]), writes=['t_cos'])
            sc.dma('sp', lambda e: e.dma_start(out=sn[:], in_=I.rope_sin[ti * 128:(ti + 1) * 128, :]), writes=['t_sin'])
            for (c0, cw) in ((0, 512), (512, 512), (1024, 256)):
                b = K.nb()
                for k in range(8):
                    sc.op('pe', lambda e, k=k: e.matmul(K.ps[b][:, 0:cw], hT2[:, k, :], wq[:, k, c0:c0 + cw], start=(k == 0), stop=(k == 7)), reads=['t_hT', 't_wq'], writes=[('ps', b)])
                sc.op('dve', lambda e: e.tensor_tensor(out=qkv[:, c0:c0 + cw], in0=K.ps[b][:, 0:cw], in1=bq[:, c0:c0 + cw], op=ALU.add), reads=[('ps', b), 't_bq'], writes=['t_qkv'])
            cb = cs[:].unsqueeze(1)
            sb_ = sn[:].unsqueeze(1)
            parts = []
            for g in range(2):
                vin = qkv[:, g * 512:(g + 1) * 512].rearrange("p (c two d) -> p c two d", two=2, d=32)
                vout = qr[:, 0:1024].rearrange("p (c g two d) -> p c g two d", g=2, two=2, d=32)
                parts.append((8, vin[:, :, 0, :], vin[:, :, 1, :], vout[:, :, g, 0, :], vout[:, :, g, 1, :]))
            vin = qkv[:, 1024:1152].rearrange("p (c two d) -> p c two d", two=2, d=32)
            vout = qr[:, 1024:1152].rearrange("p (c two d) -> p c two d", two=2, d=32)
            parts.append((2, vin[:, :, 0, :], vin[:, :, 1, :], vout[:, :, 0, :], vout[:, :, 1, :]))
            for (nh, x1, x2, o1, o2) in parts:
                shp = [128, nh, 32]
                cbb = cb.broadcast_to(shp)
                sbb = sb_.broadcast_to(shp)
                sc.op('dve', lambda e: e.tensor_tensor(out=ta[:, 0:nh, :], in0=x1, in1=cbb, op=ALU.mult), reads=['t_qkv', 't_cos'], writes=['t_ta'])
                sc.op('dve', lambda e: e.tensor_tensor(out=tb[:, 0:nh, :], in0=x2, in1=sbb, op=ALU.mult), reads=['t_qkv', 't_sin'], writes=['t_tb'])
                sc.op('dve', lambda e: e.tensor_tensor(out=o1, in0=ta[:, 0:nh, :], in1=tb[:, 0:nh, :], op=ALU.subtract), reads=['t_ta', 't_tb'], writes=['t_qr'])
                sc.op('dve', lambda e: e.tensor_tensor(out=ta[:, 0:nh, :], in0=x2, in1=cbb, op=ALU.mult), reads=['t_qkv', 't_cos', 't_ta'], writes=['t_ta'])
                sc.op('dve', lambda e: e.tensor_tensor(out=tb[:, 0:nh, :], in0=x1, in1=sbb, op=ALU.mult), reads=['t_qkv', 't_sin', 't_tb'], writes=['t_tb'])
                sc.op('dve', lambda e: e.tensor_tensor(out=o2, in0=ta[:, 0:nh, :], in1=tb[:, 0:nh, :], op=ALU.add), reads=['t_ta', 't_tb'], writes=['t_qr'])
            transpose_to(nc, sc, K, qr, 't_qr', lambda c0, nn: qT[:, c0:c0 + nn, :], 't_qT', 8, evac='act')
            bk = K.nb()
            sc.op('pe', lambda e: e.transpose(K.ps[bk][:, 0:128], qr[:, 1024:1152], K.ident[:]), reads=['t_qr', 'ident'], writes=[('ps', bk)])
            sc.op('act', lambda e: e.copy(out=kT[cu][:], in_=K.ps[bk][:, 0:128]), reads=[('ps', bk)], writes=[f't_kT{cu}'])
            sc.op('act', lambda e: e.copy(out=v1[cu][:, :, 0:64], in_=qkv[:, 1152:1280].rearrange("p (g d) -> p g d", g=2)), reads=['t_qkv'], writes=[f't_v{cu}'])
            mcur = mbs['cur0'] if ti == 0 else mbs['cur']
            mprev = mbs['prev1'] if ti == 1 else mbs['prev']
            for g in range(2):
                p0 = 64 * g
                for hf in range(2):
                    bc_ = K.nb()
                    for j in range(4):
                        c = hf * 4 + j
                        sc.op('pe', lambda e, j=j, c=c: e.matmul(K.ps[bc_][:, j * 128:(j + 1) * 128], kT[cu][p0:p0 + 64, :], qT[p0:p0 + 64, c, :], start=True, stop=False), reads=[f't_kT{cu}', 't_qT'], writes=[('ps', bc_)])
                        sc.op('pe', lambda e, j=j: e.matmul(K.ps[bc_][:, j * 128:(j + 1) * 128], K.identr[:], mcur[:], start=False, stop=True), reads=['identr', 't_mb'], writes=[('ps', bc_)])
                    sc.op('act', lambda e: e.activation(out=Ec[:], in_=K.ps[bc_][:, 0:512], func=AF.Exp, scale=0.125), reads=[('ps', bc_)], writes=['t_Ec'])
                    if ti > 0:
                        bp_ = K.nb()
                        for j in range(4):
                            c = hf * 4 + j
                            sc.op('pe', lambda e, j=j, c=c: e.matmul(K.ps[bp_][:, j * 128:(j + 1) * 128], kT[pv][p0:p0 + 64, :], qT[p0:p0 + 64, c, :], start=True, stop=False), reads=[f't_kT{pv}', 't_qT'], writes=[('ps', bp_)])
                            sc.op('pe', lambda e, j=j: e.matmul(K.ps[bp_][:, j * 128:(j + 1) * 128], K.identr[:], mprev[:], start=False, stop=True), reads=['identr', 't_mb'], writes=[('ps', bp_)])
                        sc.op('act', lambda e: e.activation(out=Ep[:], in_=K.ps[bp_][:, 0:512], func=AF.Exp, scale=0.125), reads=[('ps', bp_)], writes=['t_Ep'])
                    bo_ = K.nb()
                    for j in range(4):
                        osl = K.ps[bo_][:, j * 66:(j + 1) * 66]
                        if ti > 0:
                            sc.op('pe', lambda e, j=j, osl=osl: e.matmul(osl, Ep[:, j * 128:(j + 1) * 128], v1[pv][:, g, :], start=True, stop=False), reads=['t_Ep', f't_v{pv}'], writes=[('ps', bo_)])
                        sc.op('pe', lambda e, j=j, osl=osl: e.matmul(osl, Ec[:, j * 128:(j + 1) * 128], v1[cu][:, g, :], start=(ti == 0), stop=True), reads=['t_Ec', f't_v{cu}'], writes=[('ps', bo_)])
                    h0 = 8 * g + 4 * hf
                    sc.op('act', lambda e: e.copy(out=osb[:, h0:h0 + 4, :], in_=K.ps[bo_][:, 0:264].rearrange("p (h d) -> p h d", d=66)), reads=[('ps', bo_)], writes=['t_osb'])
            sc.op('dve', lambda e: e.tensor_tensor(out=den[:].unsqueeze(2), in0=osb[:, :, 64:65], in1=snk[:].unsqueeze(2), op=ALU.add), reads=['t_osb', 't_snk'], writes=['t_den'])
            sc.op('dve', lambda e: e.reciprocal(out=den[:], in_=den[:]), reads=['t_den'], writes=['t_den'])
            sc.op('dve', lambda e: e.tensor_tensor(out=att[:].rearrange("p (h d) -> p h d", d=64), in0=osb[:, :, 0:64], in1=den[:].unsqueeze(2).broadcast_to([128, 16, 64]), op=ALU.mult), reads=['t_osb', 't_den'], writes=['t_att'])
            transpose_to(nc, sc, K, att, 't_att', lambda c0, nn: attT[:, c0:c0 + nn, :], 't_attT', 8, evac='act')
            for hf in range(2):
                b = K.nb()
                for k in range(8):
                    sc.op('pe', lambda e, k=k: e.matmul(K.ps[b][:, 0:512], attT[:, k, :], wo[:, k, hf * 512:(hf + 1) * 512], start=(k == 0), stop=(k == 7)), reads=['t_attT', 't_wo'], writes=[('ps', b)])
                sc.op('dve', lambda e: e.scalar_tensor_tensor(out=res[:, hf * 512:(hf + 1) * 512], in0=h2[:, hf * 512:(hf + 1) * 512], scalar=ALPHA, in1=K.ps[b][:, 0:512], op0=ALU.mult, op1=ALU.add), reads=['t_h2', ('ps', b)], writes=['t_res'])
            sc.op('dve', lambda e: e.tensor_tensor(out=res[:], in0=res[:], in1=bo[:], op=ALU.add), reads=['t_res', 't_bo'], writes=['t_res'])
            layernorm_tm(sc, K, res, 't_res', g1, 't_g1', b1, 't_b1', h3, 't_h3', lt)
            store_h_and_hT(nc, sc, K, h3, 't_h3', h3T, 't_h3T', S.h3, S.h3T, ti)
            br = K.nb()
            for k in range(8):
                sc.op('pe', lambda e, k=k: e.matmul(K.ps[br][:, 0:8], h3T[:, k, :], rtr[:, k, :], start=(k == 0), stop=(k == 7)), reads=['t_h3T', 't_rtr'], writes=[('ps', br)])
            sc.op('act', lambda e: e.copy(out=lg[:], in_=K.ps[br][:, 0:8]), reads=[('ps', br)], writes=['t_lg'])
            sc.op('dve', lambda e: e.max(out=m8[:], in_=lg[:]), reads=['t_lg'], writes=['t_m8'])
            sc.op('dve', lambda e: e.tensor_single_scalar(out=gsm[:, 0:1], in_=m8[:, 0:1], scalar=-1.0, op=ALU.mult), reads=['t_m8'], writes=['t_gsm'])
            sc.op('act', lambda e: e.activation(out=ex[:], in_=lg[:], func=AF.Exp, bias=gsm[:, 0:1], scale=1.0), reads=['t_lg', 't_gsm'], writes=['t_ex'])
            sc.op('dve', lambda e: e.tensor_scalar(out=lg[:], in0=lg[:], scalar1=m8[:, 1:2], scalar2=None, op0=ALU.is_ge), reads=['t_lg', 't_m8'], writes=['t_lg'])
            sc.op('dve', lambda e: e.tensor_tensor(out=ex[:], in0=ex[:], in1=lg[:], op=ALU.mult), reads=['t_ex', 't_lg'], writes=['t_ex'])
            sc.op('dve', lambda e: e.tensor_reduce(out=gsm[:, 1:2], in_=ex[:], axis=AX.X, op=ALU.add), reads=['t_ex', 't_gsm'], writes=['t_gsm'])
            sc.op('dve', lambda e: e.reciprocal(out=gsm[:, 1:2], in_=gsm[:, 1:2]), reads=['t_gsm'], writes=['t_gsm'])
            sc.op('dve', lambda e: e.tensor_scalar(out=ex[:], in0=ex[:], scalar1=gsm[:, 1:2], scalar2=None, op0=ALU.mult), reads=['t_ex', 't_gsm'], writes=['t_ex'])
            sc.dma('sp', lambda e: e.dma_start(out=S.gates[ti * 128:(ti + 1) * 128, :], in_=ex[:]), reads=['t_ex'], writes=['S.gates'])
        sc.flush()


_PARAM_KEYS = ["meta_tokens", "ev_w_in", "ev_conv_w", "ev_conv_b", "ev_convnorm_g", "ev_convnorm_b", "ev_shift_mu",
               "ev_w0", "ev_w2", "ev_a0", "ev_a2", "ev_g2", "ev_k_k", "ev_k_a", "ev_r_k", "ev_lnx_g", "ev_lnx_b",
               "ev_w_out", "ev_ln1_g", "ev_ln1_b", "ev_ffn_gate", "ev_ffn_up", "ev_ffn_down", "ev_ln2_g", "ev_ln2_b",
               "od_w_qkv", "od_b_qkv", "od_sinks", "od_w_o", "od_b_o", "od_ln1_g", "od_ln1_b", "od_router",
               "od_exp_gate", "od_exp_up", "od_exp_down", "od_ln2_g", "od_ln2_b"]


def make_in_maps(inputs, cores):
    shared = {}
    for k in _PARAM_KEYS:
        a = np.asarray(inputs[k], dtype=np.float32)
        if k != "meta_tokens":
            a = a[0]
        if k == "ev_r_k":
            a = a.reshape(512)
        shared[k] = np.ascontiguousarray(a)
    pos = (np.arange(T, dtype=np.float64) - PADF)[:, None]
    inv = 10000.0 ** (-np.arange(32, dtype=np.float64) / 32.0)[None, :]
    shared["rope_cos"] = np.cos(pos * inv).astype(np.float32)
    shared["rope_sin"] = np.sin(pos * inv).astype(np.float32)
    maps = []
    for b in cores:
        m = dict(shared)
        m["x"] = np.ascontiguousarray(np.asarray(inputs["x"][b], dtype=np.float32))
        maps.append(m)
    return maps


def kernel(**inputs):
    nc = build_program()
    in_maps = make_in_maps(inputs, list(range(8)))
    res = run_bass_kernel_spmd(nc, in_maps, core_ids=list(range(8)))
    out = np.stack([np.asarray(r["out"]) for r in res.results], axis=0)
    return out.astype(np.float32)
```

```python
import numpy as np
from contextlib import ExitStack
import concourse.bass as bass
import concourse.mybir as mybir
from concourse.bass_utils import run_bass_kernel_spmd

F32 = mybir.dt.float32
F32R = mybir.dt.float32r
I32 = mybir.dt.int32
AF = mybir.ActivationFunctionType
ALU = mybir.AluOpType
AX = mybir.AxisListType

D = 1024
SEQ = 4096
NMETA = 16
PADF = 112
T = SEQ + NMETA + PADF
NT = T // 128
DFF0 = 2816
DFFE = 3584
NEXP = 8
ALPHA = 4.0 ** 0.25
LN_EPS = 1e-5
LNX_EPS = 64e-5
DECAY_SCALE = float(np.exp(-0.5))


SKIP_SAME_ENGINE_WAW = True


class Sched:
    EPOCH = 30000
    NDSEM = 6
    ENG = ('pe', 'act', 'dve', 'pool', 'sp')

    def __init__(self, nc=None, es=None, needs=None):
        self.dry = needs is None
        self.needs = [] if self.dry else needs
        self.nc = nc
        self.es = es
        self.gi = 0
        self.seg = []
        self.lastw = {}
        self.readers = {}
        self.last_on = {}
        if not self.dry:
            self.eobj = dict(pe=nc.tensor, act=nc.scalar, dve=nc.vector, pool=nc.gpsimd, sp=nc.sync)
            self.csem = {}
            self.nsig = {e: 0 for e in self.ENG}
            self.dsem = {q: [es.enter_context(nc.semaphore(f"d_{q}_{i}")) for i in range(self.NDSEM)]
                         for q in ('sp', 'pool')}
            self.ndma = {'sp': 0, 'pool': 0}
            self.dma_ev = {'sp': [], 'pool': []}
            self.seen = {e: {} for e in self.ENG}
            self.last_ev = {}
            self.events = {}

    def op(self, eng, fn, reads=(), writes=()):
        self._do(False, eng, fn, reads, writes)

    def dma(self, q, fn, reads=(), writes=()):
        self._do(True, q, fn, reads, writes)

    def _is_reader(self, j, writes):
        for k in writes:
            for r in self.readers.get(k, ()):
                if r[0] == j:
                    return True
        return False

    def _csem(self, eng, epoch):
        k = (eng, epoch)
        if k not in self.csem:
            self.csem[k] = self.es.enter_context(self.nc.semaphore(f"c_{eng}_{epoch}"))
        return self.csem[k]

    def _wait(self, eng, ev):
        key, sem, val = ev
        if self.seen[eng].get(key, 0) >= val:
            return
        self.eobj[eng].wait_ge(sem, val)
        self.seen[eng][key] = val

    def _do(self, isd, eng, fn, reads, writes):
        i = self.gi
        self.gi += 1
        d = set()
        raw = set()
        for k in reads:
            if k in self.lastw:
                d.add(self.lastw[k])
                raw.add(self.lastw[k])
        for k in writes:
            if k in self.lastw:
                d.add(self.lastw[k])
            for r in self.readers.get(k, ()):
                d.add(r)
        deps = []
        for (j, jd, je) in d:
            if (not jd) and je == eng and eng == 'pe':
                continue
            if SKIP_SAME_ENGINE_WAW and (not jd) and (not isd) and je == eng and (j, jd, je) not in raw and not self._is_reader(j, writes):
                continue
            deps.append(j)
            if self.dry and not jd:
                self.needs[j] = True
        me = (i, isd, eng)
        for k in writes:
            self.lastw[k] = me
            self.readers[k] = []
        for k in reads:
            self.readers.setdefault(k, []).append(me)
        if not isd:
            self.last_on[eng] = i
        if self.dry:
            self.needs.append(False)
            return
        if isd:
            k = self.ndma[eng]
            if k >= self.NDSEM:
                self._wait(eng, self.dma_ev[eng][k - self.NDSEM])
        for j in sorted(deps):
            self._wait(eng, self.events[j])
        inst = fn(self.eobj[eng])
        if isd:
            k = self.ndma[eng]
            idx = k % self.NDSEM
            val = 16 * (k // self.NDSEM + 1)
            sem = self.dsem[eng][idx]
            inst.then_inc(sem, 16)
            ev = ((eng, 'd', idx), sem, val)
            self.dma_ev[eng].append(ev)
            self.ndma[eng] = k + 1
            self.events[i] = ev
        elif self.needs[i]:
            c = self.nsig[eng]
            epoch, v = divmod(c, self.EPOCH)
            sem = self._csem(eng, epoch)
            inst.then_inc(sem, 1)
            self.nsig[eng] = c + 1
            ev = ((eng, 'c', epoch), sem, v + 1)
            self.events[i] = ev
            self.last_ev[eng] = ev

    def flush(self):
        if self.dry:
            for e, i in self.last_on.items():
                self.needs[i] = True
        else:
            for e in self.ENG:
                for e2, ev in self.last_ev.items():
                    if e2 != e or e != 'pe':
                        self._wait(e, ev)
                for q in ('sp', 'pool'):
                    for ev in self.dma_ev[q][-self.NDSEM:]:
                        self._wait(e, ev)
            self.events = {}
        self.lastw = {}
        self.readers = {}
        self.last_on = {}


class Ctx:
    pass


DECLARED = set()
DBG = dict(nt=NT, stage=99)


def build_program(upto=99, debug=False):
    nc0 = bass.Bass("TRN2", target_bir_lowering=False)
    dry = Sched()
    with ExitStack() as es0:
        _build(nc0, es0, upto, debug, dry)
    nc = bass.Bass("TRN2", target_bir_lowering=False)
    with ExitStack() as es:
        sc = Sched(nc, es, needs=dry.needs)
        _build(nc, es, upto, debug, sc)
        assert sc.gi == dry.gi
    return nc


def _build(nc, es, upto, debug, sc):
    def din(name, shape):
        if upto < 5 and name.startswith("od_exp"):
            return None
        DECLARED.add(name)
        return nc.dram_tensor(name, list(shape), F32, kind="ExternalInput").ap()

    def dscr(name, shape):
        return nc.dram_tensor(name, list(shape), F32, kind="ExternalOutput" if debug else "Internal").ap()

    def dout(name, shape):
        return nc.dram_tensor(name, list(shape), F32, kind="ExternalOutput").ap()

    I = Ctx()
    I.x = din("x", [SEQ, D])
    I.meta = din("meta_tokens", [NMETA, D])
    I.w_in = din("ev_w_in", [D, 2816])
    I.conv_w = din("ev_conv_w", [31, 512])
    I.conv_b = din("ev_conv_b", [512])
    I.cn_g = din("ev_convnorm_g", [512])
    I.cn_b = din("ev_convnorm_b", [512])
    I.mu = din("ev_shift_mu", [1792])
    I.w0 = din("ev_w0", [512])
    I.w2 = din("ev_w2", [64, 512])
    I.a0 = din("ev_a0", [512])
    I.a2 = din("ev_a2", [64, 512])
    I.g2 = din("ev_g2", [128, 512])
    I.k_k = din("ev_k_k", [512])
    I.k_a = din("ev_k_a", [512])
    I.r_k = din("ev_r_k", [512])
    I.lnx_g = din("ev_lnx_g", [512])
    I.lnx_b = din("ev_lnx_b", [512])
    I.w_out = din("ev_w_out", [D, D])
    I.ln1_g = din("ev_ln1_g", [D])
    I.ln1_b = din("ev_ln1_b", [D])
    I.f_gate = din("ev_ffn_gate", [D, DFF0])
    I.f_up = din("ev_ffn_up", [D, DFF0])
    I.f_down = din("ev_ffn_down", [DFF0, D])
    I.ln2_g = din("ev_ln2_g", [D])
    I.ln2_b = din("ev_ln2_b", [D])
    I.w_qkv = din("od_w_qkv", [D, 1280])
    I.b_qkv = din("od_b_qkv", [1280])
    I.sinks = din("od_sinks", [16])
    I.w_o = din("od_w_o", [D, D])
    I.b_o = din("od_b_o", [D])
    I.oln1_g = din("od_ln1_g", [D])
    I.oln1_b = din("od_ln1_b", [D])
    I.router = din("od_router", [D, NEXP])
    I.e_gate = din("od_exp_gate", [NEXP, D, DFFE])
    I.e_up = din("od_exp_up", [NEXP, D, DFFE])
    I.e_down = din("od_exp_down", [NEXP, DFFE, D])
    I.oln2_g = din("od_ln2_g", [D])
    I.oln2_b = din("od_ln2_b", [D])
    I.rope_cos = din("rope_cos", [T, 32])
    I.rope_sin = din("rope_sin", [T, 32])
    I.out = dout("out", [SEQ, D])

    S = Ctx()
    S.aT = dscr("s_aT", [4, 128, T])
    S.h1 = dscr("s_h1", [T, D])
    S.h1T = dscr("s_h1T", [8, 128, T])
    S.h2 = dscr("s_h2", [T, D])
    S.h2T = dscr("s_h2T", [8, 128, T])
    S.h3 = dscr("s_h3", [T, D])
    S.h3T = dscr("s_h3T", [8, 128, T])
    S.gates = dscr("s_gates", [T, NEXP])
    K = Ctx()
    K.psall = es.enter_context(nc.psum_tensor("psall", [128, 4096], F32))
    K.ps = [K.psall[:, b * 512:(b + 1) * 512] for b in range(8)]
    K.bank = [0]

    def sb(st, name, shape, dt=F32):
        return st.enter_context(nc.sbuf_tensor(name, list(shape), dt))
    K.sb = sb

    def nb():
        b = K.bank[0]
        K.bank[0] = (b + 1) % 8
        return b
    K.nb = nb

    def nb2():
        b = K.bank[0]
        if b % 2:
            b = (b + 1) % 8
        K.bank[0] = (b + 2) % 8
        return b
    K.nb2 = nb2

    with nc.Block() as block:
        @block.sync
        def _(sync):
            build_consts(nc, es, sc, K, I)
            sc.flush()
            if upto >= 1:
                phase_1a(nc, sc, K, I, S)
            if upto >= 2:
                phase_1b(nc, sc, K, I, S)
            if upto >= 3:
                phase_ffn0(nc, sc, K, I, S)
            if upto >= 4:
                phase_attn(nc, sc, K, I, S)
            if upto >= 5:
                phase_moe(nc, sc, K, I, S)
            if upto < 5:
                phase_fin(nc, sc, K, I, S)


def build_consts(nc, es, sc, K, I):
    sb = K.sb
    K.dI = sb(es, "k_dI", [128, 128], I32)
    K.ident = sb(es, "k_ident", [128, 128], F32)
    K.identr = sb(es, "k_identr", [128, 128], F32R)
    K.onesr = sb(es, "k_onesr", [128, 128], F32R)
    K.blk1r = sb(es, "k_blk1r", [128, 128], F32R)
    K.m_lt = sb(es, "k_mlt", [128, 128], F32)
    K.m_gt = sb(es, "k_mgt", [128, 128], F32)
    K.m_ge = sb(es, "k_mge", [128, 128], F32)
    K.eps_ln = sb(es, "k_epsln", [128, 1], F32)
    K.eps_lnx = sb(es, "k_epslnx", [128, 1], F32)
    K.ones_f = sb(es, "k_onesf", [128, 512], F32)
    K.zero_f = sb(es, "k_zerof", [128, 512], F32)
    sc.op('pool', lambda e: e.iota(K.dI[:], pattern=[[1, 128]], base=0, channel_multiplier=-1), writes=['dI'])
    sc.op('dve', lambda e: e.tensor_single_scalar(out=K.ident[:], in_=K.dI[:], scalar=0, op=ALU.is_equal), reads=['dI'], writes=['ident'])
    sc.op('dve', lambda e: e.tensor_copy(out=K.identr[:], in_=K.ident[:]), reads=['ident'], writes=['identr'])
    sc.op('dve', lambda e: e.tensor_single_scalar(out=K.m_lt[:], in_=K.dI[:], scalar=0, op=ALU.is_lt), reads=['dI'], writes=['m_lt'])
    sc.op('dve', lambda e: e.tensor_single_scalar(out=K.m_gt[:], in_=K.dI[:], scalar=0, op=ALU.is_gt), reads=['dI'], writes=['m_gt'])
    sc.op('dve', lambda e: e.tensor_single_scalar(out=K.m_ge[:], in_=K.dI[:], scalar=0, op=ALU.is_ge), reads=['dI'], writes=['m_ge'])
    sc.op('dve', lambda e: e.memset(K.ones_f[:], 1.0), writes=['ones_f'])
    sc.op('dve', lambda e: e.memset(K.zero_f[:], 0.0), writes=['zero_f'])
    sc.op('dve', lambda e: e.tensor_copy(out=K.onesr[:], in_=K.ones_f[:, 0:128]), reads=['ones_f'], writes=['onesr'])
    sc.op('dve', lambda e: e.tensor_copy(out=K.blk1r[:], in_=K.zero_f[:, 0:128]), reads=['zero_f'], writes=['blk1r'])
    sc.op('dve', lambda e: e.tensor_copy(out=K.blk1r[0:64, 0:64], in_=K.ones_f[0:64, 0:64]), reads=['ones_f', 'blk1r'], writes=['blk1r'])
    sc.op('dve', lambda e: e.tensor_copy(out=K.blk1r[64:128, 64:128], in_=K.ones_f[64:128, 0:64]), reads=['ones_f', 'blk1r'], writes=['blk1r'])
    sc.op('dve', lambda e: e.memset(K.eps_ln[:], LN_EPS), writes=['eps_ln'])
    sc.op('dve', lambda e: e.memset(K.eps_lnx[:], LNX_EPS), writes=['eps_lnx'])


def load_cols(nc, sc, K, st, name, vec_ap, n):
    nch = n // 128
    rows = K.sb(st, name + "_r", [nch, 128], F32)
    cols = K.sb(st, name, [128, nch], F32)
    sc.dma('sp', lambda e: e.dma_start(out=rows[:], in_=vec_ap.rearrange("(c p) -> c p", p=128)), writes=[name + "_r"])
    b = K.nb()
    sc.op('pe', lambda e: e.transpose(K.ps[b][:, 0:nch], rows[:], K.ident[0:nch, 0:nch]), reads=[name + "_r", 'ident'], writes=[('ps', b)])
    sc.op('dve', lambda e: e.tensor_copy(out=cols[:], in_=K.ps[b][:, 0:nch]), reads=[('ps', b)], writes=[name])
    return cols


def load_bcast(nc, sc, K, st, name, vec_ap, n):
    t = K.sb(st, name, [128, n], F32)
    sc.dma('sp', lambda e: e.dma_start(out=t[:], in_=vec_ap.partition_broadcast(128)), writes=[name])
    return t


def load_h_tile(nc, sc, K, I, ht, key, ti):
    if ti == 0:
        sc.op('pool', lambda e: e.memset(ht[:], 0.0), writes=[key])
        sc.dma('sp', lambda e: e.dma_start(out=ht[PADF:128, :], in_=I.meta), writes=[key])
    else:
        sc.dma('sp', lambda e: e.dma_start(out=ht[:], in_=I.x[(ti - 1) * 128: ti * 128, :]), writes=[key])


def transpose_to(nc, sc, K, src, src_key, dst_fn, dst_key, nchunk, evac='act'):
    for c0 in range(0, nchunk, 4):
        n = min(4, nchunk - c0)
        b = K.nb()
        for j in range(n):
            c = c0 + j
            sc.op('pe', lambda e, c=c, j=j, b=b: e.transpose(K.ps[b][:, j * 128:(j + 1) * 128], src[:, c * 128:(c + 1) * 128], K.ident[:]),
                  reads=[src_key, 'ident'], writes=[('ps', b)])
        dst = dst_fn(c0, n)
        if evac == 'act':
            sc.op('act', lambda e, b=b, n=n, dst=dst: e.copy(out=dst, in_=K.ps[b][:, 0:n * 128].rearrange("p (c t) -> p c t", t=128)),
                  reads=[('ps', b)], writes=[dst_key])
        else:
            sc.op('dve', lambda e, b=b, n=n, dst=dst: e.tensor_copy(out=dst, in_=K.ps[b][:, 0:n * 128].rearrange("p (c t) -> p c t", t=128)),
                  reads=[('ps', b)], writes=[dst_key])


def load_weight_r(nc, sc, K, wt, key, w_ap, kchunks, ncols, col0=0, rows0=0):
    step = max(1, 4096 // ncols) if ncols <= 2048 else 1
    for k in range(kchunks):
        for c in range(0, ncols, 2048):
            cw = min(2048, ncols - c)
            sc.dma('pool', lambda e, k=k, c=c, cw=cw: e.dma_start(
                out=wt[:, k, c:c + cw], in_=w_ap[rows0 + k * 128: rows0 + (k + 1) * 128, col0 + c: col0 + c + cw]),
                writes=[key])


def phase_1a(nc, sc, K, I, S):
    with ExitStack() as st:
        sb = K.sb
        wA = sb(st, "a_w", [128, 8, 1024], F32R)
        load_weight_r(nc, sc, K, wA, 'a_w', I.w_in, 8, 1024, col0=0)
        cw = load_cols_multi = None
        convw = sb(st, "a_convw", [128, 4, 31], F32)
        cw_rows = sb(st, "a_convw_r", [31, 512], F32)
        sc.dma('sp', lambda e: e.dma_start(out=cw_rows[:], in_=I.conv_w), writes=['a_convw_r'])
        for c in range(4):
            b = K.nb()
            sc.op('pe', lambda e, c=c, b=b: e.transpose(K.ps[b][:, 0:31], cw_rows[:, c * 128:(c + 1) * 128], K.ident[0:31, 0:31]),
                  reads=['a_convw_r', 'ident'], writes=[('ps', b)])
            sc.op('dve', lambda e, c=c, b=b: e.tensor_copy(out=convw[:, c, :], in_=K.ps[b][:, 0:31]), reads=[('ps', b)], writes=['a_convw'])
        convb = load_cols(nc, sc, K, st, "a_convb", I.conv_b, 512)
        cng = load_cols(nc, sc, K, st, "a_cng", I.cn_g, 512)
        cnb = load_cols(nc, sc, K, st, "a_cnb", I.cn_b, 512)
        SP = 512
        ht = [sb(st, f"a_ht{i}", [128, D], F32) for i in range(2)]
        hT = [sb(st, f"a_hT{i}", [128, 8, SP], F32R) for i in range(2)]
        hg = sb(st, "a_hg", [128, 4, 30 + SP], F32)
        sgt = sb(st, "a_sg", [128, SP], F32)
        acc = [sb(st, f"a_acc{i}", [128, 4, SP], F32) for i in range(2)]
        accr = sb(st, "a_accr", [128, 4, SP], F32R)
        sq = sb(st, "a_sq", [128, 4, SP], F32R)
        mean = sb(st, "a_mean", [128, SP], F32)
        msq = sb(st, "a_msq", [128, SP], F32)
        rstd = sb(st, "a_rstd", [128, SP], F32)
        xc = sb(st, "a_xc", [128, SP], F32)
        yo = [sb(st, f"a_yo{i}", [128, 4, SP], F32) for i in range(2)]
        sc.op('dve', lambda e: e.memset(hg[:], 0.0), writes=['a_hg'])
        spans = [(s, min(4, NT - s)) for s in range(0, NT, 4)]
        nld = 0
        for si, (t0, ntl) in enumerate(spans):
            n = ntl * 128
            hTc = hT[si % 2]
            hk = f"a_hT{si % 2}"
            for j in range(ntl):
                hb = ht[nld % 2]
                hbk = f"a_ht{nld % 2}"
                nld += 1
                load_h_tile(nc, sc, K, I, hb, hbk, t0 + j)
                transpose_to(nc, sc, K, hb, hbk, lambda c0, nn, j=j, hTc=hTc: hTc[:, c0:c0 + nn, j * 128:(j + 1) * 128], hk, 8,
                             evac='act' if j % 2 == 0 else 'dve')
            if si > 0:
                sc.op('dve', lambda e: e.tensor_copy(out=hg[:, :, 0:30], in_=hg[:, :, SP:SP + 30]), reads=['a_hg'], writes=['a_hg'])
            for c in range(4):
                bv = K.nb()
                bg = K.nb()
                for k in range(8):
                    sc.op('pe', lambda e, k=k, c=c, bv=bv: e.matmul(K.ps[bv][:, 0:n], wA[:, k, c * 128:(c + 1) * 128], hTc[:, k, 0:n], start=(k == 0), stop=(k == 7)),
                          reads=['a_w', hk], writes=[('ps', bv)])
                for k in range(8):
                    sc.op('pe', lambda e, k=k, c=c, bg=bg: e.matmul(K.ps[bg][:, 0:n], wA[:, k, 512 + c * 128:512 + (c + 1) * 128], hTc[:, k, 0:n], start=(k == 0), stop=(k == 7)),
                          reads=['a_w', hk], writes=[('ps', bg)])
                sc.op('act', lambda e, bg=bg: e.activation(out=sgt[:, 0:n], in_=K.ps[bg][:, 0:n], func=AF.Sigmoid), reads=[('ps', bg)], writes=['a_sg'])
                sc.op('dve', lambda e, bv=bv, c=c: e.tensor_tensor(out=hg[:, c, 30:30 + n], in0=K.ps[bv][:, 0:n], in1=sgt[:, 0:n], op=ALU.mult),
                      reads=[('ps', bv), 'a_sg'], writes=['a_hg'])
            ac = acc[si % 2]
            ak = f"a_acc{si % 2}"
            acf = ac
            for c in range(4):
                sc.op('dve', lambda e, c=c: e.tensor_scalar(out=acf[:, c, 0:n], in0=hg[:, c, 0:n], scalar1=convw[:, c, 0:1], scalar2=convb[:, c:c + 1], op0=ALU.mult, op1=ALU.add),
                      reads=['a_hg', 'a_convw', 'a_convb'], writes=[ak])
                for k in range(1, 31):
                    o = acf
                    sc.op('dve', lambda e, c=c, k=k, o=o: e.scalar_tensor_tensor(out=o[:, c, 0:n], in0=hg[:, c, k:k + n], scalar=convw[:, c, k:k + 1], in1=acf[:, c, 0:n], op0=ALU.mult, op1=ALU.add),
                          reads=['a_hg', 'a_convw', ak], writes=[ak])
            sc.op('act', lambda e: e.activation(out=sq[:, :, 0:n], in_=acf[:, :, 0:n], func=AF.Square), reads=[ak], writes=['a_sq'])
            sc.op('act', lambda e: e.copy(out=accr[:, :, 0:n], in_=acf[:, :, 0:n]), reads=[ak], writes=['a_accr'])
            b1 = K.nb()
            b2 = K.nb()
            for c in range(4):
                sc.op('pe', lambda e, c=c: e.matmul(K.ps[b1][:, 0:n], K.onesr[:], accr[:, c, 0:n], start=(c == 0), stop=(c == 3)), reads=['onesr', 'a_accr'], writes=[('ps', b1)])
            for c in range(4):
                sc.op('pe', lambda e, c=c: e.matmul(K.ps[b2][:, 0:n], K.onesr[:], sq[:, c, 0:n], start=(c == 0), stop=(c == 3)), reads=['onesr', 'a_sq'], writes=[('ps', b2)])
            sc.op('act', lambda e: e.mul(out=mean[:, 0:n], in_=K.ps[b1][:, 0:n], mul=1.0 / 512), reads=[('ps', b1)], writes=['a_mean'])
            sc.op('dve', lambda e: e.tensor_tensor(out=msq[:, 0:n], in0=mean[:, 0:n], in1=mean[:, 0:n], op=ALU.mult), reads=['a_mean'], writes=['a_msq'])
            sc.op('dve', lambda e: e.scalar_tensor_tensor(out=rstd[:, 0:n], in0=K.ps[b2][:, 0:n], scalar=1.0 / 512, in1=msq[:, 0:n], op0=ALU.mult, op1=ALU.subtract),
                  reads=[('ps', b2), 'a_msq'], writes=['a_rstd'])
            sc.op('act', lambda e: e.activation(out=rstd[:, 0:n], in_=rstd[:, 0:n], func=AF.Sqrt, bias=K.eps_ln[:, 0:1], scale=1.0), reads=['a_rstd', 'eps_ln'], writes=['a_rstd'])
            sc.op('dve', lambda e: e.reciprocal(out=rstd[:, 0:n], in_=rstd[:, 0:n]), reads=['a_rstd'], writes=['a_rstd'])
            y = yo[si % 2]
            yk = f"a_yo{si % 2}"
            for c in range(4):
                sc.op('dve', lambda e, c=c: e.tensor_tensor(out=xc[:, 0:n], in0=acf[:, c, 0:n], in1=mean[:, 0:n], op=ALU.subtract), reads=[ak, 'a_mean'], writes=['a_xc'])
                sc.op('dve', lambda e, c=c: e.tensor_tensor(out=xc[:, 0:n], in0=xc[:, 0:n], in1=rstd[:, 0:n], op=ALU.mult), reads=['a_xc', 'a_rstd'], writes=['a_xc'])
                sc.op('act', lambda e, c=c: e.activation(out=y[:, c, 0:n], in_=xc[:, 0:n], func=AF.Silu, bias=cnb[:, c:c + 1], scale=cng[:, c:c + 1]),
                      reads=['a_xc', 'a_cnb', 'a_cng'], writes=[yk])
            for c in range(4):
                sc.dma('sp', lambda e, c=c: e.dma_start(out=S.aT[c, :, t0 * 128: t0 * 128 + n], in_=y[:, c, 0:n]), reads=[yk], writes=['S.aT'])
        sc.flush()


def phase_fin(nc, sc, K, I, S):
    with ExitStack() as st:
        buf = K.sb(st, "fin_buf", [128, D], F32)
        sc.op('dve', lambda e: e.memset(buf[:], 0.0), writes=['fin_buf'])
        sc.dma('sp', lambda e: e.dma_start(out=I.out[0:128, :], in_=buf[:]), reads=['fin_buf'], writes=['out'])
        sc.flush()


def layernorm_tm(sc, K, x, xk, g_bc, gk, b_bc, bk, out, ok, tmp):
    st6, mv, sd = tmp['st6'], tmp['mv'], tmp['sd']
    for hfi in range(2):
        sc.op('dve', lambda e, hfi=hfi: e.bn_stats(out=st6[:, hfi * 6:(hfi + 1) * 6], in_=x[:, hfi * 512:(hfi + 1) * 512]), reads=[xk], writes=['ln_st6'])
    sc.op('dve', lambda e: e.bn_aggr(out=mv[:, 0:2], in_=st6[:, 0:12]), reads=['ln_st6'], writes=['ln_mv'])
    sc.op('act', lambda e: e.activation(out=sd[:, 0:1], in_=mv[:, 1:2], func=AF.Sqrt, bias=K.eps_ln[:, 0:1], scale=1.0), reads=['ln_mv', 'eps_ln'], writes=['ln_sd'])
    sc.op('dve', lambda e: e.reciprocal(out=sd[:, 0:1], in_=sd[:, 0:1]), reads=['ln_sd'], writes=['ln_sd'])
    sc.op('dve', lambda e: e.tensor_scalar(out=out[:], in0=x[:], scalar1=mv[:, 0:1], scalar2=sd[:, 0:1], op0=ALU.subtract, op1=ALU.mult),
          reads=[xk, 'ln_mv', 'ln_sd'], writes=[ok])
    sc.op('dve', lambda e: e.tensor_tensor(out=out[:], in0=out[:], in1=g_bc[:], op=ALU.mult), reads=[ok, gk], writes=[ok])
    sc.op('dve', lambda e: e.tensor_tensor(out=out[:], in0=out[:], in1=b_bc[:], op=ALU.add), reads=[ok, bk], writes=[ok])


def ln_tmp(K, st, pfx):
    return dict(st6=K.sb(st, pfx + "_st6", [128, 12], F32), mv=K.sb(st, pfx + "_mv", [128, 2], F32), sd=K.sb(st, pfx + "_sd", [128, 1], F32))


def store_h_and_hT(nc, sc, K, h, hk, hT, hTk, dram_h, dram_hT, ti):
    sc.dma('sp', lambda e: e.dma_start(out=dram_h[ti * 128:(ti + 1) * 128, :], in_=h[:]), reads=[hk], writes=['dram_h'])
    transpose_to(nc, sc, K, h, hk, lambda c0, nn: hT[:, c0:c0 + nn, :], hTk, 8, evac='act')
    for c0 in (0, 4):
        sc.dma('sp', lambda e, c0=c0: e.dma_start(out=dram_hT[c0:c0 + 4, :, ti * 128:(ti + 1) * 128].rearrange("c p t -> p c t"), in_=(hT[:, c0:c0 + 4, :].bitcast(F32) if hT.dtype == F32R else hT[:, c0:c0 + 4, :])),
               reads=[hTk], writes=['dram_hT'])


def phase_1b(nc, sc, K, I, S):
    with ExitStack() as st:
        sb = K.sb
        V3 = [128, 4, 128]
        wB = sb(st, "b_wB", [128, 8, 1792], F32R)
        load_weight_r(nc, sc, K, wB, 'b_wB', I.w_in, 8, 1792, col0=1024)
        wO = sb(st, "b_wO", [128, 8, 1024], F32R)
        load_weight_r(nc, sc, K, wO, 'b_wO', I.w_out, 8, 1024)
        wa2 = sb(st, "b_wa2", [128, 512], F32R)
        sc.dma('pool', lambda e: e.dma_start(out=wa2[0:64, :], in_=I.w2), writes=['b_wa2'])
        sc.dma('pool', lambda e: e.dma_start(out=wa2[64:128, :], in_=I.a2), writes=['b_wa2'])
        g2r = sb(st, "b_g2r", [128, 512], F32R)
        sc.dma('pool', lambda e: e.dma_start(out=g2r[:], in_=I.g2), writes=['b_g2r'])
        mu = load_cols(nc, sc, K, st, "b_mu", I.mu, 1792)
        w0c = load_cols(nc, sc, K, st, "b_w0c", I.w0, 512)
        a0c = load_cols(nc, sc, K, st, "b_a0c", I.a0, 512)
        kkc = load_cols(nc, sc, K, st, "b_kkc", I.k_k, 512)
        kac = load_cols(nc, sc, K, st, "b_kac", I.k_a, 512)
        rkc = load_cols(nc, sc, K, st, "b_rkc", I.r_k, 512)
        omka = sb(st, "b_omka", [128, 4], F32)
        sc.op('dve', lambda e: e.tensor_scalar(out=omka[:], in0=kac[:], scalar1=-1.0, scalar2=1.0, op0=ALU.mult, op1=ALU.add), reads=['b_kac'], writes=['b_omka'])
        lnxg = load_bcast(nc, sc, K, st, "b_lnxg", I.lnx_g, 512)
        lnxb = load_bcast(nc, sc, K, st, "b_lnxb", I.lnx_b, 512)
        ln1g = load_bcast(nc, sc, K, st, "b_ln1g", I.ln1_g, D)
        ln1b = load_bcast(nc, sc, K, st, "b_ln1b", I.ln1_b, D)
        indf = sb(st, "b_indf", [128, 4, 8], F32)
        ind = sb(st, "b_ind", [128, 4, 8], F32R)
        sc.op('dve', lambda e: e.memset(indf[:], 0.0), writes=['b_indf'])
        for c in range(4):
            sc.op('dve', lambda e, c=c: e.memset(indf[0:64, c, 2 * c:2 * c + 1], 1.0), reads=['b_indf'], writes=['b_indf'])
            sc.op('dve', lambda e, c=c: e.memset(indf[64:128, c, 2 * c + 1:2 * c + 2], 1.0), reads=['b_indf'], writes=['b_indf'])
        sc.op('dve', lambda e: e.tensor_copy(out=ind[:], in_=indf[:]), reads=['b_indf'], writes=['b_ind'])
        ST = [sb(st, f"b_ST{i}", V3, F32R) for i in range(2)]
        sc.op('dve', lambda e: e.tensor_copy(out=ST[0][:], in_=K.zero_f[:, 0:512].rearrange("p (c t) -> p c t", t=128)), reads=['zero_f'], writes=['b_ST0'])
        zT = sb(st, "b_zT", [128, 14, 129], F32)
        sc.op('dve', lambda e: e.memset(zT[:], 0.0), writes=['b_zT'])
        hb = [sb(st, "b_h0", [128, D], F32)]
        hT = sb(st, "b_hT", [128, 8, 128], F32R)
        zl = sb(st, "b_zl", [128, 14, 128], F32)
        lo = sb(st, "b_lo", [128, 128], F32R)
        sgl = sb(st, "b_sgl", [128, 128], F32R)
        g_tm = sb(st, "b_gtm", [128, 512], F32)
        sgw = sb(st, "b_sgw", V3, F32)
        a_t = sb(st, "b_a", V3, F32)
        Lc = sb(st, "b_Lc", V3, F32)
        Wt = sb(st, "b_Wt", V3, F32)
        Winv = sb(st, "b_Winv", V3, F32)
        Wp = sb(st, "b_Wp", V3, F32)
        kk = sb(st, "b_kk", V3, F32)
        sqk = sb(st, "b_sqk", V3, F32R)
        nrm = Lc
        t1 = sb(st, "b_t1", V3, F32)
        kmod = sgw
        kt = sb(st, "b_kt", V3, F32R)
        ab = sb(st, "b_ab", V3, F32R)
        kb = sb(st, "b_kb", V3, F32R)
        rt = sb(st, "b_rt", V3, F32R)
        rkr = sb(st, "b_rkr", V3, F32R)
        bs = sb(st, "b_bs", [128, 8], F32)
        Vt = sb(st, "b_V", [128, 512], F32R)
        ab_tm = sb(st, "b_abtm", [128, 512], F32R)
        kb_tm = sb(st, "b_kbtm", [128, 512], F32R)
        H3 = [128, 8, 128]
        Pm = [sb(st, f"b_P{i}", H3, F32R) for i in range(2)]
        Qm = [sb(st, f"b_Q{i}", H3, F32R) for i in range(2)]
        Xs = sb(st, "b_X0", H3, F32R)
        Xm = [Xs, Xs]
        BT, ArT, BrT = Pm[0], Pm[1], Qm[0]
        Zs = Qm[1][:, 0:4, :].rearrange("p c t -> p (c t)")
        U = Qm[1][:, 4:8, :].rearrange("p c t -> p (c t)")
        ysb = sb(st, "b_ysb", [128, 512], F32)
        s1 = sb(st, "b_s1", [128, 8], F32)
        s2 = sb(st, "b_s2", [128, 8], F32)
        mixT = hT
        res = zl[:, 0:8, :].rearrange("p c t -> p (c t)")
        lt = ln_tmp(K, st, "b_ln")

        def bc4(ap):
            return ap.unsqueeze(2).broadcast_to(V3)

        def mask4(m):
            return m[:].unsqueeze(1).broadcast_to(V3)

        def v4(ap):
            return ap.rearrange("p (c t) -> p c t", t=128)

        def v8(ap):
            return ap.rearrange("p (h i) -> p h i", i=64)

        for ti in range(DBG["nt"]):
            h = hb[0]
            hk = "b_h0"
            load_h_tile(nc, sc, K, I, h, hk, ti)
            transpose_to(nc, sc, K, h, hk, lambda c0, nn: hT[:, c0:c0 + nn, :], 'b_hT', 8, evac='act')
            for c0 in range(0, 14, 4):
                nn = min(4, 14 - c0)
                b = K.nb()
                for j in range(nn):
                    for k in range(8):
                        sc.op('pe', lambda e, j=j, k=k: e.matmul(K.ps[b][:, j * 128:(j + 1) * 128], wB[:, k, (c0 + j) * 128:(c0 + j + 1) * 128], hT[:, k, :], start=(k == 0), stop=(k == 7)),
                              reads=['b_wB', 'b_hT'], writes=[('ps', b)])
                sc.op('act', lambda e: e.copy(out=zT[:, c0:c0 + nn, 1:129], in_=K.ps[b][:, 0:nn * 128].rearrange("p (c t) -> p c t", t=128)), reads=[('ps', b)], writes=['b_zT'])
            sc.op('dve', lambda e: e.tensor_tensor(out=zl[:], in0=zT[:, :, 0:128], in1=zT[:, :, 1:129], op=ALU.subtract), reads=['b_zT'], writes=['b_zl'])
            sc.op('dve', lambda e: e.tensor_tensor(out=zl[:], in0=zl[:], in1=mu[:].unsqueeze(2).broadcast_to([128, 14, 128]), op=ALU.mult), reads=['b_zl', 'b_mu'], writes=['b_zl'])
            sc.op('dve', lambda e: e.tensor_tensor(out=zl[:], in0=zl[:], in1=zT[:, :, 1:129], op=ALU.add), reads=['b_zl', 'b_zT'], writes=['b_zl'])
            sc.op('act', lambda e: e.copy(out=zT[:, :, 0:1], in_=zT[:, :, 128:129]), reads=['b_zT'], writes=['b_zT'])
            if DBG["stage"] <= 1:
                continue
            r_ = zl[:, 0:4, :]
            k_ = zl[:, 4:8, :]
            sc.op('act', lambda e: e.activation(out=lo[0:64, :], in_=zl[0:64, 12, :], func=AF.Tanh), reads=['b_zl'], writes=['b_lo'])
            sc.op('act', lambda e: e.copy(out=lo[64:128, :], in_=zl[64:128, 12, :]), reads=['b_zl'], writes=['b_lo'])
            sc.op('act', lambda e: e.activation(out=sgl[:], in_=zl[:, 13, :], func=AF.Sigmoid), reads=['b_zl'], writes=['b_sgl'])
            bw = K.nb()
            for c in range(4):
                sc.op('pe', lambda e, c=c: e.matmul(K.ps[bw][:, c * 128:(c + 1) * 128], wa2[0:64, c * 128:(c + 1) * 128], lo[0:64, :], start=True, stop=True), reads=['b_wa2', 'b_lo'], writes=[('ps', bw)])
            for c in range(4):
                sc.op('act', lambda e, c=c: e.activation(out=sgw[:, c, :], in_=K.ps[bw][:, c * 128:(c + 1) * 128], func=AF.Sigmoid, bias=w0c[:, c:c + 1], scale=1.0), reads=[('ps', bw), 'b_w0c'], writes=['b_sgw'])
            ba = K.nb()
            for c in range(4):
                sc.op('pe', lambda e, c=c: e.matmul(K.ps[ba][:, c * 128:(c + 1) * 128], wa2[64:128, c * 128:(c + 1) * 128], lo[64:128, :], start=True, stop=True), reads=['b_wa2', 'b_lo'], writes=[('ps', ba)])
            for c in range(4):
                sc.op('act', lambda e, c=c: e.activation(out=a_t[:, c, :], in_=K.ps[ba][:, c * 128:(c + 1) * 128], func=AF.Sigmoid, bias=a0c[:, c:c + 1], scale=1.0), reads=[('ps', ba), 'b_a0c'], writes=['b_a'])
            bg = K.nb()
            sc.op('pe', lambda e: e.matmul(K.ps[bg][:, 0:512], sgl[:], g2r[:], start=True, stop=True), reads=['b_sgl', 'b_g2r'], writes=[('ps', bg)])
            sc.op('act', lambda e: e.copy(out=g_tm[:], in_=K.ps[bg][:, 0:512]), reads=[('ps', bg)], writes=['b_gtm'])
            for c in range(4):
                sc.op('dve', lambda e, c=c: e.tensor_tensor_scan(out=Lc[:, c, :], data0=K.ones_f[:, 0:128], data1=sgw[:, c, :], initial=0.0, op0=ALU.mult, op1=ALU.add), reads=['b_sgw', 'ones_f'], writes=['b_Lc'])
            sc.op('dve', lambda e: e.tensor_tensor(out=t1[:], in0=Lc[:], in1=sgw[:], op=ALU.subtract), reads=['b_Lc', 'b_sgw'], writes=['b_t1'])
            sc.op('act', lambda e: e.activation(out=Wt[:], in_=Lc[:], func=AF.Exp, scale=-DECAY_SCALE), reads=['b_Lc'], writes=['b_Wt'])
            sc.op('act', lambda e: e.activation(out=Winv[:], in_=Lc[:], func=AF.Exp, scale=DECAY_SCALE), reads=['b_Lc'], writes=['b_Winv'])
            sc.op('act', lambda e: e.activation(out=Wp[:], in_=t1[:], func=AF.Exp, scale=-DECAY_SCALE), reads=['b_t1'], writes=['b_Wp'])
            sc.op('dve', lambda e: e.tensor_tensor(out=kk[:], in0=k_, in1=bc4(kkc[:]), op=ALU.mult), reads=['b_zl', 'b_kkc'], writes=['b_kk'])
            sc.op('act', lambda e: e.activation(out=sqk[:], in_=kk[:], func=AF.Square), reads=['b_kk'], writes=['b_sqk'])
            bsq = K.nb()
            for c in range(4):
                sc.op('pe', lambda e, c=c: e.matmul(K.ps[bsq][:, c * 128:(c + 1) * 128], K.blk1r[:], sqk[:, c, :], start=True, stop=True), reads=['blk1r', 'b_sqk'], writes=[('ps', bsq)])
            sc.op('act', lambda e: e.activation(out=nrm[:], in_=v4(K.ps[bsq][:, 0:512]), func=AF.Sqrt), reads=[('ps', bsq)], writes=['b_Lc'])
            sc.op('dve', lambda e: e.tensor_scalar_max(out=nrm[:], in0=nrm[:], scalar1=1e-12), reads=['b_Lc'], writes=['b_Lc'])
            sc.op('dve', lambda e: e.reciprocal(out=nrm[:], in_=nrm[:]), reads=['b_Lc'], writes=['b_Lc'])
            sc.op('dve', lambda e: e.tensor_tensor(out=kk[:], in0=kk[:], in1=nrm[:], op=ALU.mult), reads=['b_kk', 'b_Lc'], writes=['b_kk'])
            sc.op('dve', lambda e: e.tensor_tensor(out=t1[:], in0=a_t[:], in1=bc4(kac[:]), op=ALU.mult), reads=['b_a', 'b_kac'], writes=['b_t1'])
            sc.op('dve', lambda e: e.tensor_tensor(out=t1[:], in0=t1[:], in1=bc4(omka[:]), op=ALU.add), reads=['b_t1', 'b_omka'], writes=['b_t1'])
            sc.op('dve', lambda e: e.tensor_tensor(out=kmod[:], in0=k_, in1=t1[:], op=ALU.mult), reads=['b_zl', 'b_t1'], writes=['b_sgw'])
            sc.op('dve', lambda e: e.tensor_tensor(out=kt[:], in0=kk[:], in1=Wp[:], op=ALU.mult), reads=['b_kk', 'b_Wp'], writes=['b_kt'])
            sc.op('dve', lambda e: e.tensor_tensor(out=t1[:], in0=kk[:], in1=a_t[:], op=ALU.mult), reads=['b_kk', 'b_a', 'b_t1'], writes=['b_t1'])
            sc.op('dve', lambda e: e.tensor_tensor(out=ab[:], in0=t1[:], in1=Winv[:], op=ALU.mult), reads=['b_t1', 'b_Winv'], writes=['b_ab'])
            sc.op('dve', lambda e: e.tensor_tensor(out=kb[:], in0=kmod[:], in1=Winv[:], op=ALU.mult), reads=['b_sgw', 'b_Winv'], writes=['b_kb'])
            sc.op('dve', lambda e: e.tensor_tensor(out=rt[:], in0=r_, in1=Wt[:], op=ALU.mult), reads=['b_zl', 'b_Wt'], writes=['b_rt'])
            sc.op('dve', lambda e: e.tensor_tensor(out=t1[:], in0=r_, in1=kmod[:], op=ALU.mult), reads=['b_zl', 'b_sgw', 'b_t1'], writes=['b_t1'])
            sc.op('dve', lambda e: e.tensor_tensor(out=rkr[:], in0=t1[:], in1=bc4(rkc[:]), op=ALU.mult), reads=['b_t1', 'b_rkc'], writes=['b_rkr'])
            bb = K.nb()
            for c in range(4):
                sc.op('pe', lambda e, c=c: e.matmul(K.ps[bb][:, 0:8], rkr[:, c, :], ind[:, c, :], start=(c == 0), stop=(c == 3)), reads=['b_rkr', 'b_ind'], writes=[('ps', bb)])
            sc.op('act', lambda e: e.copy(out=bs[:], in_=K.ps[bb][:, 0:8]), reads=[('ps', bb)], writes=['b_bs'])
            if DBG["stage"] <= 2:
                continue
            for (src_fn, dst, dk, rk_) in ((lambda c: zl[:, 8 + c, :], Vt, 'b_V', 'b_zl'),
                                          (lambda c: ab[:, c, :], ab_tm, 'b_abtm', 'b_ab'),
                                          (lambda c: kb[:, c, :], kb_tm, 'b_kbtm', 'b_kb'))[DBG.get('sub0', 0):DBG.get('sub1', 3)]:
                b = K.nb()
                for c in range(4):
                    if rk_ == 'b_zl':
                        sc.op('pe', lambda e, c=c: e.transpose(K.ps[b][:, c * 128:(c + 1) * 128], src_fn(c), K.ident[:]), reads=[rk_, 'ident'], writes=[('ps', b)])
                    else:
                        sc.op('pe', lambda e, c=c: e.transpose(K.ps[b][:, c * 128:(c + 1) * 128].bitcast(F32R), src_fn(c), K.identr[:]), reads=[rk_, 'identr'], writes=[('ps', b)])
                sc.op('act', lambda e: e.copy(out=dst[:], in_=K.ps[b][:, 0:512]), reads=[('ps', b)], writes=[dk])
            if DBG["stage"] <= 3:
                continue
            def headmm(L, Lk, Rr, Rk, mask, dst, dk):
                b2 = K.nb2()
                for par in range(2):
                    p0 = 64 * par
                    for c in range(4):
                        sc.op('pe', lambda e, c=c: e.matmul(K.ps[b2 + par][:, c * 128:(c + 1) * 128], L[p0:p0 + 64, c, :], Rr[p0:p0 + 64, c, :], start=True, stop=True),
                              reads=[Lk, Rk], writes=[('ps', b2 + par)])
                sc.op('dve', lambda e: e.tensor_tensor(out=dst[:].rearrange("p (c q) t -> p q c t", q=2),
                                                       in0=K.psall[:, b2 * 512:(b2 + 2) * 512].rearrange("p (q c t) -> p q c t", q=2, t=128),
                                                       in1=mask[:].unsqueeze(1).unsqueeze(1).broadcast_to([128, 2, 4, 128]), op=ALU.mult),
                      reads=[('ps', b2), ('ps', b2 + 1)], writes=[dk])
            headmm(kt, 'b_kt', ab, 'b_ab', K.m_lt, Pm[0], 'b_P0')
            headmm(ab, 'b_ab', kt, 'b_kt', K.m_gt, Qm[0], 'b_Q0')
            if DBG["stage"] <= 4:
                continue
            sc.op('dve', lambda e: e.tensor_tensor(out=Xm[0][:], in0=K.ident[:].unsqueeze(1).broadcast_to(H3), in1=Qm[0][:].bitcast(F32), op=ALU.subtract), reads=['ident', 'b_Q0'], writes=['b_X0'])
            cur = 0
            for lev in range(1, 7):
                nxt = 1 - cur
                Pc, Qc, Xc = Pm[cur], Qm[cur], Xm[cur]
                Pn, Qn, Xn = Pm[nxt], Qm[nxt], Xm[nxt]
                def ps8(b2):
                    return K.psall[:, b2 * 512:(b2 + 2) * 512].rearrange("p (h t) -> p h t", t=128)
                bP = K.nb2()
                for hd in range(8):
                    sc.op('pe', lambda e, hd=hd: e.matmul(K.ps[bP + hd // 4][:, (hd % 4) * 128:(hd % 4 + 1) * 128], Qc[:, hd, :], Pc[:, hd, :], start=True, stop=True),
                          reads=[f'b_Q{cur}', f'b_P{cur}'], writes=[('ps', bP + hd // 4)])
                if lev < 6:
                    bQ = K.nb2()
                    for hd in range(8):
                        sc.op('pe', lambda e, hd=hd: e.matmul(K.ps[bQ + hd // 4][:, (hd % 4) * 128:(hd % 4 + 1) * 128], Pc[:, hd, :], Qc[:, hd, :], start=True, stop=True),
                              reads=[f'b_Q{cur}', f'b_P{cur}'], writes=[('ps', bQ + hd // 4)])
                sc.op('act', lambda e: e.copy(out=Pn[:], in_=ps8(bP)), reads=[('ps', bP), ('ps', bP + 1)], writes=[f'b_P{nxt}'])
                if lev < 6:
                    sc.op('dve', lambda e: e.tensor_copy(out=Qn[:], in_=ps8(bQ)), reads=[('ps', bQ), ('ps', bQ + 1)], writes=[f'b_Q{nxt}'])
                bX = K.nb2()
                for hd in range(8):
                    sc.op('pe', lambda e, hd=hd: e.matmul(K.ps[bX + hd // 4][:, (hd % 4) * 128:(hd % 4 + 1) * 128], Pn[:, hd, :], Xc[:, hd, :], start=True, stop=True),
                          reads=[f'b_P{nxt}', 'b_X0'], writes=[('ps', bX + hd // 4)])
                sc.op('dve', lambda e: e.tensor_tensor(out=Xn[:], in0=ps8(bX), in1=Xc[:].bitcast(F32), op=ALU.add),
                      reads=[('ps', bX), ('ps', bX + 1), 'b_X0'], writes=['b_X0'])
                cur = nxt
            if DBG["stage"] <= 5:
                continue
            X = Xm[cur]
            headmm(kb, 'b_kb', kt, 'b_kt', K.m_gt, BT, 'b_P0')
            headmm(ab, 'b_ab', rt, 'b_rt', K.m_ge, ArT, 'b_P1')
            headmm(kb, 'b_kb', rt, 'b_rt', K.m_ge, BrT, 'b_Q0')
            Xk = 'b_X0'
            S0 = ST[ti % 2]
            S0k = f'b_ST{ti % 2}'
            S1 = ST[1 - ti % 2]
            S1k = f'b_ST{1 - ti % 2}'
            def q4(ap, par):
                return ap.rearrange("p (c q i) -> p c q i", q=2, i=64)[:, :, par, :]
            def ps_q(b2):
                return K.psall[:, b2 * 512:(b2 + 2) * 512].rearrange("p (q x) -> p q x", q=2)[:, :, 0:256].rearrange("p q (c i) -> p q c i", i=64)

            def sb_q(ap):
                return ap.rearrange("p (c q i) -> p q c i", q=2, i=64)
            bz2 = K.nb2()
            for par in range(2):
                bz = bz2 + par
                p0 = 64 * par
                for c in range(4):
                    hd = 2 * c + par
                    sc.op('pe', lambda e, hd=hd, c=c: e.matmul(K.ps[bz][:, c * 64:(c + 1) * 64], kt[p0:p0 + 64, c, :], S0[p0:p0 + 64, c, p0:p0 + 64], start=True, stop=False),
                          reads=['b_kt', S0k], writes=[('ps', bz)])
                    sc.op('pe', lambda e, hd=hd, c=c: e.matmul(K.ps[bz][:, c * 64:(c + 1) * 64], BT[:, hd, :], Vt[:, hd * 64:(hd + 1) * 64], start=False, stop=True),
                          reads=['b_P0', 'b_V'], writes=[('ps', bz)])
            sc.op('act', lambda e: e.mul(out=sb_q(Zs), in_=ps_q(bz2), mul=-1.0), reads=[('ps', bz2), ('ps', bz2 + 1)], writes=['b_Q1'])
            bu = K.nb()
            for hd in range(8):
                sc.op('pe', lambda e, hd=hd: e.matmul(K.ps[bu][:, hd * 64:(hd + 1) * 64], X[:, hd, :], Zs[:, hd * 64:(hd + 1) * 64], start=True, stop=True),
                      reads=[Xk, 'b_Q1'], writes=[('ps', bu)])
            sc.op('act', lambda e: e.copy(out=U, in_=K.ps[bu][:, 0:512]), reads=[('ps', bu)], writes=['b_Q1'])
            by2 = K.nb2()
            for par in range(2):
                by = by2 + par
                p0 = 64 * par
                for c in range(4):
                    hd = 2 * c + par
                    sc.op('pe', lambda e, hd=hd, c=c: e.matmul(K.ps[by][:, c * 64:(c + 1) * 64], rt[p0:p0 + 64, c, :], S0[p0:p0 + 64, c, p0:p0 + 64], start=True, stop=False),
                          reads=['b_rt', S0k], writes=[('ps', by)])
                    sc.op('pe', lambda e, hd=hd, c=c: e.matmul(K.ps[by][:, c * 64:(c + 1) * 64], ArT[:, hd, :], U[:, hd * 64:(hd + 1) * 64], start=False, stop=False),
                          reads=['b_P1', 'b_Q1'], writes=[('ps', by)])
                    sc.op('pe', lambda e, hd=hd, c=c: e.matmul(K.ps[by][:, c * 64:(c + 1) * 64], BrT[:, hd, :], Vt[:, hd * 64:(hd + 1) * 64], start=False, stop=True),
                          reads=['b_Q0', 'b_V'], writes=[('ps', by)])
            sc.op('act', lambda e: e.copy(out=sb_q(ysb[:]), in_=ps_q(by2)), reads=[('ps', by2), ('ps', by2 + 1)], writes=['b_ysb'])
            bs2 = K.nb()
            for c in range(4):
                cs = slice(c * 128, (c + 1) * 128)
                sc.op('pe', lambda e, cs=cs: e.matmul(K.ps[bs2][:, cs], ab_tm[:, cs], U[:, cs], start=True, stop=False), reads=['b_abtm', 'b_Q1'], writes=[('ps', bs2)])
                sc.op('pe', lambda e, cs=cs: e.matmul(K.ps[bs2][:, cs], kb_tm[:, cs], Vt[:, cs], start=False, stop=False), reads=['b_kbtm', 'b_V'], writes=[('ps', bs2)])
                sc.op('pe', lambda e, cs=cs, c=c: e.matmul(K.ps[bs2][:, cs], K.identr[:], S0[:, c, :], start=False, stop=True), reads=['identr', S0k], writes=[('ps', bs2)])
            sc.op('dve', lambda e: e.tensor_tensor(out=S1[:], in0=v4(K.ps[bs2][:, 0:512]), in1=Wt[:, :, 127:128].broadcast_to(V3), op=ALU.mult), reads=[('ps', bs2), 'b_Wt'], writes=[S1k])
            if DBG["stage"] <= 6:
                continue
            sc.op('dve', lambda e: e.tensor_reduce(out=s1[:], in_=v8(ysb[:]), axis=AX.X, op=ALU.add), reads=['b_ysb'], writes=['b_s1'])
            sc.op('dve', lambda e: e.tensor_single_scalar(out=s1[:], in_=s1[:], scalar=1.0 / 64, op=ALU.mult), reads=['b_s1'], writes=['b_s1'])
            sc.op('dve', lambda e: e.tensor_tensor(out=v8(ysb[:]), in0=v8(ysb[:]), in1=s1[:].unsqueeze(2).broadcast_to([128, 8, 64]), op=ALU.subtract), reads=['b_ysb', 'b_s1'], writes=['b_ysb'])
            sc.op('act', lambda e: e.activation(out=res[:, 0:512], in_=ysb[:], func=AF.Square), reads=['b_ysb'], writes=['b_zl'])
            sc.op('dve', lambda e: e.tensor_reduce(out=s2[:], in_=v8(res[:, 0:512]), axis=AX.X, op=ALU.add), reads=['b_zl'], writes=['b_s2'])
            sc.op('act', lambda e: e.activation(out=s2[:], in_=s2[:], func=AF.Sqrt, bias=K.eps_lnx[:, 0:1], scale=1.0 / 64), reads=['b_s2', 'eps_lnx'], writes=['b_s2'])
            sc.op('dve', lambda e: e.reciprocal(out=s2[:], in_=s2[:]), reads=['b_s2'], writes=['b_s2'])
            sc.op('dve', lambda e: e.tensor_tensor(out=v8(ysb[:]), in0=v8(ysb[:]), in1=s2[:].unsqueeze(2).broadcast_to([128, 8, 64]), op=ALU.mult), reads=['b_ysb', 'b_s2'], writes=['b_ysb'])
            sc.op('dve', lambda e: e.tensor_tensor(out=ysb[:], in0=ysb[:], in1=lnxg[:], op=ALU.mult), reads=['b_ysb', 'b_lnxg'], writes=['b_ysb'])
            sc.op('dve', lambda e: e.tensor_tensor(out=ysb[:], in0=ysb[:], in1=lnxb[:], op=ALU.add), reads=['b_ysb', 'b_lnxb'], writes=['b_ysb'])
            sc.op('dve', lambda e: e.tensor_tensor(out=v8(res[:, 512:1024]), in0=v8(Vt[:].bitcast(F32)), in1=bs[:].unsqueeze(2).broadcast_to([128, 8, 64]), op=ALU.mult), reads=['b_V', 'b_bs', 'b_zl'], writes=['b_zl'])
            sc.op('dve', lambda e: e.tensor_tensor(out=ysb[:], in0=ysb[:], in1=res[:, 512:1024], op=ALU.add), reads=['b_ysb', 'b_zl'], writes=['b_ysb'])
            sc.op('dve', lambda e: e.tensor_tensor(out=ysb[:], in0=ysb[:], in1=g_tm[:], op=ALU.mult), reads=['b_ysb', 'b_gtm'], writes=['b_ysb'])
            if DBG["stage"] <= 7:
                continue
            sc.dma('pool', lambda e: e.dma_start(out=mixT[:, 0:4, :], in_=S.aT[:, :, ti * 128:(ti + 1) * 128].rearrange("c p t -> p c t")), writes=['b_hT'])
            transpose_to(nc, sc, K, ysb, 'b_ysb', lambda c0, nn: mixT[:, 4 + c0:4 + c0 + nn, :], 'b_hT', 4, evac='act')
            for hf in range(2):
                b = K.nb()
                for k in range(8):
                    sc.op('pe', lambda e, k=k: e.matmul(K.ps[b][:, 0:512], mixT[:, k, :], wO[:, k, hf * 512:(hf + 1) * 512], start=(k == 0), stop=(k == 7)),
                          reads=['b_hT', 'b_hT', 'b_wO'], writes=[('ps', b)])
                sc.op('dve', lambda e: e.scalar_tensor_tensor(out=res[:, hf * 512:(hf + 1) * 512], in0=h[:, hf * 512:(hf + 1) * 512], scalar=ALPHA, in1=K.ps[b][:, 0:512], op0=ALU.mult, op1=ALU.add),
                      reads=[hk, ('ps', b)], writes=['b_zl'])
            if DBG["stage"] <= 8:
                continue
            layernorm_tm(sc, K, res, 'b_zl', ln1g, 'b_ln1g', ln1b, 'b_ln1b', h, hk, lt)
            store_h_and_hT(nc, sc, K, h, hk, hT, 'b_hT', S.h1, S.h1T, ti)
        sc.flush()


def ffn_generic(nc, sc, K, pfx, hT_d, h_d, experts, FF, gates_d, ln_g, ln_b, out_fn, FP=2):
    nfg = FF // (128 * FP)
    groups = [(0, 9), (9, 8), (17, 8), (25, 8)]
    with ExitStack() as st:
        sb = K.sb
        GM = 9 * 128
        hT = sb(st, pfx + "hT", [128, 8, GM], F32R)
        yac = sb(st, pfx + "yac", [128, 9, D], F32)
        actT = sb(st, pfx + "actT", [128, FP, GM], F32R)
        sg = sb(st, pfx + "sg", [128, 512], F32)
        wg = [sb(st, pfx + f"wg{i}", [128, 8, FP * 128], F32R) for i in range(2)]
        wu = [sb(st, pfx + f"wu{i}", [128, 8, FP * 128], F32R) for i in range(2)]
        wd = [sb(st, pfx + f"wd{i}", [128, FP, D], F32R) for i in range(2)]
        gbc = load_bcast(nc, sc, K, st, pfx + "lng", ln_g, D)
        bbc = load_bcast(nc, sc, K, st, pfx + "lnb", ln_b, D)
        gt = sb(st, pfx + "gt", [128, 9, NEXP], F32)
        hoT = sb(st, pfx + "hoT", [128, 8, 128], F32R) if gates_d is None else None
        lt = ln_tmp(K, st, pfx + "ln")
        it = 0
        for (t0, nt) in groups[:DBG.get('fng', 4)]:
            n = nt * 128
            for k in range(8):
                sc.dma('pool', lambda e, k=k: e.dma_start(out=hT[:, k, 0:n], in_=hT_d[k, :, t0 * 128:t0 * 128 + n]), writes=[pfx + 'hT'])
            sc.dma('sp', lambda e: e.dma_start(out=yac[:, 0:nt, :], in_=h_d[t0 * 128:t0 * 128 + n, :].rearrange("(j p) d -> p j d", p=128)), writes=[pfx + 'yac'])
            sc.op('act', lambda e: e.mul(out=yac[:, 0:nt, :], in_=yac[:, 0:nt, :], mul=ALPHA), reads=[pfx + 'yac'], writes=[pfx + 'yac'])
            if gates_d is not None:
                sc.dma('sp', lambda e: e.dma_start(out=gt[:, 0:nt, :], in_=gates_d[t0 * 128:t0 * 128 + n, :].rearrange("(j p) d -> p j d", p=128)), writes=[pfx + 'gt'])
            spans = [(0, 3), (3, 3), (6, 3)] if nt == 9 else [(0, 4), (4, 4)]
            if DBG.get('fst', 99) <= 1:
                continue
            for ei, (Wg, Wu, Wd) in enumerate(experts):
                for fg in range(nfg):
                    bi = it % 2
                    it += 1
                    f0 = fg * FP * 128
                    sc.dma('pool', lambda e: e.dma_start(out=wg[bi][:], in_=Wg[:, f0:f0 + FP * 128].rearrange("(k p) f -> p k f", p=128)), writes=[pfx + f'wg{bi}'])
                    sc.dma('pool', lambda e: e.dma_start(out=wu[bi][:], in_=Wu[:, f0:f0 + FP * 128].rearrange("(k p) f -> p k f", p=128)), writes=[pfx + f'wu{bi}'])
                    sc.dma('pool', lambda e: e.dma_start(out=wd[bi][:], in_=Wd[f0:f0 + FP * 128, :].rearrange("(c p) d -> p c d", p=128)), writes=[pfx + f'wd{bi}'])
                    for (s0, sn) in spans:
                        c0 = s0 * 128
                        m = sn * 128
                        for fc in range(FP):
                            bgp = K.nb()
                            bup = K.nb()
                            for k in range(8):
                                sc.op('pe', lambda e, k=k: e.matmul(K.ps[bgp][:, 0:m], wg[bi][:, k, fc * 128:(fc + 1) * 128], hT[:, k, c0:c0 + m], start=(k == 0), stop=(k == 7)),
                                      reads=[pfx + f'wg{bi}', pfx + 'hT'], writes=[('ps', bgp)])
                            for k in range(8):
                                sc.op('pe', lambda e, k=k: e.matmul(K.ps[bup][:, 0:m], wu[bi][:, k, fc * 128:(fc + 1) * 128], hT[:, k, c0:c0 + m], start=(k == 0), stop=(k == 7)),
                                      reads=[pfx + f'wu{bi}', pfx + 'hT'], writes=[('ps', bup)])
                            sc.op('act', lambda e: e.activation(out=sg[:, 0:m], in_=K.ps[bgp][:, 0:m], func=AF.Silu), reads=[('ps', bgp)], writes=[pfx + 'sg'])
                            sc.op('dve', lambda e: e.tensor_tensor(out=actT[:, fc, c0:c0 + m], in0=K.ps[bup][:, 0:m], in1=sg[:, 0:m], op=ALU.mult),
                                  reads=[('ps', bup), pfx + 'sg'], writes=[pfx + 'actT'])
                    for j in range(nt):
                        for hf in range(2):
                            b = K.nb()
                            for fc in range(FP):
                                sc.op('pe', lambda e, fc=fc: e.matmul(K.ps[b][:, 0:512], actT[:, fc, j * 128:(j + 1) * 128], wd[bi][:, fc, hf * 512:(hf + 1) * 512], start=(fc == 0), stop=(fc == FP - 1)),
                                      reads=[pfx + 'actT', pfx + f'wd{bi}'], writes=[('ps', b)])
                            ysl = yac[:, j, hf * 512:(hf + 1) * 512]
                            if gates_d is not None:
                                sc.op('dve', lambda e: e.scalar_tensor_tensor(out=ysl, in0=K.ps[b][:, 0:512], scalar=gt[:, j, ei:ei + 1], in1=ysl, op0=ALU.mult, op1=ALU.add),
                                      reads=[('ps', b), pfx + 'gt', pfx + 'yac'], writes=[pfx + 'yac'])
                            else:
                                sc.op('dve', lambda e: e.tensor_tensor(out=ysl, in0=K.ps[b][:, 0:512], in1=ysl, op=ALU.add), reads=[('ps', b), pfx + 'yac'], writes=[pfx + 'yac'])
            if DBG.get('fst', 99) <= 2:
                continue
            for j in range(nt):
                ho = yac[:, j, :]
                layernorm_tm(sc, K, ho, pfx + 'yac', gbc, pfx + 'lng', bbc, pfx + 'lnb', ho, pfx + 'yac', lt)
                if DBG.get('fst', 99) <= 3:
                    continue
                out_fn(t0 + j, ho, pfx + 'yac', hoT, pfx + 'hoT')
        sc.flush()


def phase_ffn0(nc, sc, K, I, S):
    def out_fn(ti, ho, hok, hoT, hoTk):
        store_h_and_hT(nc, sc, K, ho, hok, hoT, hoTk, S.h2, S.h2T, ti)
    ffn_generic(nc, sc, K, "f_", S.h1T, S.h1, [(I.f_gate, I.f_up, I.f_down)], DFF0, None, I.ln2_g, I.ln2_b, out_fn)


def phase_moe(nc, sc, K, I, S):
    def out_fn(ti, ho, hok, hoT, hoTk):
        if ti == 0:
            return
        sc.dma('sp', lambda e: e.dma_start(out=I.out[(ti - 1) * 128:ti * 128, :], in_=ho[:]), reads=[hok], writes=['out'])
    experts = [(I.e_gate[e], I.e_up[e], I.e_down[e]) for e in range(NEXP)]
    ffn_generic(nc, sc, K, "m_", S.h3T, S.h3, experts, DFFE, S.gates, I.oln2_g, I.oln2_b, out_fn, FP=4)


def phase_attn(nc, sc, K, I, S):
    with ExitStack() as st:
        sb = K.sb
        wq = sb(st, "t_wq", [128, 8, 1280], F32R)
        load_weight_r(nc, sc, K, wq, 't_wq', I.w_qkv, 8, 1280)
        wo = sb(st, "t_wo", [128, 8, 1024], F32R)
        load_weight_r(nc, sc, K, wo, 't_wo', I.w_o, 8, 1024)
        bq = load_bcast(nc, sc, K, st, "t_bq", I.b_qkv, 1280)
        bo = load_bcast(nc, sc, K, st, "t_bo", I.b_o, D)
        g1 = load_bcast(nc, sc, K, st, "t_g1", I.oln1_g, D)
        b1 = load_bcast(nc, sc, K, st, "t_b1", I.oln1_b, D)
        snk = load_bcast(nc, sc, K, st, "t_snk", I.sinks, 16)
        sc.op('act', lambda e: e.activation(out=snk[:], in_=snk[:], func=AF.Exp), reads=['t_snk'], writes=['t_snk'])
        rtr = sb(st, "t_rtr", [128, 8, NEXP], F32R)
        sc.dma('pool', lambda e: e.dma_start(out=rtr[:], in_=I.router.rearrange("(k p) e -> p k e", p=128)), writes=['t_rtr'])
        rowi = sb(st, "t_rowi", [128, 128], I32)
        rowm = sb(st, "t_rowm", [128, 128], F32)
        tmpm = sb(st, "t_tmpm", [128, 128], F32)
        sc.op('pool', lambda e: e.iota(rowi[:], pattern=[[0, 128]], base=-PADF, channel_multiplier=1), writes=['t_rowi'])
        sc.op('dve', lambda e: e.tensor_single_scalar(out=rowm[:], in_=rowi[:], scalar=0, op=ALU.is_ge), reads=['t_rowi'], writes=['t_rowm'])
        mbs = {}
        for nm, m, rm in (('cur', K.m_ge, False), ('prev', K.m_lt, False), ('cur0', K.m_ge, True), ('prev1', K.m_lt, True)):
            t = sb(st, "t_mb_" + nm, [128, 128], F32R)
            if rm:
                sc.op('dve', lambda e, m=m: e.tensor_tensor(out=tmpm[:], in0=m[:], in1=rowm[:], op=ALU.mult), reads=['t_rowm', 't_tmpm'], writes=['t_tmpm'])
                sc.op('dve', lambda e, t=t: e.tensor_scalar(out=t[:], in0=tmpm[:], scalar1=-1.0, scalar2=30000.0, op0=ALU.add, op1=ALU.mult), reads=['t_tmpm'], writes=['t_mb'])
            else:
                sc.op('dve', lambda e, t=t, m=m: e.tensor_scalar(out=t[:], in0=m[:], scalar1=-1.0, scalar2=30000.0, op0=ALU.add, op1=ALU.mult), writes=['t_mb'])
            mbs[nm] = t
        hT2 = sb(st, "t_hT", [128, 8, 128], F32R)
        h2 = sb(st, "t_h2", [128, D], F32)
        qkv = sb(st, "t_qkv", [128, 1280], F32)
        qr = sb(st, "t_qr", [128, 1152], F32)
        ta = sb(st, "t_ta", [128, 8, 32], F32)
        tb = sb(st, "t_tb", [128, 8, 32], F32)
        cs = sb(st, "t_cos", [128, 32], F32)
        sn = sb(st, "t_sin", [128, 32], F32)
        qT = sb(st, "t_qT", [128, 8, 128], F32R)
        kT = [sb(st, f"t_kT{i}", [128, 128], F32R) for i in range(2)]
        v1 = [sb(st, f"t_v{i}", [128, 2, 66], F32R) for i in range(2)]
        Ec = sb(st, "t_Ec", [128, 512], F32R)
        Ep = sb(st, "t_Ep", [128, 512], F32R)
        osb = sb(st, "t_osb", [128, 16, 66], F32)
        den = sb(st, "t_den", [128, 16], F32)
        att = sb(st, "t_att", [128, D], F32)
        attT = sb(st, "t_attT", [128, 8, 128], F32R)
        res = sb(st, "t_res", [128, D], F32)
        h3 = sb(st, "t_h3", [128, D], F32)
        h3T = sb(st, "t_h3T", [128, 8, 128], F32R)
        lg = sb(st, "t_lg", [128, 8], F32)
        m8 = sb(st, "t_m8", [128, 8], F32)
        ex = sb(st, "t_ex", [128, 8], F32)
        gsm = sb(st, "t_gsm", [128, 2], F32)
        lt = ln_tmp(K, st, "t_ln")
        for i in range(2):
            sc.op('dve', lambda e, i=i: e.tensor_copy(out=v1[i][:, :, 0:64], in_=K.zero_f[:, 0:128].rearrange("p (g d) -> p g d", g=2)), reads=['zero_f'], writes=[f't_v{i}'])
            sc.op('dve', lambda e, i=i: e.tensor_copy(out=v1[i][:, :, 64:65], in_=K.ones_f[:, 0:2].unsqueeze(2)), reads=['ones_f', f't_v{i}'], writes=[f't_v{i}'])
            sc.op('dve', lambda e, i=i: e.tensor_copy(out=v1[i][:, :, 65:66], in_=K.zero_f[:, 0:2].unsqueeze(2)), reads=['zero_f', f't_v{i}'], writes=[f't_v{i}'])
        B83 = [128, 8, 32]
        for ti in range(NT):
            cu, pv = ti % 2, 1 - ti % 2
            sc.dma('pool', lambda e: e.dma_start(out=hT2[:], in_=S.h2T[:, :, ti * 128:(ti + 1) * 128].rearrange("c p t -> p c t")), writes=['t_hT'])
            sc.dma('sp', lambda e: e.dma_start(out=h2[:], in_=S.h2[ti * 128:(ti + 1) * 128, :]), writes=['t_h2'])
            sc.dma('sp', lambda e: e.dma_start(out=cs[:], in_=I.rope_cos[ti * 128:(ti + 1) * 128, :]), writes=['t_cos'])
            sc.dma('sp', lambda e: e.dma_start(out=sn[:], in_=I.rope_sin[ti * 128:(ti + 1) * 128, :]), writes=['t_sin'])
            for (c0, cw) in ((0, 512), (512, 512), (1024, 256)):
                b = K.nb()
                for k in range(8):
                    sc.op('pe', lambda e, k=k: e.matmul(K.ps[b][:, 0:cw], hT2[:, k, :], wq[:, k, c0:c0 + cw], start=(k == 0), stop=(k == 7)), reads=['t_hT', 't_wq'], writes=[('ps', b)])
                sc.op('dve', lambda e: e.tensor_tensor(out=qkv[:, c0:c0 + cw], in0=K.ps[b][:, 0:cw], in1=bq[:, c0:c0 + cw], op=ALU.add), reads=[('ps', b), 't_bq'], writes=['t_qkv'])
            cb = cs[:].unsqueeze(1)
            sb_ = sn[:].unsqueeze(1)
            parts = []
            for g in range(2):
                vin = qkv[:, g * 512:(g + 1) * 512].rearrange("p (c two d) -> p c two d", two=2, d=32)
                vout = qr[:, 0:1024].rearrange("p (c g two d) -> p c g two d", g=2, two=2, d=32)
                parts.append((8, vin[:, :, 0, :], vin[:, :, 1, :], vout[:, :, g, 0, :], vout[:, :, g, 1, :]))
            vin = qkv[:, 1024:1152].rearrange("p (c two d) -> p c two d", two=2, d=32)
            vout = qr[:, 1024:1152].rearrange("p (c two d) -> p c two d", two=2, d=32)
            parts.append((2, vin[:, :, 0, :], vin[:, :, 1, :], vout[:, :, 0, :], vout[:, :, 1, :]))
            for (nh, x1, x2, o1, o2) in parts:
                shp = [128, nh, 32]
                cbb = cb.broadcast_to(shp)
                sbb = sb_.broadcast_to(shp)
                sc.op('dve', lambda e: e.tensor_tensor(out=ta[:, 0:nh, :], in0=x1, in1=cbb, op=ALU.mult), reads=['t_qkv', 't_cos'], writes=['t_ta'])
                sc.op('dve', lambda e: e.tensor_tensor(out=tb[:, 0:nh, :], in0=x2, in1=sbb, op=ALU.mult), reads=['t_qkv', 't_sin'], writes=['t_tb'])
                sc.op('dve', lambda e: e.tensor_tensor(out=o1, in0=ta[:, 0:nh, :], in1=tb[:, 0:nh, :], op=ALU.subtract), reads=['t_ta', 't_tb'], writes=['t_qr'])
                sc.op('dve', lambda e: e.tensor_tensor(out=ta[:, 0:nh, :], in0=x2, in1=cbb, op=ALU.mult), reads=['t_qkv', 't_cos', 't_ta'], writes=['t_ta'])
                sc.op('dve', lambda e: e.tensor_tensor(out=tb[:, 0:nh, :], in0=x1, in1=sbb, op=ALU.mult), reads=['t_qkv', 't_sin', 't_tb'], writes=['t_tb'])
                sc.op('dve', lambda e: e.tensor_tensor(out=o2, in0=ta[:, 0:nh, :], in1=tb[:, 0:nh, :], op=ALU.add), reads=['t_ta', 't_tb'], writes=['t_qr'])
            transpose_to(nc, sc, K, qr, 't_qr', lambda c0, nn: qT[:, c0:c0 + nn, :], 't_qT', 8, evac='act')
            bk = K.nb()
            sc.op('pe', lambda e: e.transpose(K.ps[bk][:, 0:128], qr[:, 1024:1152], K.ident[:]), reads=['t_qr', 'ident'], writes=[('ps', bk)])
            sc.op('act', lambda e: e.copy(out=kT[cu][:], in_=K.ps[bk][:, 0:128]), reads=[('ps', bk)], writes=[f't_kT{cu}'])
            sc.op('act', lambda e: e.copy(out=v1[cu][:, :, 0:64], in_=qkv[:, 1152:1280].rearrange("p (g d) -> p g d", g=2)), reads=['t_qkv'], writes=[f't_v{cu}'])
            mcur = mbs['cur0'] if ti == 0 else mbs['cur']
            mprev = mbs['prev1'] if ti == 1 else mbs['prev']
            for g in range(2):
                p0 = 64 * g
                for hf in range(2):
                    bc_ = K.nb()
                    for j in range(4):
                        c = hf * 4 + j
                        sc.op('pe', lambda e, j=j, c=c: e.matmul(K.ps[bc_][:, j * 128:(j + 1) * 128], kT[cu][p0:p0 + 64, :], qT[p0:p0 + 64, c, :], start=True, stop=False), reads=[f't_kT{cu}', 't_qT'], writes=[('ps', bc_)])
                        sc.op('pe', lambda e, j=j: e.matmul(K.ps[bc_][:, j * 128:(j + 1) * 128], K.identr[:], mcur[:], start=False, stop=True), reads=['identr', 't_mb'], writes=[('ps', bc_)])
                    sc.op('act', lambda e: e.activation(out=Ec[:], in_=K.ps[bc_][:, 0:512], func=AF.Exp, scale=0.125), reads=[('ps', bc_)], writes=['t_Ec'])
                    if ti > 0:
                        bp_ = K.nb()
                        for j in range(4):
                            c = hf * 4 + j
                            sc.op('pe', lambda e, j=j, c=c: e.matmul(K.ps[bp_][:, j * 128:(j + 1) * 128], kT[pv][p0:p0 + 64, :], qT[p0:p0 + 64, c, :], start=True, stop=False), reads=[f't_kT{pv}', 't_qT'], writes=[('ps', bp_)])
                            sc.op('pe', lambda e, j=j: e.matmul(K.ps[bp_][:, j * 128:(j + 1) * 128], K.identr[:], mprev[:], start=False, stop=True), reads=['identr', 't_mb'], writes=[('ps', bp_)])
                        sc.op('act', lambda e: e.activation(out=Ep[:], in_=K.ps[bp_][:, 0:512], func=AF.Exp, scale=0.125), reads=[('ps', bp_)], writes=['t_Ep'])
                    bo_ = K.nb()
                    for j in range(4):
                        osl = K.ps[bo_][:, j * 66:(j + 1) * 66]
                        if ti > 0:
                            sc.op('pe', lambda e, j=j, osl=osl: e.matmul(osl, Ep[:, j * 128:(j + 1) * 128], v1[pv][:, g, :], start=True, stop=False), reads=['t_Ep', f't_v{pv}'], writes=[('ps', bo_)])
                        sc.op('pe', lambda e, j=j, osl=osl: e.matmul(osl, Ec[:, j * 128:(j + 1) * 128], v1[cu][:, g, :], start=(ti == 0), stop=True), reads=['t_Ec', f't_v{cu}'], writes=[('ps', bo_)])
                    h0 = 8 * g + 4 * hf
                    sc.op('act', lambda e: e.copy(out=osb[:, h0:h0 + 4, :], in_=K.ps[bo_][:, 0:264].rearrange("p (h d) -> p h d", d=66)), reads=[('ps', bo_)], writes=['t_osb'])
            sc.op('dve', lambda e: e.tensor_tensor(out=den[:].unsqueeze(2), in0=osb[:, :, 64:65], in1=snk[:].unsqueeze(2), op=ALU.add), reads=['t_osb', 't_snk'], writes=['t_den'])
            sc.op('dve', lambda e: e.reciprocal(out=den[:], in_=den[:]), reads=['t_den'], writes=['t_den'])
            sc.op('dve', lambda e: e.tensor_tensor(out=att[:].rearrange("p (h d) -> p h d", d=64), in0=osb[:, :, 0:64], in1=den[:].unsqueeze(2).broadcast_to([128, 16, 64]), op=ALU.mult), reads=['t_osb', 't_den'], writes=['t_att'])
            transpose_to(nc, sc, K, att, 't_att', lambda c0, nn: attT[:, c0:c0 + nn, :], 't_attT', 8, evac='act')
            for hf in range(2):
                b = K.nb()
                for k in range(8):
                    sc.op('pe', lambda e, k=k: e.matmul(K.ps[b][:, 0:512], attT[:, k, :], wo[:, k, hf * 512:(hf + 1) * 512], start=(k == 0), stop=(k == 7)), reads=['t_attT', 't_wo'], writes=[('ps', b)])
                sc.op('dve', lambda e: e.scalar_tensor_tensor(out=res[:, hf * 512:(hf + 1) * 512], in0=h2[:, hf * 512:(hf + 1) * 512], scalar=ALPHA, in1=K.ps[b][:, 0:512], op0=ALU.mult, op1=ALU.add), reads=['t_h2', ('ps', b)], writes=['t_res'])
            sc.op('dve', lambda e: e.tensor_tensor(out=res[:], in0=res[:], in1=bo[:], op=ALU.add), reads=['t_res', 't_bo'], writes=['t_res'])
            layernorm_tm(sc, K, res, 't_res', g1, 't_g1', b1, 't_b1', h3, 't_h3', lt)
            store_h_and_hT(nc, sc, K, h3, 't_h3', h3T, 't_h3T', S.h3, S.h3T, ti)
            br = K.nb()
            for k in range(8):
                sc.op('pe', lambda e, k=k: e.matmul(K.ps[br][:, 0:8], h3T[:, k, :], rtr[:, k, :], start=(k == 0), stop=(k == 7)), reads=['t_h3T', 't_rtr'], writes=[('ps', br)])
            sc.op('act', lambda e: e.copy(out=lg[:], in_=K.ps[br][:, 0:8]), reads=[('ps', br)], writes=['t_lg'])
            sc.op('dve', lambda e: e.max(out=m8[:], in_=lg[:]), reads=['t_lg'], writes=['t_m8'])
            sc.op('dve', lambda e: e.tensor_single_scalar(out=gsm[:, 0:1], in_=m8[:, 0:1], scalar=-1.0, op=ALU.mult), reads=['t_m8'], writes=['t_gsm'])
            sc.op('act', lambda e: e.activation(out=ex[:], in_=lg[:], func=AF.Exp, bias=gsm[:, 0:1], scale=1.0), reads=['t_lg', 't_gsm'], writes=['t_ex'])
            sc.op('dve', lambda e: e.tensor_scalar(out=lg[:], in0=lg[:], scalar1=m8[:, 1:2], scalar2=None, op0=ALU.is_ge), reads=['t_lg', 't_m8'], writes=['t_lg'])
            sc.op('dve', lambda e: e.tensor_tensor(out=ex[:], in0=ex[:], in1=lg[:], op=ALU.mult), reads=['t_ex', 't_lg'], writes=['t_ex'])
            sc.op('dve', lambda e: e.tensor_reduce(out=gsm[:, 1:2], in_=ex[:], axis=AX.X, op=ALU.add), reads=['t_ex', 't_gsm'], writes=['t_gsm'])
            sc.op('dve', lambda e: e.reciprocal(out=gsm[:, 1:2], in_=gsm[:, 1:2]), reads=['t_gsm'], writes=['t_gsm'])
            sc.op('dve', lambda e: e.tensor_scalar(out=ex[:], in0=ex[:], scalar1=gsm[:, 1:2], scalar2=None, op0=ALU.mult), reads=['t_ex', 't_gsm'], writes=['t_ex'])
            sc.dma('sp', lambda e: e.dma_start(out=S.gates[ti * 128:(ti + 1) * 128, :], in_=ex[:]), reads=['t_ex'], writes=['S.gates'])
        sc.flush()


_PARAM_KEYS = ["meta_tokens", "ev_w_in", "ev_conv_w", "ev_conv_b", "ev_convnorm_g", "ev_convnorm_b", "ev_shift_mu",
               "ev_w0", "ev_w2", "ev_a0", "ev_a2", "ev_g2", "ev_k_k", "ev_k_a", "ev_r_k", "ev_lnx_g", "ev_lnx_b",
               "ev_w_out", "ev_ln1_g", "ev_ln1_b", "ev_ffn_gate", "ev_ffn_up", "ev_ffn_down", "ev_ln2_g", "ev_ln2_b",
               "od_w_qkv", "od_b_qkv", "od_sinks", "od_w_o", "od_b_o", "od_ln1_g", "od_ln1_b", "od_router",
               "od_exp_gate", "od_exp_up", "od_exp_down", "od_ln2_g", "od_ln2_b"]


def make_in_maps(inputs, cores):
    shared = {}
    for k in _PARAM_KEYS:
        a = np.asarray(inputs[k], dtype=np.float32)
        if k != "meta_tokens":
            a = a[0]
        if k == "ev_r_k":
            a = a.reshape(512)
        shared[k] = np.ascontiguousarray(a)
    pos = (np.arange(T, dtype=np.float64) - PADF)[:, None]
    inv = 10000.0 ** (-np.arange(32, dtype=np.float64) / 32.0)[None, :]
    shared["rope_cos"] = np.cos(pos * inv).astype(np.float32)
    shared["rope_sin"] = np.sin(pos * inv).astype(np.float32)
    maps = []
    for b in cores:
        m = dict(shared)
        m["x"] = np.ascontiguousarray(np.asarray(inputs["x"][b], dtype=np.float32))
        maps.append(m)
    return maps


def kernel(**inputs):
    nc = build_program()
    in_maps = make_in_maps(inputs, list(range(8)))
    res = run_bass_kernel_spmd(nc, in_maps, core_ids=list(range(8)))
    out = np.stack([np.asarray(r["out"]) for r in res.results], axis=0)
    return out.astype(np.float32)
```
